# Optimizing a Trainium2 kernel written in Bass

```python
import math
import jax
import jax.numpy as jnp
from jax import lax
import numpy as np

D_MODEL = 1024
BATCH = 4
SEQ = 4096
DEPTH = 2

HEAD_DIM = 64
A_HEADS = 6
A_WIDTH = A_HEADS * HEAD_DIM
B_Q_HEADS = 6
B_KV_HEADS = 2
B_WIDTH = B_Q_HEADS * HEAD_DIM
B_KV_WIDTH = B_KV_HEADS * HEAD_DIM
C_HEADS = 4
C_WIDTH = C_HEADS * HEAD_DIM
D_MIX = A_WIDTH + B_WIDTH + C_WIDTH
IN_SPLITS = (3 * A_WIDTH, A_WIDTH, 2 * A_HEADS, 2 * A_HEADS,
             B_WIDTH, B_KV_WIDTH, B_KV_WIDTH,
             2 * C_WIDTH, C_WIDTH, C_WIDTH, C_WIDTH)
D_IN = sum(IN_SPLITS)
CONV_K = 5
GDN_CHUNK = 64
HGRN_CHUNK = 64
Q_BLOCK = 128
GRID_W = 64
ROPE_AXIS_DIM = HEAD_DIM // 2
ROPE_THETA = 10000.0
MEM_TOKENS = 256
X_HEADS = 4
X_HEAD_DIM = D_MODEL // X_HEADS
D_FF = 2816
N_EXPERTS = 8
TOP_K = 2
D_FF_EXPERT = 3584
MOE_BLOCK = 128
N_DENSE = (DEPTH + 1) // 2
N_MOE = DEPTH // 2
DN_ALPHA = (2 * DEPTH) ** 0.25
DN_BETA = (8 * DEPTH) ** -0.25
LN_EPS = 1e-5
RMS_EPS = 1e-6

kernel_name = 'hybrid_parallel_heads_deepnorm_encoder'


def layer_norm(x, g, b):
    xf = x.astype(jnp.float32)
    xc = xf - jnp.mean(xf, axis=-1, keepdims=True)
    var = jnp.mean(xc * xc, axis=-1, keepdims=True)
    return (xc * lax.rsqrt(var + LN_EPS) * g + b).astype(x.dtype)


def rms_norm(x, w):
    xf = x.astype(jnp.float32)
    return (xf * lax.rsqrt(jnp.mean(xf * xf, axis=-1, keepdims=True) + RMS_EPS) * w).astype(x.dtype)


def l2_norm(x):
    xf = x.astype(jnp.float32)
    return xf * lax.rsqrt(jnp.sum(xf * xf, axis=-1, keepdims=True) + RMS_EPS)


def split_last(x, sizes):
    idx = [int(i) for i in np.cumsum(sizes)[:-1]]
    return jnp.split(x, idx, axis=-1)


def to_heads(t, n_heads):
    b, s, w = t.shape
    return t.reshape(b, s, n_heads, w // n_heads).transpose(0, 2, 1, 3)


def flip_seq(t):
    return jnp.flip(t, axis=2)


def centered_depthwise_conv(x, w):
    k, c = w.shape
    return lax.conv_general_dilated(
        x, w[:, None, :].astype(x.dtype), window_strides=(1,), padding=[(k // 2, k // 2)],
        dimension_numbers=('NWC', 'WIO', 'NWC'), feature_group_count=c)


def axial_rope_tables(seq_len):
    rows = seq_len // GRID_W
    row = jnp.repeat(jnp.arange(rows, dtype=jnp.float32), GRID_W)
    col = jnp.tile(jnp.arange(GRID_W, dtype=jnp.float32), rows)
    inv_freq = ROPE_THETA ** (-jnp.arange(0, ROPE_AXIS_DIM, 2, dtype=jnp.float32) / ROPE_AXIS_DIM)
    ang = jnp.stack([row[:, None] * inv_freq, col[:, None] * inv_freq], axis=1)
    return jnp.cos(ang), jnp.sin(ang)


def apply_axial_rope(x, cos, sin):
    xs = x.astype(jnp.float32).reshape(*x.shape[:-1], 2, 2, ROPE_AXIS_DIM // 2)
    x1, x2 = xs[..., 0, :], xs[..., 1, :]
    out = jnp.stack([x1 * cos - x2 * sin, x2 * cos + x1 * sin], axis=-2)
    return out.reshape(x.shape).astype(x.dtype)


def gated_delta_rule(q, k, v, g, beta):
    f32 = jnp.float32
    b, h, s, dk = q.shape
    dv = v.shape[-1]
    c = GDN_CHUNK
    n = s // c
    q = q.astype(f32).reshape(b, h, n, c, dk)
    k = k.astype(f32).reshape(b, h, n, c, dk)
    v = v.astype(f32).reshape(b, h, n, c, dv)
    g = jnp.cumsum(g.astype(f32).reshape(b, h, n, c), axis=-1)
    beta = beta.astype(f32).reshape(b, h, n, c, 1)
    pos = jnp.arange(c)
    incl = pos[:, None] >= pos[None, :]
    strict = pos[:, None] > pos[None, :]
    decay = jnp.exp(jnp.where(incl, g[..., :, None] - g[..., None, :], -jnp.inf))
    k_beta = k * beta
    lower = jnp.where(strict, jnp.einsum('bhnid,bhnjd->bhnij', k_beta, k) * decay, 0.0)
    lhs = lower + jnp.eye(c, dtype=f32)
    rhs = jnp.concatenate([v * beta, k_beta * jnp.exp(g)[..., None]], axis=-1)
    sol = lax.linalg.triangular_solve(lhs, rhs, left_side=True, lower=True)
    u, w = sol[..., :dv], sol[..., dv:]
    attn = jnp.einsum('bhnid,bhnjd->bhnij', q, k) * decay
    q_dec = q * jnp.exp(g)[..., None]
    k_dec = k * jnp.exp(g[..., -1:] - g)[..., None]
    chunk_decay = jnp.exp(g[..., -1])

    def step(state, xs):
        u_n, w_n, attn_n, qd_n, kd_n, cd_n = xs
        v_new = u_n - jnp.einsum('bhcd,bhde->bhce', w_n, state)
        out = (jnp.einsum('bhcd,bhde->bhce', qd_n, state)
               + jnp.einsum('bhij,bhje->bhie', attn_n, v_new))
        state = state * cd_n[..., None, None] + jnp.einsum('bhcd,bhce->bhde', kd_n, v_new)
        return state, out

    xs = tuple(jnp.moveaxis(t, 2, 0) for t in (u, w, attn, q_dec, k_dec, chunk_decay))
    _, out = lax.scan(step, jnp.zeros((b, h, dk, dv), f32), xs)
    return jnp.moveaxis(out, 0, 2).reshape(b, h, s, dv)


def hgrn2_chunked(q, k, v, log_f):
    f32 = jnp.float32
    b, h, s, dk = q.shape
    dv = v.shape[-1]
    c = HGRN_CHUNK
    n = s // c
    q = q.astype(f32).reshape(b, h, n, c, dk)
    k = k.astype(f32).reshape(b, h, n, c, dk)
    v = v.astype(f32).reshape(b, h, n, c, dv)
    cum = jnp.cumsum(log_f.astype(f32).reshape(b, h, n, c, dk), axis=-2)
    q_dec = q * jnp.exp(cum)
    k_dec = k * jnp.exp(cum[..., -1:, :] - cum)
    chunk_decay = jnp.exp(cum[..., -1, :])
    pos = jnp.arange(c)
    incl = (pos[:, None] >= pos[None, :])[:, :, None]

    def step(state, xs):
        q_n, k_n, v_n, cum_n, qd_n, kd_n, cd_n = xs
        rel = jnp.exp(jnp.where(incl, cum_n[..., :, None, :] - cum_n[..., None, :, :], -jnp.inf))
        a = jnp.einsum('bhid,bhjd,bhijd->bhij', q_n, k_n, rel)
        out = (jnp.einsum('bhid,bhde->bhie', qd_n, state)
               + jnp.einsum('bhij,bhje->bhie', a, v_n))
        state = state * cd_n[..., :, None] + jnp.einsum('bhjd,bhje->bhde', kd_n, v_n)
        return state, out

    xs = tuple(jnp.moveaxis(t, 2, 0) for t in (q, k, v, cum, q_dec, k_dec, chunk_decay))
    _, out = lax.scan(step, jnp.zeros((b, h, dk, dv), f32), xs)
    return jnp.moveaxis(out, 0, 2).reshape(b, h, s, dv)


def hgrn_lower_bounds(logits):
    p = jax.nn.softmax(logits.astype(jnp.float32), axis=0)
    cum = jnp.cumsum(p, axis=0)
    return cum - cum[0]


def blocked_bidirectional_gqa(q, k, v):
    b, hq, s, d = q.shape
    hkv = k.shape[1]
    grp = hq // hkv
    nb = s // Q_BLOCK
    qb = q.reshape(b, hkv, grp, nb, Q_BLOCK, d).transpose(3, 0, 1, 2, 4, 5)
    scale = d ** -0.5

    def one_block(qi):
        sc = jnp.einsum('bkgqd,bksd->bkgqs', qi, k).astype(jnp.float32) * scale
        p = jax.nn.softmax(sc, axis=-1).astype(v.dtype)
        return jnp.einsum('bkgqs,bksd->bkgqd', p, v)

    o = lax.map(one_block, qb)
    return o.transpose(1, 0, 4, 2, 3, 5).reshape(b, s, hq * d)


def hybrid_token_mixer(x, rope_cos, rope_sin, w_in, conv_w, a_log, dt_bias, gdn_norm_w,
                       q_norm_w, k_norm_w, lower_bound, hgrn_norm_w, w_out):
    f32 = jnp.float32
    b, s, _ = x.shape
    (a_qkv, a_z, a_beta, a_decay, b_q, b_k, b_v,
     c_f, c_i, c_q, c_g) = split_last(x @ w_in, IN_SPLITS)

    def dir_heads(t):
        return t.astype(f32).reshape(b, s, 2, A_HEADS).transpose(2, 0, 3, 1)

    qkv = jax.nn.silu(centered_depthwise_conv(a_qkv, conv_w))
    aq, ak, av = jnp.split(qkv, 3, axis=-1)
    aq = l2_norm(to_heads(aq, A_HEADS)) * (HEAD_DIM ** -0.5)
    ak = l2_norm(to_heads(ak, A_HEADS))
    av = to_heads(av, A_HEADS)
    beta = jax.nn.sigmoid(dir_heads(a_beta))
    log_decay = (-jnp.exp(a_log.astype(f32))[:, None, :, None]
                 * jax.nn.softplus(dir_heads(a_decay) + dt_bias.astype(f32)[:, None, :, None]))
    oa = gated_delta_rule(aq, ak, av, log_decay[0], beta[0])
    oa = oa + flip_seq(gated_delta_rule(flip_seq(aq), flip_seq(ak), flip_seq(av),
                                        flip_seq(log_decay[1]), flip_seq(beta[1])))
    oa = rms_norm(oa.transpose(0, 2, 1, 3), gdn_norm_w) * jax.nn.silu(
        a_z.astype(f32).reshape(b, s, A_HEADS, HEAD_DIM))
    oa = oa.reshape(b, s, A_WIDTH)

    bq = rms_norm(b_q.reshape(b, s, B_Q_HEADS, HEAD_DIM), q_norm_w).transpose(0, 2, 1, 3)
    bk = rms_norm(b_k.reshape(b, s, B_KV_HEADS, HEAD_DIM), k_norm_w).transpose(0, 2, 1, 3)
    bv = to_heads(b_v, B_KV_HEADS)
    bq = apply_axial_rope(bq, rope_cos, rope_sin)
    bk = apply_axial_rope(bk, rope_cos, rope_sin)
    ob = blocked_bidirectional_gqa(bq, bk, bv)

    lb = lower_bound.astype(f32)
    f_gate = lb + (1.0 - lb) * jax.nn.sigmoid(c_f.astype(f32).reshape(b, s, 2, C_WIDTH))
    log_f = jnp.log(f_gate)
    k_in = 1.0 - f_gate
    cq = to_heads(jax.nn.silu(c_q.astype(f32)), C_HEADS) * (HEAD_DIM ** -0.5)
    ci = to_heads(c_i.astype(f32), C_HEADS)
    oc = hgrn2_chunked(cq, to_heads(k_in[:, :, 0], C_HEADS), ci, to_heads(log_f[:, :, 0], C_HEADS))
    oc = oc + flip_seq(hgrn2_chunked(flip_seq(cq), flip_seq(to_heads(k_in[:, :, 1], C_HEADS)),
                                     flip_seq(ci), flip_seq(to_heads(log_f[:, :, 1], C_HEADS))))
    oc = rms_norm(oc.transpose(0, 2, 1, 3), hgrn_norm_w) * jax.nn.sigmoid(
        c_g.astype(f32).reshape(b, s, C_HEADS, HEAD_DIM))
    oc = oc.reshape(b, s, C_WIDTH)

    mixed = jnp.concatenate([oa.astype(x.dtype), ob.astype(x.dtype), oc.astype(x.dtype)], axis=-1)
    return mixed @ w_out


def memory_cross_attention(x, mem, wq, wk, wv, wo):
    b, s, _ = x.shape
    m = mem.shape[1]
    q = (x @ wq).reshape(b, s, X_HEADS, X_HEAD_DIM)
    k = (mem @ wk).reshape(b, m, X_HEADS, X_HEAD_DIM)
    v = (mem @ wv).reshape(b, m, X_HEADS, X_HEAD_DIM)
    sc = jnp.einsum('bshd,bmhd->bhsm', q, k).astype(jnp.float32) * (X_HEAD_DIM ** -0.5)
    p = jax.nn.softmax(sc, axis=-1).astype(v.dtype)
    o = jnp.einsum('bhsm,bmhd->bshd', p, v).reshape(b, s, X_HEADS * X_HEAD_DIM)
    return o @ wo


def swiglu(x, wg, wu, wd):
    return (jax.nn.silu(x @ wg) * (x @ wu)) @ wd


def moe_swiglu(x, w_router, w_gate, w_up, w_down):
    b, s, d = x.shape
    n = b * s
    xt = x.reshape(n, d)
    logits = (xt @ w_router).astype(jnp.float32)
    top_logit, top_e = lax.top_k(logits, TOP_K)
    top_w = jax.nn.softmax(top_logit, axis=-1)
    flat_e = top_e.reshape(-1)
    flat_tok = jnp.repeat(jnp.arange(n, dtype=jnp.int32), TOP_K)
    flat_w = top_w.reshape(-1)
    order = jnp.argsort(flat_e)
    e_sorted = flat_e[order]
    counts = jnp.bincount(flat_e, length=N_EXPERTS)
    padded = (counts + MOE_BLOCK - 1) // MOE_BLOCK * MOE_BLOCK
    pad_end = jnp.cumsum(padded)
    pad_start = pad_end - padded
    start = jnp.cumsum(counts) - counts
    dest = pad_start[e_sorted] + jnp.arange(n * TOP_K, dtype=jnp.int32) - start[e_sorted]
    n_rows = n * TOP_K + N_EXPERTS * MOE_BLOCK
    n_blocks = n_rows // MOE_BLOCK
    row_tok = jnp.zeros((n_rows,), jnp.int32).at[dest].set(flat_tok[order])
    row_w = jnp.zeros((n_rows,), jnp.float32).at[dest].set(flat_w[order])
    block_e = jnp.minimum(
        jnp.searchsorted(pad_end, jnp.arange(n_blocks, dtype=jnp.int32) * MOE_BLOCK, side='right'),
        N_EXPERTS - 1)
    xb = xt[row_tok].reshape(n_blocks, MOE_BLOCK, d)

    def expert_block(args):
        xs, e = args
        hdn = jax.nn.silu(xs @ w_gate[e]) * (xs @ w_up[e])
        return hdn @ w_down[e]

    yb = lax.map(expert_block, (xb, block_e)).reshape(n_rows, d)
    y = jnp.zeros((n, d), jnp.float32).at[row_tok].add(yb.astype(jnp.float32) * row_w[:, None])
    return y.astype(x.dtype).reshape(b, s, d)


def setup_inputs(seed: int = 0) -> dict:
    key = jax.random.key(seed)
    keys = iter(jax.random.split(key, 40))
    f32 = jnp.float32
    L = DEPTH

    def normal(shape, scale):
        return jax.random.normal(next(keys), shape, f32) * scale

    def gain(shape):
        return 1.0 + normal(shape, 0.02)

    x = normal((BATCH, SEQ, D_MODEL), 1.0)
    mem = normal((BATCH, MEM_TOKENS, D_MODEL), 1.0)
    w_in = normal((L, D_MODEL, D_IN), D_MODEL ** -0.5)
    conv_w = normal((L, CONV_K, 3 * A_WIDTH), CONV_K ** -0.5)
    gdn_a_log = jnp.log(jax.random.uniform(next(keys), (L, 2, A_HEADS), f32, 1.0, 16.0))
    dt = jnp.exp(jax.random.uniform(next(keys), (L, 2, A_HEADS), f32, math.log(1e-3), math.log(1e-1)))
    gdn_dt_bias = dt + jnp.log(-jnp.expm1(-dt))
    gdn_norm_w = gain((L, HEAD_DIM))
    q_norm_w = gain((L, HEAD_DIM))
    k_norm_w = gain((L, HEAD_DIM))
    hgrn_lb_logits = normal((L, C_WIDTH), 0.1)
    hgrn_norm_w = gain((L, HEAD_DIM))
    w_out = normal((L, D_MIX, D_MODEL), D_MIX ** -0.5 * DN_BETA)
    ln1_g = gain((L, D_MODEL))
    ln1_b = normal((L, D_MODEL), 0.02)
    xq = normal((L, D_MODEL, X_HEADS * X_HEAD_DIM), D_MODEL ** -0.5)
    xk = normal((L, D_MODEL, X_HEADS * X_HEAD_DIM), D_MODEL ** -0.5)
    xv = normal((L, D_MODEL, X_HEADS * X_HEAD_DIM), D_MODEL ** -0.5)
    xo = normal((L, X_HEADS * X_HEAD_DIM, D_MODEL), (X_HEADS * X_HEAD_DIM) ** -0.5 * DN_BETA)
    ln2_g = gain((L, D_MODEL))
    ln2_b = normal((L, D_MODEL), 0.02)
    ffn_wg = normal((N_DENSE, D_MODEL, D_FF), D_MODEL ** -0.5)
    ffn_wu = normal((N_DENSE, D_MODEL, D_FF), D_MODEL ** -0.5)
    ffn_wd = normal((N_DENSE, D_FF, D_MODEL), D_FF ** -0.5 * DN_BETA)
    moe_router = normal((N_MOE, D_MODEL, N_EXPERTS), D_MODEL ** -0.5)
    moe_wg = normal((N_MOE, N_EXPERTS, D_MODEL, D_FF_EXPERT), D_MODEL ** -0.5)
    moe_wu = normal((N_MOE, N_EXPERTS, D_MODEL, D_FF_EXPERT), D_MODEL ** -0.5)
    moe_wd = normal((N_MOE, N_EXPERTS, D_FF_EXPERT, D_MODEL), D_FF_EXPERT ** -0.5 * DN_BETA)
    ln3_g = gain((L, D_MODEL))
    ln3_b = normal((L, D_MODEL), 0.02)
    return {'x': x, 'mem': mem, 'w_in': w_in, 'conv_w': conv_w, 'gdn_a_log': gdn_a_log,
            'gdn_dt_bias': gdn_dt_bias, 'gdn_norm_w': gdn_norm_w, 'q_norm_w': q_norm_w,
            'k_norm_w': k_norm_w, 'hgrn_lb_logits': hgrn_lb_logits, 'hgrn_norm_w': hgrn_norm_w,
            'w_out': w_out, 'ln1_g': ln1_g, 'ln1_b': ln1_b, 'xq': xq, 'xk': xk, 'xv': xv, 'xo': xo,
            'ln2_g': ln2_g, 'ln2_b': ln2_b, 'ffn_wg': ffn_wg, 'ffn_wu': ffn_wu, 'ffn_wd': ffn_wd,
            'moe_router': moe_router, 'moe_wg': moe_wg, 'moe_wu': moe_wu, 'moe_wd': moe_wd,
            'ln3_g': ln3_g, 'ln3_b': ln3_b}


def reference(x, mem, w_in, conv_w, gdn_a_log, gdn_dt_bias, gdn_norm_w, q_norm_w, k_norm_w,
              hgrn_lb_logits, hgrn_norm_w, w_out, ln1_g, ln1_b, xq, xk, xv, xo, ln2_g, ln2_b,
              ffn_wg, ffn_wu, ffn_wd, moe_router, moe_wg, moe_wu, moe_wd, ln3_g, ln3_b):
    seq_len = x.shape[1]
    rope_cos, rope_sin = axial_rope_tables(seq_len)
    lower_bounds = hgrn_lower_bounds(hgrn_lb_logits)
    for l in range(DEPTH):
        h = hybrid_token_mixer(x, rope_cos, rope_sin, w_in[l], conv_w[l], gdn_a_log[l], gdn_dt_bias[l],
                               gdn_norm_w[l], q_norm_w[l], k_norm_w[l], lower_bounds[l],
                               hgrn_norm_w[l], w_out[l])
        x = layer_norm(DN_ALPHA * x + h, ln1_g[l], ln1_b[l])
        c = memory_cross_attention(x, mem, xq[l], xk[l], xv[l], xo[l])
        x = layer_norm(DN_ALPHA * x + c, ln2_g[l], ln2_b[l])
        if l % 2 == 0:
            f = swiglu(x, ffn_wg[l // 2], ffn_wu[l // 2], ffn_wd[l // 2])
        else:
            f = moe_swiglu(x, moe_router[l // 2], moe_wg[l // 2], moe_wu[l // 2], moe_wd[l // 2])
        x = layer_norm(DN_ALPHA * x + f, ln3_g[l], ln3_b[l])
    return x
```

```python
import os
import numpy as np
from contextlib import ExitStack
import concourse.bass as bass
import concourse.mybir as mybir
from concourse.bass_utils import run_bass_kernel_spmd

F32 = mybir.dt.float32
BF16 = mybir.dt.bfloat16
AF = mybir.ActivationFunctionType
ALU = mybir.AluOpType
AX = mybir.AxisListType


class T:
    def __init__(self, h, name):
        self.h = h
        self.name = name
        self.lw = None
        self.rd = {}
        self.psum = False

    def __getitem__(self, idx):
        return V(self, self.h[idx])

    @property
    def v(self):
        return V(self, self.h[:])


class V:
    def __init__(self, t, ap):
        self.t = getattr(t, 't', t)
        self.ap = ap

    def __getitem__(self, idx):
        return V(self.t, self.ap[idx])


class K:
    NRING = 6

    def __init__(self, nc, es):
        self.nc = nc
        self.es = es
        self.es0 = es
        self.eng = {'pe': nc.tensor, 'dve': nc.vector, 'act': nc.scalar,
                    'pool': nc.gpsimd, 'sp': nc.sync}
        self.sem = {e: es.enter_context(nc.semaphore('s_' + e)) for e in self.eng}
        self.cnt = {e: 0 for e in self.eng}
        self.known = {e: {} for e in self.eng}
        self.ring = {}
        for q in ('sp', 'pool', 'act'):
            self.ring[q] = [[es.enter_context(nc.semaphore('d_%s%d' % (q, i))), 0]
                            for i in range(self.NRING)]
        self.ring_i = {q: 0 for q in self.ring}
        self.nalloc = 0
        self.ninstr = 0

    def sb(self, shape, dt=F32, name=None):
        self.nalloc += 1
        name = name or 'sb%d' % self.nalloc
        h = self.es.enter_context(self.nc.sbuf_tensor(name, list(shape), dt))
        return T(h, name)

    def ps(self, shape, dt=F32, name=None):
        self.nalloc += 1
        name = name or 'ps%d' % self.nalloc
        h = self.es.enter_context(self.nc.psum_tensor(name, list(shape), dt))
        t = T(h, name)
        t.psum = True
        return t

    def dram(self, name, shape, dt=F32, kind=None):
        if kind is None:
            h = self.nc.dram_tensor(name, list(shape), dt)
        else:
            h = self.nc.dram_tensor(name, list(shape), dt, kind=kind)
        return T(h, name)

    def _wait(self, e, ev):
        if ev is None:
            return
        sem, val = ev[0], ev[1]
        kn = self.known[e]
        if kn.get(sem.name, 0) >= val:
            return
        self.eng[e].wait_ge(sem, val)
        kn[sem.name] = val
        self.ninstr += 1

    def _pre(self, e, reads, writes):
        for v in reads:
            t = v.t
            if t.lw is not None:
                if not (e == 'pe' and t.lw[2] == 'pe'):
                    self._wait(e, t.lw)
            if t.psum:
                for src, ev in t.rd.items():
                    if src != e:
                        self._wait(e, ev)
        for v in writes:
            t = v.t
            if t.lw is not None and not (e == 'pe' and t.lw[2] == 'pe'):
                self._wait(e, t.lw)
            for src, ev in t.rd.items():
                if not (e == 'pe' and src == 'pe'):
                    self._wait(e, ev)

    def _post(self, src, ev3, reads, writes):
        for v in writes:
            v.t.lw = ev3
            v.t.rd = {}
        for v in reads:
            if v.t.lw is ev3:
                continue
            v.t.rd[src] = (ev3[0], ev3[1])

    def emit(self, e, fn, reads, writes):
        reads = [r for r in reads if isinstance(r, V)]
        writes = [w for w in writes if isinstance(w, V)]
        self._pre(e, reads, writes)
        ins = fn(self.eng[e])
        self.cnt[e] += 1
        ins.then_inc(self.sem[e], 1)
        self.ninstr += 1
        ev = (self.sem[e], self.cnt[e], e)
        self._post(e, ev, reads, writes)
        return ins

    def dma(self, out, in_, q='sp', **kw):
        ring = self.ring[q]
        i = self.ring_i[q]
        self.ring_i[q] = (i + 1) % len(ring)
        slot = ring[i]
        sem, uses = slot
        if uses > 0:
            self._wait(q, (sem, 16 * uses))
        self._pre(q, [in_], [out])
        ins = self.eng[q].dma_start(out=out.ap, in_=in_.ap, **kw)
        ins.then_inc(sem, 16)
        slot[1] = uses + 1
        self.ninstr += 1
        src = 'dma_' + sem.name
        ev = (sem, 16 * (uses + 1), src)
        self._post(src, ev, [in_], [out])
        return ins

    def allgather_pairs(self, out_t, in_t):
        self.ncc = getattr(self, 'ncc', 0) + 1
        sem = self.es0.enter_context(self.nc.semaphore('cc%d' % self.ncc))
        self._pre('pool', [in_t.v], [out_t.v])
        ins = self.eng['pool'].collective_compute(
            "AllGather", ALU.bypass, replica_groups=[[0, 1], [2, 3], [4, 5], [6, 7]],
            ins=[in_t.h.ap().opt()], outs=[out_t.h.ap().opt()])
        ins.then_inc(sem, 1)
        self.ninstr += 1
        ev = (sem, 1, 'cc%d' % self.ncc)
        self._post(ev[2], ev, [in_t.v], [out_t.v])
        self.ccsems = getattr(self, 'ccsems', []) + [sem]

    def finish(self, e='sp'):
        for q, ring in self.ring.items():
            for sem, uses in ring:
                if uses > 0:
                    self._wait(e, (sem, 16 * uses))

    @staticmethod
    def _a(x):
        return x.ap if isinstance(x, V) else x

    def mm(self, out, lhsT, rhs, start=True, stop=True, **kw):
        tp = kw.get('tile_position')
        rows = (tp[0] if tp else 0, lhsT.ap.shape[0])
        t = out.t
        if t.lw is not None and t.lw[2] == 'pe' and getattr(t, 'pe_rows', rows) != rows:
            self._wait('pe', t.lw)
        t.pe_rows = rows
        return self.emit('pe', lambda g: g.matmul(out.ap, lhsT.ap, rhs.ap, start=start, stop=stop, **kw),
                         [lhsT, rhs], [out])

    def tr(self, out, in_, ident):
        rows = (0, in_.ap.shape[0])
        t = out.t
        if t.lw is not None and t.lw[2] == 'pe' and getattr(t, 'pe_rows', rows) != rows:
            self._wait('pe', t.lw)
        t.pe_rows = rows
        return self.emit('pe', lambda g: g.transpose(out.ap, in_.ap, ident.ap), [in_, ident], [out])

    def act(self, out, in_, func, bias=0.0, scale=1.0, accum=None, e='act'):
        a = self._a
        kw = {}
        if accum is not None:
            kw['accum_out'] = accum.ap
        return self.emit('act', lambda g: g.activation(out.ap, in_.ap, func, bias=a(bias), scale=a(scale), **kw),
                         [in_, bias, scale], [out] + ([accum] if accum is not None else []))

    def tt(self, out, a_, b_, op, e='dve'):
        return self.emit(e, lambda g: g.tensor_tensor(out.ap, a_.ap, b_.ap, op), [a_, b_], [out])

    def ts(self, out, in_, s1, s2=None, op0=ALU.mult, op1=None, e='dve', accum=None):
        a = self._a
        kw = {}
        if op1 is not None:
            kw['op1'] = op1
        if accum is not None:
            kw['accum_out'] = accum.ap
        return self.emit(e, lambda g: g.tensor_scalar(out.ap, in_.ap, a(s1), a(s2), op0, **kw),
                         [in_, s1, s2], [out] + ([accum] if accum is not None else []))

    def stt(self, out, in0, scalar, in1, op0, op1, e='dve'):
        a = self._a
        return self.emit(e, lambda g: g.scalar_tensor_tensor(out.ap, in0.ap, a(scalar), in1.ap, op0, op1),
                         [in0, scalar, in1], [out])

    def copy(self, out, in_, e='dve'):
        if e == 'act':
            return self.emit('act', lambda g: g.copy(out.ap, in_.ap), [in_], [out])
        return self.emit(e, lambda g: g.tensor_copy(out.ap, in_.ap), [in_], [out])

    def memset(self, out, val, e='dve'):
        return self.emit(e, lambda g: g.memset(out.ap, val), [], [out])

    def recip(self, out, in_):
        return self.emit('dve', lambda g: g.reciprocal(out.ap, in_.ap), [in_], [out])

    def reduce(self, out, in_, op, axis=AX.X, e='dve'):
        return self.emit(e, lambda g: g.tensor_reduce(out.ap, in_.ap, axis, op), [in_], [out])

    def bn_stats(self, out, in_):
        return self.emit('dve', lambda g: g.bn_stats(out.ap, in_.ap), [in_], [out])

    def bn_aggr(self, out, in_):
        return self.emit('dve', lambda g: g.bn_aggr(out.ap, in_.ap), [in_], [out])

    def max8(self, out, in_):
        return self.emit('dve', lambda g: g.max(out.ap, in_.ap), [in_], [out])


ALPHA = float((2 * 2) ** 0.25)
LN_EPS = 1e-5
NEXP = 8
NT = 16
TOK = 2048


def barrier(k):
    for e in k.eng:
        for f in k.eng:
            if f != e and k.cnt[f] > 0:
                k._wait(e, (k.sem[f], k.cnt[f]))
        for q, ring in k.ring.items():
            for sem, uses in ring:
                if uses > 0:
                    k._wait(e, (sem, 16 * uses))


def load_w_rows(k, dst, src_ap, nk, q='pool'):
    for kc in range(nk):
        k.dma(dst[:, kc, :], V(src_ap.t, src_ap.ap[kc * 128:(kc + 1) * 128, :]), q=q)


def layer_norm_tile(k, t, g_bc, b_bc, out, small):
    st, mv, rstd, nmr = small['st'], small['mv'], small['rstd'], small['nmr']
    k.bn_stats(st[:, 0, :], t[:, 0:512])
    k.bn_stats(st[:, 1, :], t[:, 512:1024])
    k.bn_aggr(mv.v, st.v)
    k.act(rstd.v, mv[:, 1:2], AF.Sqrt, bias=small['eps'].v)
    k.recip(rstd.v, rstd.v)
    k.stt(nmr.v, mv[:, 0:1], -1.0, rstd.v, ALU.mult, ALU.mult)
    k.act(t.v, t.v, AF.Identity, bias=nmr.v, scale=rstd.v)
    k.tt(t.v, t.v, g_bc.v, ALU.mult)
    k.tt(out.v, t.v, b_bc.v, ALU.add)


def proj_ln(k, es, srcT, w_d, res_d, g_d, b_d, out_d, outT, ident, PS, outT32=None, out_q='pool'):
    with ExitStack() as s2:
        k2 = k
        old = k.es
        k.es = s2
        w = k.sb([128, 8, 1024], BF16)
        g_bc = k.sb([128, 1024]); b_bc = k.sb([128, 1024])
        small = dict(st=k.sb([128, 2, 6]), mv=k.sb([128, 2]), rstd=k.sb([128, 1]), nmr=k.sb([128, 1]), eps=k.sb([128, 1]))
        k.memset(small['eps'].v, LN_EPS)
        xt = [k.sb([128, 1024]) for _ in range(2)]
        tt_ = [k.sb([128, 1024]) for _ in range(2)]
        ot = [k.sb([128, 1024]) for _ in range(2)]
        load_w_rows(k, w, w_d.v, 8)
        k.dma(g_bc.v, V(g_d, g_d.h[:].partition_broadcast(128)))
        k.dma(b_bc.v, V(b_d, b_d.h[:].partition_broadcast(128)))
        def mm_stage(tt):
            k.dma(xt[tt % 2].v, res_d[tt * 128:(tt + 1) * 128, :])
            for nt in range(2):
                p = PS[(tt % 2) * 4 + nt]
                for kc in range(8):
                    k.mm(p.v, srcT[:, kc, tt * 128:(tt + 1) * 128], w[:, kc, nt * 512:(nt + 1) * 512],
                         start=(kc == 0), stop=(kc == 7))

        def ln_stage(tt):
            x_ = xt[tt % 2]; t_ = tt_[tt % 2]; o_ = ot[tt % 2]
            for nt in range(2):
                p = PS[(tt % 2) * 4 + nt]
                k.stt(t_[:, nt * 512:(nt + 1) * 512], x_[:, nt * 512:(nt + 1) * 512], ALPHA, p.v, ALU.mult, ALU.add)
            layer_norm_tile(k, t_, g_bc, b_bc, o_, small)
            if out_d is not None:
                k.dma(out_d[tt * 128:(tt + 1) * 128, :], o_.v, q=out_q)

        def tr_stage(tt):
            o_ = ot[tt % 2]
            for half in range(2):
                p = PS[(tt % 2) * 4 + 2 + half]
                for j in range(4):
                    kc = half * 4 + j
                    k.tr(p[:, j * 128:(j + 1) * 128], o_[:, kc * 128:(kc + 1) * 128], ident.v)
                k.copy(outT[:, half * 4:half * 4 + 4, tt * 128:(tt + 1) * 128],
                       V(p, p.h[:].rearrange("p (j t) -> p j t", j=4)), e='act')
                if outT32 is not None:
                    k.copy(outT32[tt % 2][:, half * 4:half * 4 + 4, :],
                           V(p, p.h[:].rearrange("p (j t) -> p j t", j=4)), e='dve')

        mm_stage(0)
        for tt in range(NT):
            ln_stage(tt)
            if tt + 1 < NT:
                mm_stage(tt + 1)
            tr_stage(tt)
            if outT32 is not None and tt >= 1:
                outT32[2](tt - 1, outT32[(tt - 1) % 2])
        if outT32 is not None:
            outT32[2](NT - 1, outT32[(NT - 1) % 2])
        barrier(k)
        k.es = old


def cross_attn(k, es, x1T, memT_d, xq_d, xk_d, xv_d, attnT, PS, ones_bf):
    with ExitStack() as s2:
        old = k.es
        k.es = s2
        memT = k.sb([128, 8, 256], BF16)
        load_w_rows(k, memT, memT_d.v, 8)
        kT = k.sb([128, 8, 256], BF16)
        Vm = k.sb([128, 2, 1024], BF16)
        qT = k.sb([128, 8, TOK], BF16)
        with ExitStack() as s3:
            k.es = s3
            wk = k.sb([128, 8, 1024], BF16)
            wv = k.sb([128, 8, 1024], BF16)
            wq = k.sb([128, 8, 1024], BF16)
            load_w_rows(k, wk, xk_d.v, 8)
            load_w_rows(k, wv, xv_d.v, 8)
            load_w_rows(k, wq, xq_d.v, 8)
            for mt in range(8):
                p = PS[mt % 2]
                for kc in range(8):
                    k.mm(p[:, 0:256], wk[:, kc, mt * 128:(mt + 1) * 128], memT[:, kc, :], start=(kc == 0), stop=(kc == 7))
                k.copy(kT[:, mt, :], p[:, 0:256], e='act')
            for m in range(2):
                for nt in range(2):
                    p = PS[2 + nt]
                    for kc in range(8):
                        k.mm(p.v, memT[:, kc, m * 128:(m + 1) * 128], wv[:, kc, nt * 512:(nt + 1) * 512],
                             start=(kc == 0), stop=(kc == 7))
                    k.copy(Vm[:, m, nt * 512:(nt + 1) * 512], p.v, e='dve')
            i = 0
            for mt in range(8):
                for n in range(4):
                    p = PS[4 + i % 4]; i += 1
                    for kc in range(8):
                        k.mm(p.v, wq[:, kc, mt * 128:(mt + 1) * 128], x1T[:, kc, n * 512:(n + 1) * 512],
                             start=(kc == 0), stop=(kc == 7))
                    k.copy(qT[:, mt, n * 512:(n + 1) * 512], p.v, e=('act' if i % 2 else 'dve'))
            barrier(k)
        k.es = s2
        pT = [[k.sb([128, 512], BF16) for _ in range(2)] for _ in range(2)]
        rs = [k.sb([128, 512]) for _ in range(2)]
        its = [(h, n) for h in range(4) for n in range(4)]

        def scores(i):
            h, n = its[i]
            pp = pT[i % 2]
            for m in range(2):
                p = PS[(i % 2) * 5 + m]
                for dc in range(2):
                    k.mm(p.v, kT[:, 2 * h + dc, m * 128:(m + 1) * 128], qT[:, 2 * h + dc, n * 512:(n + 1) * 512],
                         start=(dc == 0), stop=(dc == 1))
                k.act(pp[m].v, p.v, AF.Exp, scale=1.0 / 16.0)

        scores(0)
        for i, (h, n) in enumerate(its):
            pp = pT[i % 2]; r_ = rs[i % 2]
            if i + 1 < len(its):
                scores(i + 1)
            ps_ = PS[2]
            for m in range(2):
                k.mm(ps_.v, ones_bf.v, pp[m].v, start=(m == 0), stop=(m == 1))
            k.recip(r_.v, ps_.v)
            for dc in range(2):
                p = PS[3 + dc]
                for m in range(2):
                    k.mm(p.v, Vm[:, m, (2 * h + dc) * 128:(2 * h + dc + 1) * 128], pp[m].v, start=(m == 0), stop=(m == 1))
                k.tt(attnT[:, 2 * h + dc, n * 512:(n + 1) * 512], p.v, r_.v, ALU.mult)
        barrier(k)
        k.es = old


def ffn(k, es, x2T, experts, acc, PS, gateT=None, sel=None):
    with ExitStack() as s2:
        old = k.es
        k.es = s2
        G = 4
        wgt = [k.sb([128, 8, 128], BF16) for _ in range(2)]
        wut = [k.sb([128, 8, 128], BF16) for _ in range(2)]
        wdt = [k.sb([128, G, 1024], BF16) for _ in range(2)]
        hT = [k.sb([128, G, TOK], BF16) for _ in range(1)]
        sg = [k.sb([128, 512]) for _ in range(2)]
        first = True
        wi = 0
        gi = 0
        for e, (wg_d, wu_d, wd_d) in enumerate(experts):
            F = wg_d.h.shape[1]
            nch = F // 128
            for g0 in range(0, nch, G):
                gn = min(G, nch - g0)
                wd_ = wdt[gi % 2]; gi += 1
                h_ = hT[0]
                for j in range(gn):
                    k.dma(wd_[:, j, :], wd_d[(g0 + j) * 128:(g0 + j + 1) * 128, :], q='pool')
                for j in range(gn):
                    mt = g0 + j
                    wg_ = wgt[wi % 2]; wu_ = wut[wi % 2]; wi += 1
                    k.dma(wg_.v, V(wg_d, wg_d.h[:, mt * 128:(mt + 1) * 128].rearrange("(kc p) m -> p kc m", p=128)), q='pool')
                    k.dma(wu_.v, V(wu_d, wu_d.h[:, mt * 128:(mt + 1) * 128].rearrange("(kc p) m -> p kc m", p=128)), q='pool')
                    for n in range(4):
                        pg = PS[n % 2]; pu = PS[2 + n % 2]; s_ = sg[n % 2]
                        for kc in range(8):
                            k.mm(pg.v, wg_[:, kc, :], x2T[:, kc, n * 512:(n + 1) * 512], start=(kc == 0), stop=(kc == 7))
                        for kc in range(8):
                            k.mm(pu.v, wu_[:, kc, :], x2T[:, kc, n * 512:(n + 1) * 512], start=(kc == 0), stop=(kc == 7))
                        k.act(s_.v, pg.v, AF.Silu)
                        k.tt(h_[:, j, n * 512:(n + 1) * 512], s_.v, pu.v, ALU.mult)
                for tt in range(NT):
                    for nt in range(2):
                        pd = PS[4 + (tt * 2 + nt) % 2]
                        for j in range(gn):
                            k.mm(pd.v, h_[:, j, tt * 128:(tt + 1) * 128], wd_[:, j, nt * 512:(nt + 1) * 512],
                                 start=(j == 0), stop=(j == gn - 1))
                        a_ = acc[:, tt, nt * 512:(nt + 1) * 512]
                        if gateT is None:
                            if first:
                                k.copy(a_, pd.v, e='act')
                            else:
                                k.tt(a_, a_, pd.v, ALU.add)
                        else:
                            g_ = gateT[:, tt, e:e + 1]
                            if first:
                                k.ts(a_, pd.v, g_, None, op0=ALU.mult)
                            else:
                                k.stt(a_, pd.v, g_, a_, ALU.mult, ALU.add)
                first = False
        barrier(k)
        k.es = old


def final_ln(k, es, acc, res_d, g_d, b_d, out_d, outT_d, ident, PS):
    with ExitStack() as s2:
        old = k.es
        k.es = s2
        g_bc = k.sb([128, 1024]); b_bc = k.sb([128, 1024])
        small = dict(st=k.sb([128, 2, 6]), mv=k.sb([128, 2]), rstd=k.sb([128, 1]), nmr=k.sb([128, 1]), eps=k.sb([128, 1]))
        k.memset(small['eps'].v, LN_EPS)
        xt = [k.sb([128, 1024]) for _ in range(2)]
        tt_ = [k.sb([128, 1024]) for _ in range(2)]
        ot = [k.sb([128, 1024]) for _ in range(2)]
        oT = [k.sb([128, 8, 128], BF16) for _ in range(2)]
        k.dma(g_bc.v, V(g_d, g_d.h[:].partition_broadcast(128)))
        k.dma(b_bc.v, V(b_d, b_d.h[:].partition_broadcast(128)))
        for tt in range(NT):
            x_ = xt[tt % 2]; t_ = tt_[tt % 2]; o_ = ot[tt % 2]
            k.dma(x_.v, res_d[tt * 128:(tt + 1) * 128, :])
            k.stt(t_.v, x_.v, ALPHA, acc[:, tt, :], ALU.mult, ALU.add)
            layer_norm_tile(k, t_, g_bc, b_bc, o_, small)
            k.dma(out_d[tt * 128:(tt + 1) * 128, :], o_.v, q='pool')
            if outT_d is not None:
                oT_ = oT[tt % 2]
                for half in range(2):
                    p = PS[2 + half]
                    for j in range(4):
                        kc = half * 4 + j
                        k.tr(p[:, j * 128:(j + 1) * 128], o_[:, kc * 128:(kc + 1) * 128], ident.v)
                    k.copy(oT_[:, half * 4:half * 4 + 4, :], V(p, p.h[:].rearrange("p (j t) -> p j t", j=4)), e='act')
                if callable(outT_d):
                    outT_d(tt, oT_)
                else:
                    k.dma(V(outT_d, outT_d.h[:, tt * 128:(tt + 1) * 128].rearrange("(kc p) t -> p kc t", p=128)), oT_.v)
        barrier(k)
        k.es = old


def moe_gate_tile(k, lg_ps, gate_tok, tt, tmp):
    lg, mx, nm1, ex, selm, den = tmp['lg'], tmp['mx'], tmp['nm1'], tmp['ex'], tmp['sel'], tmp['den']
    k.copy(lg.v, lg_ps, e='dve')
    k.max8(mx.v, lg.v)
    k.ts(nm1.v, mx[:, 0:1], -1.0, None, op0=ALU.mult)
    k.act(ex.v, lg.v, AF.Exp, bias=nm1.v)
    k.ts(selm.v, lg.v, mx[:, 1:2], None, op0=ALU.is_ge)
    k.tt(ex.v, ex.v, selm.v, ALU.mult)
    k.reduce(den.v, ex.v, ALU.add)
    k.recip(den.v, den.v)
    k.ts(gate_tok[:, tt, :], ex.v, den.v, None, op0=ALU.mult)


def phase_b(k, es, io, moe, PS=None, tag=''):
    ident = k.sb([128, 128])
    ones_bf = k.sb([128, 128], BF16)
    k.dma(ident.v, io['ident'].v)
    k.memset(ones_bf.v, 1.0)
    if PS is None:
        PS = [k.ps([128, 512]) for _ in range(8)]
    x1_d = io.get('x1_dbg') or k.dram('x1_scr' + tag, [TOK, 1024])
    x2_d = io.get('x2_dbg') or k.dram('x2_scr' + tag, [TOK, 1024])
    bufB = k.sb([128, 8, TOK], BF16)
    gateT = None
    outT32 = None
    if moe:
        gateT = k.sb([128, NT, 8])
        wr = k.sb([128, 8, 8])
        k.dma(wr.v, V(io['router'], io['router'].h[:].rearrange("(kc p) e -> p kc e", p=128)))
        sel = k.sb([8, 8 * 128])
        k.dma(sel.v, io['sel8'].v)
        tmp = dict(lg=k.sb([128, 8]), mx=k.sb([128, 8]), nm1=k.sb([128, 1]), ex=k.sb([128, 8]), sel=k.sb([128, 8]),
                   den=k.sb([128, 1]), gt=k.sb([128, 8]))
        x32 = [k.sb([128, 8, 128]) for _ in range(2)]

        def route(tt, xT32):
            p = PS[6]
            for kc in range(8):
                k.mm(p[:, 0:8], xT32[:, kc, :], wr[:, kc, :], start=(kc == 0), stop=(kc == 7))
            moe_gate_tile(k, p[:, 0:8], gateT, tt, tmp)
        outT32 = [x32[0], x32[1], route]
    sA = ExitStack()
    old_es = k.es
    k.es = sA
    bufA = k.sb([128, 8, TOK], BF16)
    if io.get('mix_loader') is not None:
        with k.nc.named_scope('mixload' + tag):
            io['mix_loader'](bufA)
    else:
        for kc in range(8):
            k.dma(bufA[:, kc, :], io['mixT'][kc * 128:(kc + 1) * 128, :])
    with k.nc.named_scope('projln1' + tag):
        proj_ln(k, es, bufA, io['w_out'], io['x'], io['ln1_g'], io['ln1_b'], x1_d, bufB, ident, PS)
    with k.nc.named_scope('xattn' + tag):
        cross_attn(k, es, bufB, io['memT'], io['xq'], io['xk'], io['xv'], bufA, PS, ones_bf)
    with k.nc.named_scope('projln2' + tag):
        proj_ln(k, es, bufA, io['xo'], x1_d, io['ln2_g'], io['ln2_b'], x2_d, bufB, ident, PS, outT32=outT32)
    barrier(k)
    sA.close()
    k.es = old_es
    acc = k.sb([128, NT, 1024])
    if moe:
        experts = [(V(io['moe_wg'], io['moe_wg'].h[e]), V(io['moe_wu'], io['moe_wu'].h[e]), V(io['moe_wd'], io['moe_wd'].h[e])) for e in range(NEXP)]
        experts = [tuple(Tsub(v) for v in ex) for ex in experts]
        ffn(k, es, bufB, experts, acc, PS, gateT=gateT, sel=sel)
    else:
        ffn(k, es, bufB, [(io['ffn_wg'], io['ffn_wu'], io['ffn_wd'])], acc, PS)
    with k.nc.named_scope('finalln' + tag):
        final_ln(k, es, acc, x2_d, io['ln3_g'], io['ln3_b'], io['out'], io.get('outT'), ident, PS)


class Tsub:
    def __init__(self, v):
        self.t = v.t
        self.h = v.ap
        self.name = v.t.name

    def __getitem__(self, idx):
        return V(self.t, self.h[idx])

    @property
    def v(self):
        return V(self.t, self.h)


SEQ = 4096
NBLK = 8
RMS_EPS = 1e-6
PAD = 2

O_AQ, O_AK, O_AV, O_AZ, O_AB, O_AD = 0, 384, 768, 1152, 1536, 1548
O_BQ, O_BK, O_BV = 1560, 1944, 2072
O_CF, O_CI, O_CQ, O_CG = 2200, 2712, 2968, 3224


def mmt(k, out, lhsT, rhs, start=True, stop=True, tp=None):
    if tp is None or tp == (0, 0):
        return k.mm(out, lhsT, rhs, start=start, stop=stop)
    return k.mm(out, lhsT, rhs, start=start, stop=stop, tile_position=tp)


def load_xT(k, xT, src_fn):
    k.memset(xT[:, :, 0:PAD], 0.0)
    k.memset(xT[:, :, PAD + SEQ:PAD + SEQ + PAD], 0.0)
    for kc in range(8):
        for hh in range(2):
            k.dma(xT[:, kc, PAD + hh * 2048:PAD + (hh + 1) * 2048], src_fn(kc, hh), q='pool')


def consts_a(k, io):
    c = {}
    for n in ['ident', 'bd64', 'rt128']:
        c[n] = k.sb([128, 128])
        k.dma(c[n].v, io[n].v)
    c['ones_bf'] = k.sb([128, 128], BF16)
    k.memset(c['ones_bf'].v, 1.0)
    c['eps'] = k.sb([128, 1])
    k.memset(c['eps'].v, RMS_EPS)
    return c


def rope_norm(k, ps, nwcol, cos_, sin_, out, c, W, PSr):
    xs, sq, rn, xn, t1 = W['xs'], W['sq'], W['rn'], W['xn'], W['t1']
    k.copy(xs.v, ps.v, e='act')
    k.tt(sq.v, xs.v, xs.v, ALU.mult)
    k.mm(PSr[0].v, c['bd64'].v, sq.v)
    k.act(rn.v, PSr[0].v, AF.Sqrt, bias=c['eps'].v, scale=1.0 / 64.0)
    k.recip(rn.v, rn.v)
    k.stt(xn.v, xs.v, nwcol, rn.v, ALU.mult, ALU.mult)
    k.mm(PSr[1].v, c['rt128'].v, xn.v)
    k.tt(t1.v, xn.v, cos_.v, ALU.mult)
    k.tt(sq.v, PSr[1].v, sin_.v, ALU.mult)
    k.tt(out, t1.v, sq.v, ALU.add)


def gqa_prepare(k, io, xT, c, PS, mix_units):
    wg = k.sb([128, 8, 384], BF16)
    wbv = k.sb([128, 8, 64], BF16)
    load_w_rows(k, wg, io['wg'].v, 8)
    load_w_rows(k, wbv, io['wbv'].v, 8)
    gnw = k.sb([128, 3])
    k.dma(gnw.v, io['gnw'].v)
    kT = k.sb([128, SEQ], BF16)
    Vsb = k.sb([128, 32, 128], BF16)
    cos_ = [k.sb([128, 512]) for _ in range(2)]
    sin_ = [k.sb([128, 512]) for _ in range(2)]
    W = dict(xs=k.sb([128, 512]), sq=k.sb([128, 512]), rn=k.sb([128, 512]), xn=k.sb([128, 512]), t1=k.sb([128, 512]))
    qT = [k.sb([128, 512], BF16) for _ in range(2)]
    pT = [k.sb([128, 512], BF16) for _ in range(3)]
    pT4 = [k.sb([128, 512], BF16) for _ in range(3)]
    rs = k.sb([128, 512])
    pacc = k.sb([128, 512])
    pacc2 = k.sb([128, 512])
    ones32 = k.sb([128, 64])
    k.memset(ones32.v, 1.0)
    for blk in range(NBLK):
        s = blk * 512
        cs, sn = cos_[blk % 2], sin_[blk % 2]
        k.dma(cs.v, io['cos'][:, s:s + 512])
        k.dma(sn.v, io['sin'][:, s:s + 512])
        p = PS[0]
        for kc in range(8):
            k.mm(p.v, wg[:, kc, 128:256], xT[:, kc, PAD + s:PAD + s + 512], start=(kc == 0), stop=(kc == 7))
        rope_norm(k, p, gnw[:, 1:2], cs, sn, kT[:, s:s + 512], c, W, PS[1:3])
        for t in range(4):
            ti = blk * 4 + t
            pv = PS[3 + t % 2]
            for kc in range(8):
                k.mm(pv[:, 0:64], xT[:, kc, PAD + ti * 128:PAD + (ti + 1) * 128], wbv[:, kc, :], start=(kc == 0), stop=(kc == 7))
            k.copy(Vsb[:, ti, 0:64], pv[:, 0:64], e='act')
            k.copy(Vsb[:, ti, 64:128], pv[:, 0:64], e='dve')

    def attn(PSa):
        for blk in range(NBLK):
            s = blk * 512
            cs, sn = cos_[blk % 2], sin_[blk % 2]
            k.dma(cs.v, io['cos'][:, s:s + 512])
            k.dma(sn.v, io['sin'][:, s:s + 512])
            for qi, (c0, nwc) in enumerate([(0, 0), (256, 2)]):
                p = PSa[0]
                for kc in range(8):
                    k.mm(p.v, wg[:, kc, c0:c0 + 128], xT[:, kc, PAD + s:PAD + s + 512], start=(kc == 0), stop=(kc == 7))
                rope_norm(k, p, gnw[:, nwc:nwc + 1], cs, sn, qT[qi].v, c, W, [PSa[1], PSa[0]])
                yield
            dests = mix_units(blk)
            for h in range(3):
                r = 64 if h == 1 else 0
                qsrc = qT[1] if h == 2 else qT[0]
                po = 64 if h >= 1 else 0
                pv = PSa[2][po:po + 64, :]

                GB = 0
                if GB and len(PSa) >= 7:
                    banks = [PSa[3:3 + GB], PSa[0:2] + PSa[6:7]] if GB == 3 else None
                    banks = [[PSa[0], PSa[1], PSa[3]], [PSa[4], PSa[5], PSa[6]]]
                    ngrp = 32 // GB + (1 if 32 % GB else 0)

                    def sgroup(g):
                        for j in range(GB):
                            kt = g * GB + j
                            if kt < 32:
                                mmt(k, banks[g % 2][j].v, kT[r:r + 64, kt * 128:(kt + 1) * 128], qsrc[r:r + 64, :], tp=(r, 0))
                    pTg = [pT[0], pT[1], pT[2], pT4[0], pT4[1], pT4[2]]
                    sgroup(0)
                    for g in range(ngrp):
                        for j in range(GB):
                            kt = g * GB + j
                            if kt < 32:
                                k.act(pTg[(g % 2) * 3 + j].v, banks[g % 2][j].v, AF.Exp, scale=0.125)
                        if g + 1 < ngrp:
                            sgroup(g + 1)
                        for j in range(GB):
                            kt = g * GB + j
                            if kt < 32:
                                p_ = pTg[(g % 2) * 3 + j]
                                k.mm(PSa[2].v, Vsb[:, kt, :], p_.v, start=(kt == 0), stop=(kt == 31))
                                eng_, acc_ = ('dve', pacc) if kt % 2 == 0 else ('pool', pacc2)
                                if kt < 2:
                                    k.copy(acc_.v, p_.v, e=eng_)
                                else:
                                    k.tt(acc_.v, acc_.v, p_.v, ALU.add, e=eng_)
                        yield
                else:
                    def scores(kt):
                        mmt(k, PSa[kt % 2].v, kT[r:r + 64, kt * 128:(kt + 1) * 128], qsrc[r:r + 64, :], tp=(r, 0))
                    scores(0)
                    for kt in range(32):
                        p_ = pT[kt % 3]
                        k.act(p_.v, PSa[kt % 2].v, AF.Exp, scale=0.125)
                        if kt + 1 < 32:
                            scores(kt + 1)
                        k.mm(PSa[2].v, Vsb[:, kt, :], p_.v, start=(kt == 0), stop=(kt == 31))
                        eng_, acc_ = ('dve', pacc) if kt % 2 == 0 else ('pool', pacc2)
                        if kt < 2:
                            k.copy(acc_.v, p_.v, e=eng_)
                        else:
                            k.tt(acc_.v, acc_.v, p_.v, ALU.add, e=eng_)
                        yield
                sm = PSa[0][po:po + 64, :]
                mmt(k, sm, ones32[:, 0:64], pacc.v, start=True, stop=False, tp=(0, po))
                mmt(k, sm, ones32[:, 0:64], pacc2.v, start=False, stop=True, tp=(0, po))
                k.recip(rs[po:po + 64, :], sm)
                k.tt(dests[h], pv, rs[po:po + 64, :], ALU.mult)
                yield
            mix_units(blk, done=True)
    return attn


def gqa(k, io, xT, c, PS, mix_units):
    with ExitStack() as s2:
        old = k.es
        k.es = s2
        attn = gqa_prepare(k, io, xT, c, PS, mix_units)
        for _ in attn([PS[5], PS[6], PS[3], PS[4], PS[0], PS[1], PS[2]]):
            pass
        barrier(k)
        k.es = old


def hgrn(k, io, xT, c, PS, layer, mix_dest, tick=None):
    if tick is None:
        tick = lambda n: None
    with ExitStack() as s2:
        old = k.es
        k.es = s2
        wfm = k.sb([128, 8, 512], BF16)
        wtm = k.sb([128, 8, 384], BF16)
        load_w_rows(k, wfm, io['wh_fm'].v, 8)
        load_w_rows(k, wtm, io['wh_tm'].v, 8)
        hnw = k.sb([128, 1]); k.dma(hnw.v, io['hnw'].v)
        lb_col = k.sb([128, 1]); oml_col = k.sb([128, 1]); lb_row = k.sb([128, 128]); oml_row = k.sb([128, 128])
        if layer == 0:
            k.memset(lb_col.v, 0.0); k.memset(lb_row.v, 0.0)
        else:
            lc = k.sb([128, 2]); lr = k.sb([128, 2, 128])
            k.dma(lc.v, io['lbl_col'].v)
            k.dma(lr.v, V(io['lbl_row'], io['lbl_row'].h[:].partition_broadcast(128)))
            k.tt(lb_col.v, lc[:, 1:2], lc[:, 0:1], ALU.subtract)
            k.act(lb_col.v, lb_col.v, AF.Sigmoid)
            k.tt(lb_row.v, lr[:, 1, :], lr[:, 0, :], ALU.subtract)
            k.act(lb_row.v, lb_row.v, AF.Sigmoid)
        k.ts(oml_col.v, lb_col.v, -1.0, 1.0, op0=ALU.mult, op1=ALU.add)
        k.ts(oml_row.v, lb_row.v, -1.0, 1.0, op0=ALU.mult, op1=ALU.add)
        ofw_d = k.dram('hg_ofw_l%d' % layer, [128, SEQ])
        fT = k.sb([128, 512]); kTf = k.sb([128, 512]); qs = k.sb([128, 512]); gate = k.sb([128, 512])
        obuf = k.sb([128, 512]); ofw = k.sb([128, 512])
        S = k.sb([128, 64])
        W = {n: [k.sb([128, 128]) for _ in range(2)] for n in ['ftok', 'logf', 'ktok', 'vtok', 'kitok', 'E', 'Einv', 'qd', 'ki', 'at0', 'at1']}
        sc = [k.sb([128, 6]) for _ in range(2)]
        Sm = [k.sb([128, 64]) for _ in range(2)]
        tmpu = [k.sb([128, 64]) for _ in range(2)]
        sq = k.sb([128, 512]); rn = k.sb([128, 512])
        for d in range(2):
            sfx = 'f' if d == 0 else 'b'
            trx = k.sb([128, 132]); trt = k.sb([128, 128]); mki = k.sb([128, 128])
            k.dma(trx.v, io['trx_' + sfx][:, 0:132]); k.dma(trt.v, io['trt_' + sfx].v); k.dma(mki.v, io['mki_' + sfx].v)
            k.memset(S.v, 0.0)
            blks = range(NBLK) if d == 0 else range(NBLK - 1, -1, -1)
            it = 0
            for blk in blks:
                s = blk * 512
                p = PS[2]
                for kc in range(8):
                    k.mm(p.v, wfm[:, kc, d * 128:(d + 1) * 128], xT[:, kc, PAD + s:PAD + s + 512], start=(kc == 0), stop=(kc == 7))
                k.act(fT.v, p.v, AF.Sigmoid)
                k.ts(fT.v, fT.v, oml_col.v, lb_col.v, op0=ALU.mult, op1=ALU.add)
                k.ts(kTf.v, fT.v, -1.0, 1.0, op0=ALU.mult, op1=ALU.add)
                p = PS[3]
                for kc in range(8):
                    k.mm(p.v, wfm[:, kc, 256:384], xT[:, kc, PAD + s:PAD + s + 512], start=(kc == 0), stop=(kc == 7))
                k.act(qs.v, p.v, AF.Silu)
                if d == 1:
                    p = PS[2]
                    for kc in range(8):
                        k.mm(p.v, wfm[:, kc, 384:512], xT[:, kc, PAD + s:PAD + s + 512], start=(kc == 0), stop=(kc == 7))
                    k.act(gate.v, p.v, AF.Sigmoid)
                    k.dma(ofw.v, ofw_d[:, s:s + 512])
                tiles = range(4) if d == 0 else range(3, -1, -1)
                HGL = 9
                for t in tiles:
                    if HGL < 1: break
                    w = {n: W[n][it % 2] for n in W}
                    sc_ = sc[it % 2]
                    it += 1
                    tc = slice(t * 128, (t + 1) * 128)
                    tok0 = PAD + s + t * 128
                    ptm = PS[2]
                    c0 = 0 if d == 0 else 128
                    for kc in range(8):
                        k.mm(ptm[:, 0:256], xT[:, kc, tok0:tok0 + 128], wtm[:, kc, c0:c0 + 256], start=(kc == 0), stop=(kc == 7))
                    cf_ps = ptm[:, 0:128] if d == 0 else ptm[:, 128:256]
                    ci_ps = ptm[:, 128:256] if d == 0 else ptm[:, 0:128]
                    k.act(w['ftok'].v, cf_ps, AF.Exp, scale=-1.0)
                    k.copy(w['vtok'].v, ci_ps, e='act')
                    k.ts(w['ftok'].v, w['ftok'].v, 1.0, None, op0=ALU.add)
                    k.recip(w['ftok'].v, w['ftok'].v)
                    k.tt(w['ftok'].v, w['ftok'].v, oml_row.v, ALU.mult)
                    k.tt(w['ftok'].v, w['ftok'].v, lb_row.v, ALU.add)
                    k.act(w['logf'].v, w['ftok'].v, AF.Ln)
                    k.ts(w['ktok'].v, w['ftok'].v, -1.0, 1.0, op0=ALU.mult, op1=ALU.add)
                    if HGL < 2: continue
                    tick(2)
                    pc1 = PS[3]
                    k.mm(pc1[:, 0:128], trt.v, w['logf'].v)
                    pc2 = V(PS[3], PS[3].h[:, 256:512])
                    k.mm(pc2[:, 0:132], w['logf'].v, trx.v)
                    k.act(w['kitok'].v, pc1[:, 0:128], AF.Exp, scale=-1.0)
                    k.tt(w['kitok'].v, w['kitok'].v, w['ktok'].v, ALU.mult)
                    k.act(w['E'].v, pc2[:, 0:128], AF.Exp)
                    k.act(w['Einv'].v, pc2[:, 0:128], AF.Exp, scale=-1.0)
                    k.copy(sc_[:, 0:4], pc2[:, 128:132], e='act')
                    k.tt(sc_[:, 4:6], sc_[:, 2:4], sc_[:, 0:2], ALU.subtract)
                    k.act(sc_.v, sc_.v, AF.Exp)
                    k.stt(w['qd'].v, qs[:, tc], 0.125, w['E'].v, ALU.mult, ALU.mult)
                    k.tt(w['ki'].v, kTf[:, tc], w['Einv'].v, ALU.mult)
                    if HGL < 3: continue
                    tick(2)
                    atm = [w['at0'], w['at1']]
                    for hh in range(2):
                        ph = hh * 64
                        pa_ = PS[5]
                        mmt(k, pa_[:, 0:128], w['ki'][ph:ph + 64, :], w['qd'][ph:ph + 64, :], tp=(ph, 0))
                        k.tt(atm[hh].v, pa_[:, 0:128], mki.v, ALU.mult)
                    if HGL < 4: continue
                    tick(2)
                    pso = PS[6]
                    chunks = (0, 1) if d == 0 else (1, 0)
                    for ci_, cc in enumerate(chunks):
                        pc_ = cc * 64
                        cols = slice(pc_, pc_ + 64)
                        sm_ = Sm[ci_]; tu = tmpu[ci_]
                        k.ts(sm_.v, S.v, sc_[:, cc:cc + 1], None, op0=ALU.mult)
                        psu = PS[7]
                        for hh in range(2):
                            ph = hh * 64
                            mmt(k, pso[ph:ph + 64, cols], sm_[ph:ph + 64, :], w['qd'][ph:ph + 64, cols], start=True, stop=False, tp=(ph, ph))
                            mmt(k, pso[ph:ph + 64, cols], w['vtok'][pc_:pc_ + 64, ph:ph + 64], atm[hh][pc_:pc_ + 64, cols], start=False, stop=True, tp=(pc_, ph))
                            mmt(k, psu[ph:ph + 64, 0:64], w['kitok'][pc_:pc_ + 64, ph:ph + 64], w['vtok'][pc_:pc_ + 64, ph:ph + 64], tp=(pc_, ph))
                        k.ts(tu.v, psu[:, 0:64], sc_[:, 4 + cc:5 + cc], None, op0=ALU.mult)
                        k.stt(S.v, S.v, sc_[:, 2 + cc:3 + cc], tu.v, ALU.mult, ALU.add)
                        tick(3)
                    if d == 0:
                        k.copy(obuf[:, tc], pso[:, 0:128], e='act')
                    else:
                        k.tt(obuf[:, tc], pso[:, 0:128], ofw[:, tc], ALU.add)
                if d == 0:
                    k.dma(ofw_d[:, s:s + 512], obuf.v)
                else:
                    if io.get('hg_dbg') is not None:
                        k.dma(io['hg_dbg'][:, s:s + 512], obuf.v)
                    k.tt(sq.v, obuf.v, obuf.v, ALU.mult)
                    pn = PS[2]
                    k.mm(pn.v, c['bd64'].v, sq.v)
                    k.act(rn.v, pn.v, AF.Sqrt, bias=c['eps'].v, scale=1.0 / 64.0)
                    k.recip(rn.v, rn.v)
                    k.stt(sq.v, obuf.v, hnw.v, rn.v, ALU.mult, ALU.mult)
                    k.tt(mix_dest(blk), sq.v, gate.v, ALU.mult)
                    mix_dest(blk, done=True)
        barrier(k)
        k.es = old


def gdn(k, io, xT, c, PS, layer, mix_dest):
    with k.nc.named_scope('gdn%d' % layer):
        return _gdn(k, io, xT, c, PS, layer, mix_dest)


def _gdn(k, io, xT, c, PS, layer, mix_dest):
    with ExitStack() as s2:
        old = k.es
        k.es = s2
        wcs = [k.sb([128, 8, 640], BF16) for _ in range(5)]
        with ExitStack() as s3:
            k.es = s3
            cwb = k.sb([128, 5, 640])
            k.dma(cwb.v, V(io['cw'], io['cw'].h[:].partition_broadcast(128)))
            stg = [k.sb([128, 640]) for _ in range(2)]
            for kc in range(8):
                st = stg[kc % 2]
                k.dma(st.v, io['wc'][kc * 128:(kc + 1) * 128, :])
                for j in range(5):
                    k.tt(wcs[j][:, kc, :], st.v, cwb[:, j, :], ALU.mult)
            barrier(k)
        k.es = s2
        wz = k.sb([128, 8, 256], BF16)
        wgt = k.sb([128, 8, 12], BF16)
        load_w_rows(k, wz, io['wz'].v, 8)
        load_w_rows(k, wgt, io['wgt'].v, 8)
        gpar = k.sb([128, 12])
        k.dma(gpar.v, V(io['gpar'], io['gpar'].h[:].partition_broadcast(128)))
        negA = k.sb([128, 6])
        k.act(negA.v, gpar[:, 0:6], AF.Exp)
        k.ts(negA.v, negA.v, -1.0, None, op0=ALU.mult)
        gnorm = k.sb([128, 1]); k.dma(gnorm.v, io['gnorm'].v)
        sel3 = k.sb([3, 384]); k.dma(sel3.v, io['sel3'].v)
        ofw_d = [k.dram('gd_ofw%d_l%d' % (i, layer), [128, SEQ]) for i in range(2)]
        cstash = [k.dram('gd_cs%d_l%d' % (i, layer), [128, SEQ]) for i in range(5)]
        cs = [k.sb([128, 512]) for _ in range(5)]
        zs = [k.sb([128, 512]) for _ in range(2)]
        sq = k.sb([128, 512]); rn = k.sb([128, 512])
        obuf = [k.sb([128, 512]) for _ in range(2)]
        ofw = [k.sb([128, 512]) for _ in range(2)]
        k.memset(obuf[1].v, 0.0)
        S = [k.sb([128, 64]) for _ in range(3)]
        TOK = k.sb([128, 4, 128])
        gsm = {n: k.sb([128, 6]) for n in ['bd', 'gcl', 'x1']}
        gcol = {n: k.sb([128, 3]) for n in ['beta', 'nbeta', 'g', 'ngc', 'bg', 'kdc', 't']}
        gT = k.sb([3, 256])
        H = [{n: k.sb([128, 128]) for n in ['D', 'DT', 'at', 'tmp', 'tmp2']} for _ in range(3)]
        identbf = k.sb([128, 128], BF16)
        k.copy(identbf.v, c['ident'].v)
        for h in range(3):
            for n in ['T0', 'T1']:
                H[h][n] = k.sb([128, 384], BF16)
            for n in ['vb', 'kbg']:
                H[h][n] = k.sb([128, 64], BF16)
            for n in ['kdec', 'u', 'vnew']:
                H[h][n] = k.sb([128, 64])
            H[h]['wT'] = k.sb([128, 128]); H[h]['qd'] = k.sb([128, 128]); H[h]['egb'] = k.sb([128, 128]); H[h]['ecd0'] = k.sb([128, 2]); H[h]['ecd1'] = k.sb([128, 2])
        RB = [0, 64, 0]
        QT = [cs[0], cs[0], cs[2]]
        KT = [cs[1], cs[1], cs[3]]
        KTOK = [(0, 0), (0, 64), (3, 0)]
        VTOK = [(1, 0), (1, 64), (2, 64)]
        PO = [0, 64, 0]
        for d in range(2):
            sfx = 'f' if d == 0 else 'b'
            tb = k.sb([128, 256]); negs = k.sb([128, 128]); negi = k.sb([128, 128])
            k.dma(tb.v, io['tb_' + sfx].v); k.dma(negs.v, io['negs_' + sfx].v); k.dma(negi.v, io['negi_' + sfx].v)
            for h in range(3):
                k.memset(S[h].v, 0.0)
            blks = range(NBLK) if d == 0 else range(NBLK - 1, -1, -1)
            for blk in blks:
                s = blk * 512
                if d == 0:
                    for ct in range(5):
                        p = PS[ct % 2]
                        n_ = 0
                        for j in range(5):
                            for kc in range(8):
                                k.mm(p.v, wcs[j][:, kc, ct * 128:(ct + 1) * 128], xT[:, kc, s + j:s + j + 512], start=(n_ == 0), stop=(n_ == 39))
                                n_ += 1
                        k.act(cs[ct].v, p.v, AF.Silu)
                    for ct, scl, rows in [(0, 0.125, 128), (1, 1.0, 128), (2, 0.125, 64), (3, 1.0, 128)]:
                        k.tt(sq.v, cs[ct].v, cs[ct].v, ALU.mult)
                        pn = PS[2]
                        k.mm(pn.v, c['bd64'].v, sq.v)
                        k.act(rn.v, pn.v, AF.Sqrt, bias=c['eps'].v)
                        k.recip(rn.v, rn.v)
                        k.stt(cs[ct][0:rows, :], cs[ct][0:rows, :], scl, rn[0:rows, :], ALU.mult, ALU.mult)
                    for ct in range(5):
                        k.dma(cstash[ct][:, s:s + 512], cs[ct].v, q='pool')
                else:
                    for ct in range(5):
                        k.dma(cs[ct].v, cstash[ct][:, s:s + 512])
                if d == 1:
                    for zi in range(2):
                        p = PS[zi]
                        for kc in range(8):
                            k.mm(p.v, wz[:, kc, zi * 128:(zi + 1) * 128], xT[:, kc, PAD + s:PAD + s + 512], start=(kc == 0), stop=(kc == 7))
                        k.act(zs[zi].v, p.v, AF.Silu)
                    for i in range(2):
                        k.dma(ofw[i].v, ofw_d[i][:, s:s + 512])
                tiles = list(range(4)) if d == 0 else list(range(3, -1, -1))

                def Gstage(t, par):
                    tc = slice(t * 128, (t + 1) * 128)
                    tok0 = PAD + s + t * 128
                    ptr = PS[3]
                    for si, src in enumerate([cs[1], cs[4], cs[2], cs[3]]):
                        k.tr(ptr[:, si * 128:(si + 1) * 128], src[:, tc], c['ident'].v)
                    pg = PS[4]
                    for kc in range(8):
                        k.mm(pg[:, 0:6], xT[:, kc, tok0:tok0 + 128], wgt[:, kc, d * 6:(d + 1) * 6], start=(kc == 0), stop=(kc == 7))
                    yield
                    k.copy(gsm['bd'].v, pg[:, 0:6], e='dve')
                    k.copy(TOK.v, V(ptr, ptr.h[:].rearrange("p (a b) -> p a b", a=4)), e='dve')
                    yield
                    k.act(gcol['beta'].v, gsm['bd'][:, 0:3], AF.Exp, scale=-1.0)
                    k.tt(gcol['t'].v, gsm['bd'][:, 3:6], gpar[:, 6 + d * 3:9 + d * 3], ALU.add)
                    k.act(gcol['t'].v, gcol['t'].v, AF.Exp)
                    k.ts(gcol['beta'].v, gcol['beta'].v, 1.0, None, op0=ALU.add)
                    k.recip(gcol['beta'].v, gcol['beta'].v)
                    yield
                    k.act(gcol['t'].v, gcol['t'].v, AF.Ln, bias=1.0)
                    k.tt(gcol['g'].v, gcol['t'].v, negA[:, d * 3:(d + 1) * 3], ALU.mult)
                    yield
                    k.mm(pg[:, 8:11], tb[:, 0:128], gcol['g'].v)
                    k.mm(pg[:, 11:14], tb[:, 128:256], gcol['g'].v)
                    pg2 = PS[5]
                    k.mm(pg2[0:3, 0:256], gcol['g'].v, tb.v)
                    yield
                    k.copy(gsm['gcl'].v, pg[:, 8:14], e='dve')
                    k.copy(gT.v, pg2[0:3, 0:256], e='act')
                    k.ts(gcol['ngc'].v, gsm['gcl'][:, 0:3], -1.0, None, op0=ALU.mult)
                    k.ts(gcol['nbeta'].v, gcol['beta'].v, -1.0, None, op0=ALU.mult)
                    k.act(gcol['bg'].v, gsm['gcl'][:, 0:3], AF.Exp)
                    k.tt(gcol['kdc'].v, gsm['gcl'][:, 3:6], gsm['gcl'][:, 0:3], ALU.subtract)
                    yield
                    k.tt(gcol['bg'].v, gcol['bg'].v, gcol['beta'].v, ALU.mult)
                    k.act(gcol['kdc'].v, gcol['kdc'].v, AF.Exp)
                    for h in range(3):
                        pb = PS[h]
                        k.mm(pb.v[:, 256:512], sel3[:, h * 128:(h + 1) * 128], gT.v)
                    yield
                    for h in range(3):
                        hh = H[h]; rb = RB[h]; pb = PS[h]
                        k.tt(hh['tmp'].v, negs.v, pb[:, 256:384], ALU.subtract)
                        k.tt(hh['tmp2'].v, pb[:, 256:384], negi.v, ALU.add)
                        k.act(hh['egb'][rb:rb + 64, :], pb[rb:rb + 64, 256:384], AF.Exp)
                        k.act(hh['ecd%d' % par][rb:rb + 64, 0:1], pb[rb:rb + 64, 384:385], AF.Exp)
                        k.act(hh['ecd%d' % par][rb:rb + 64, 1:2], pb[rb:rb + 64, 448:449], AF.Exp)
                        yield
                    for h in range(3):
                        hh = H[h]
                        k.act(hh['D'].v, hh['tmp'].v, AF.Exp, bias=gsm['gcl'][:, h:h + 1])
                        k.act(hh['DT'].v, hh['tmp2'].v, AF.Exp, bias=gcol['ngc'][:, h:h + 1])
                    yield

                def Mstage(t):
                    tc = slice(t * 128, (t + 1) * 128)
                    for h in range(3):
                        hh = H[h]; rb = RB[h]; ph_ = PS[h]
                        kT_ = KT[h][rb:rb + 64, tc]; qT_ = QT[h][rb:rb + 64, tc]
                        mmt(k, ph_[:, 0:128], kT_, kT_, tp=(rb, 0))
                        mmt(k, ph_[:, 128:256], kT_, qT_, tp=(rb, 0))
                        k.stt(hh['T0'][:, 0:128], ph_[:, 0:128], gcol['nbeta'][:, h:h + 1], hh['D'].v, ALU.mult, ALU.mult)
                        k.tt(hh['at'].v, ph_[:, 128:256], hh['DT'].v, ALU.mult)
                        k.tt(hh['qd'][rb:rb + 64, :], qT_, hh['egb'][rb:rb + 64, :], ALU.mult)
                        ks, ko = KTOK[h]; vs, vo = VTOK[h]
                        k.ts(hh['vb'].v, TOK[:, vs, vo:vo + 64], gcol['beta'][:, h:h + 1], None, op0=ALU.mult)
                        k.ts(hh['kbg'].v, TOK[:, ks, ko:ko + 64], gcol['bg'][:, h:h + 1], None, op0=ALU.mult)
                        k.ts(hh['kdec'].v, TOK[:, ks, ko:ko + 64], gcol['kdc'][:, h:h + 1], None, op0=ALU.mult)
                    for h in range(3):
                        hh = H[h]; ph_ = PS[h]
                        k.mm(ph_[:, 128:256], hh['T0'][:, 0:128], identbf.v)
                        k.copy(hh['T0'][:, 128:256], ph_[:, 128:256], e='act')
                    Tc, Tn = 'T0', 'T1'
                    for lvl in range(6):
                        for h in range(3):
                            hh = H[h]; ph_ = PS[h]
                            if lvl == 0:
                                k.mm(ph_[:, 128:256], hh[Tc][:, 0:128], hh[Tc][:, 128:256])
                            elif lvl <= 3:
                                k.mm(ph_[:, 128:384], hh[Tc][:, 0:128], hh[Tc][:, 128:384])
                            else:
                                k.mm(ph_[:, 256:384], hh[Tc][:, 0:128], hh[Tc][:, 256:384])
                            if lvl <= 4:
                                k.mm(ph_[:, 0:128], hh[Tc][:, 128:256], hh[Tc][:, 0:128])
                        for h in range(3):
                            hh = H[h]; ph_ = PS[h]
                            if lvl <= 3:
                                k.copy(hh[Tn][:, 0:256], ph_[:, 0:256], e='act')
                            elif lvl == 4:
                                k.copy(hh[Tn][:, 0:128], ph_[:, 0:128], e='act')
                            if lvl == 0:
                                k.tt(hh[Tn][:, 256:384], hh[Tc][:, 128:256], c['ident'].v, ALU.add)
                            else:
                                k.tt(hh[Tn][:, 256:384], hh[Tc][:, 256:384], ph_[:, 256:384], ALU.add)
                        Tc, Tn = Tn, Tc
                    for h in range(3):
                        hh = H[h]; rb = RB[h]; ph_ = PS[h]
                        X_ = hh[Tc][:, 256:384]
                        k.mm(ph_[:, 384:448], X_, hh['vb'].v)
                        mmt(k, ph_[rb:rb + 64, 0:128], hh['kbg'].v, X_, tp=(0, rb))
                        k.copy(hh['u'].v, ph_[:, 384:448], e='act')
                        k.copy(hh['wT'][rb:rb + 64, :], ph_[rb:rb + 64, 0:128], e='dve')

                def Sstage(t, par):
                    tc = slice(t * 128, (t + 1) * 128)
                    pso = PS[7]; psx = PS[6]
                    chunks = (0, 1) if d == 0 else (1, 0)
                    for cc in chunks:
                        pc_ = cc * 64
                        cols = slice(pc_, pc_ + 64)
                        for h in (0, 2, 1):
                            hh = H[h]; rb = RB[h]
                            mmt(k, psx[pc_:pc_ + 64, h * 64:(h + 1) * 64], hh['wT'][rb:rb + 64, cols], S[h][rb:rb + 64, :], tp=(rb, pc_))
                        yield
                        for h in (0, 2, 1):
                            hh = H[h]
                            k.tt(hh['vnew'][pc_:pc_ + 64, :], hh['u'][pc_:pc_ + 64, :], psx[pc_:pc_ + 64, h * 64:(h + 1) * 64], ALU.subtract)
                        yield
                        for h in (0, 2, 1):
                            hh = H[h]; rb = RB[h]; po = PO[h]
                            oc_ = slice((128 if h == 2 else 0) + pc_, (128 if h == 2 else 0) + pc_ + 64)
                            mmt(k, pso[po:po + 64, oc_], S[h][rb:rb + 64, :], hh['qd'][rb:rb + 64, cols], start=True, stop=False, tp=(rb, po))
                            mmt(k, pso[po:po + 64, oc_], hh['vnew'][pc_:pc_ + 64, :], hh['at'][pc_:pc_ + 64, cols], start=False, stop=True, tp=(pc_, po))
                            mmt(k, psx[rb:rb + 64, 256 + h * 64:256 + (h + 1) * 64], hh['kdec'][pc_:pc_ + 64, :], hh['vnew'][pc_:pc_ + 64, :], tp=(pc_, rb))
                        yield
                        for h in (0, 2, 1):
                            hh = H[h]; rb = RB[h]
                            k.stt(S[h][rb:rb + 64, :], S[h][rb:rb + 64, :], hh['ecd%d' % par][rb:rb + 64, cc:cc + 1],
                                  psx[rb:rb + 64, 256 + h * 64:256 + (h + 1) * 64], ALU.mult, ALU.add)
                        yield
                    if d == 0:
                        k.copy(obuf[0][:, tc], pso[:, 0:128], e='act')
                        k.copy(obuf[1][0:64, tc], pso[0:64, 128:256], e='act')
                    else:
                        k.tt(obuf[0][:, tc], pso[:, 0:128], ofw[0][:, tc], ALU.add)
                        k.tt(obuf[1][0:64, tc], pso[0:64, 128:256], ofw[1][0:64, tc], ALU.add)

                def merged(gens):
                    alive = list(gens)
                    while alive:
                        for g_ in list(alive):
                            try:
                                next(g_)
                            except StopIteration:
                                alive.remove(g_)

                merged([Gstage(tiles[0], 0)])
                for ti, t in enumerate(tiles):
                    Mstage(t)
                    gens = [Sstage(t, ti % 2)]
                    if ti + 1 < len(tiles):
                        gens.append(Gstage(tiles[ti + 1], (ti + 1) % 2))
                    merged(gens)
                if d == 0:
                    for i in range(2):
                        k.dma(ofw_d[i][:, s:s + 512], obuf[i].v, q='pool')
                else:
                    if io.get('gd_dbg') is not None:
                        for i in range(2):
                            k.dma(io['gd_dbg'][i * 128:(i + 1) * 128, s:s + 512], obuf[i].v)
                    dst = mix_dest(blk)
                    for i in range(2):
                        rows = 128 if i == 0 else 64
                        k.tt(sq.v, obuf[i].v, obuf[i].v, ALU.mult)
                        pn = PS[2]
                        k.mm(pn.v, c['bd64'].v, sq.v)
                        k.act(rn.v, pn.v, AF.Sqrt, bias=c['eps'].v, scale=1.0 / 64.0)
                        k.recip(rn.v, rn.v)
                        k.stt(sq[0:rows, :], obuf[i][0:rows, :], gnorm[0:rows, :], rn[0:rows, :], ALU.mult, ALU.mult)
                        k.tt(dst[i], sq[0:rows, :], zs[i][0:rows, :], ALU.mult)
                    mix_dest(blk, done=True)
        barrier(k)
        k.es = old


O_AQ, O_AK, O_AV, O_AZ, O_AB, O_AD = 0, 384, 768, 1152, 1536, 1548
O_BQ, O_BK, O_BV = 1560, 1944, 2072
O_CF, O_CI, O_CQ, O_CG = 2200, 2712, 2968, 3224

def host_consts():
    c = {}
    c['ident'] = np.eye(128, dtype=np.float32)
    bd = np.zeros((128,128), np.float32); bd[:64,:64]=1; bd[64:,64:]=1
    c['bd64'] = bd
    rt = np.zeros((128,128), np.float32)
    for b in range(4):
        for i in range(16):
            rt[b*32+i+16, b*32+i] = -1.0
            rt[b*32+i, b*32+i+16] = 1.0
    c['rt128'] = rt
    s = np.arange(4096)
    pos = np.stack([(s//64).astype(np.float32), (s%64).astype(np.float32)], 0)
    inv = (np.float32(10000.0) ** (-np.arange(0, 32, 2, dtype=np.float32) / np.float32(32))).astype(np.float32)
    d = np.arange(64)
    ang = (pos[d//32][:, :] * inv[d%16][:, None]).astype(np.float32)
    c['cos'] = np.ascontiguousarray(np.concatenate([np.cos(ang), np.cos(ang)], 0).astype(np.float32))
    c['sin'] = np.ascontiguousarray(np.concatenate([np.sin(ang), np.sin(ang)], 0).astype(np.float32))
    return c

def gqa_w(w_in_l, qw, kw, hf):
    z64 = np.zeros((1024,64), np.float32)
    bq = lambda h: w_in_l[:, O_BQ+h*64:O_BQ+(h+1)*64]
    bk = w_in_l[:, O_BK+hf*64:O_BK+(hf+1)*64]
    wg = np.concatenate([bq(3*hf), bq(3*hf+1), bk, bk, bq(3*hf+2), z64], 1)
    wbv = w_in_l[:, O_BV+hf*64:O_BV+(hf+1)*64]
    gnw = np.stack([np.concatenate([qw,qw]), np.concatenate([kw,kw]), np.concatenate([qw, np.zeros(64,np.float32)])], 1)
    return dict(wg=np.ascontiguousarray(wg), wbv=np.ascontiguousarray(wbv), gnw=np.ascontiguousarray(gnw.astype(np.float32)))

def scan_consts():
    c = {}
    idx = np.arange(128); ch = idx // 64
    same = ch[:, None] == ch[None, :]
    for sfx in ('f', 'b'):
        if sfx == 'f':
            tri = same & (idx[None, :] <= idx[:, None])
            mid = ch * 64 + 31
        else:
            tri = same & (idx[None, :] >= idx[:, None])
            mid = ch * 64 + 32
        tri = tri.astype(np.float32)
        trirel = tri - tri[mid, :]
        cext = np.zeros((128, 4), np.float32)
        for cc in range(2):
            cext[:, cc] = tri[cc * 64 + (31 if sfx == 'f' else 32), :]
            cext[:, 2 + cc] = (ch == cc).astype(np.float32)
        c['trt_' + sfx] = np.ascontiguousarray(trirel.T)
        c['trx_' + sfx] = np.ascontiguousarray(np.concatenate([trirel.T, cext, np.zeros((128, 124), np.float32)], 1))
        c['mki_' + sfx] = np.ascontiguousarray(tri.T)
        c['tria_' + sfx] = np.ascontiguousarray(tri.T)
    return c

def hgrn_w(w_in_l, lbl, hnw, hf):
    hs = [2*hf, 2*hf+1]
    def cols(o): return np.concatenate([w_in_l[:, o+h*64:o+(h+1)*64] for h in hs], 1)
    wfm = np.concatenate([cols(O_CF), cols(O_CF+256), cols(O_CQ), cols(O_CG)], 1)
    wtm = np.concatenate([cols(O_CF), cols(O_CI), cols(O_CF+256)], 1)
    ch = np.concatenate([np.arange(h*64,(h+1)*64) for h in hs])
    return dict(wh_fm=np.ascontiguousarray(wfm), wh_tm=np.ascontiguousarray(wtm),
                lbl_col=np.ascontiguousarray(lbl[:, ch].T.astype(np.float32)), lbl_row=np.ascontiguousarray(lbl[:, ch].astype(np.float32)),
                hnw=np.ascontiguousarray(np.concatenate([hnw,hnw])[:,None].astype(np.float32)))

def gdn_consts():
    c = {}
    idx = np.arange(128); ch = idx // 64
    same = ch[:, None] == ch[None, :]
    bd = same.astype(np.float32)
    for sfx in ('f', 'b'):
        if sfx == 'f':
            incl = same & (idx[None, :] <= idx[:, None])
            strict = same & (idx[None, :] < idx[:, None])
        else:
            incl = same & (idx[None, :] >= idx[:, None])
            strict = same & (idx[None, :] > idx[:, None])
        c['tb_' + sfx] = np.ascontiguousarray(np.concatenate([incl.T.astype(np.float32), bd], 1))
        c['negs_' + sfx] = np.where(strict, 0.0, -30000.0).astype(np.float32)
        c['negi_' + sfx] = np.where(incl.T, 0.0, -30000.0).astype(np.float32)
    c['sel3'] = np.repeat(np.eye(3, dtype=np.float32), 128, axis=1)
    return c

def gdn_w(w_in_l, conv_w_l, a_log_l, dt_bias_l, gnw, hf):
    hs = [3*hf, 3*hf+1, 3*hf+2]
    q = lambda h: O_AQ + h*64; kk = lambda h: O_AK + h*64; v = lambda h: O_AV + h*64
    chans = [q(hs[0]), q(hs[1]), kk(hs[0]), kk(hs[1]), q(hs[2]), v(hs[2]), kk(hs[2]), None, v(hs[0]), v(hs[1])]
    wc = np.zeros((1024, 640), np.float32); cw = np.zeros((5, 640), np.float32)
    for u, o in enumerate(chans):
        if o is None: continue
        wc[:, u*64:(u+1)*64] = w_in_l[:, o:o+64]
        cw[:, u*64:(u+1)*64] = conv_w_l[:, o:o+64]
    wz = np.zeros((1024, 256), np.float32)
    for u, h in enumerate(hs):
        wz[:, u*64:(u+1)*64] = w_in_l[:, O_AZ + h*64:O_AZ + (h+1)*64]
    cols = [O_AB + 0*6 + h for h in hs] + [O_AD + 0*6 + h for h in hs] + [O_AB + 6 + h for h in hs] + [O_AD + 6 + h for h in hs]
    wgt = np.ascontiguousarray(w_in_l[:, cols])
    gpar = np.concatenate([a_log_l[0, hs], a_log_l[1, hs], dt_bias_l[0, hs], dt_bias_l[1, hs]]).astype(np.float32)
    return dict(wc=wc, cw=cw, wz=wz, wgt=wgt, gpar=gpar, gnorm=np.concatenate([gnw, gnw])[:, None].astype(np.float32))


A_INPUTS = {
    'wc': [1024, 640], 'cw': [5, 640], 'wz': [1024, 256], 'wgt': [1024, 12], 'gpar': [12], 'gnorm': [128, 1],
    'wh_fm': [1024, 512], 'wh_tm': [1024, 384], 'hnw': [128, 1], 'wg': [1024, 384], 'wbv': [1024, 64], 'gnw': [128, 3],
}
A_CONSTS = {
    'ident': [128, 128], 'bd64': [128, 128], 'rt128': [128, 128], 'cos': [128, SEQ], 'sin': [128, SEQ], 'sel3': [3, 384],
    'tb_f': [128, 256], 'negs_f': [128, 128], 'negi_f': [128, 128], 'tb_b': [128, 256], 'negs_b': [128, 128], 'negi_b': [128, 128],
    'trx_f': [128, 256], 'trt_f': [128, 128], 'mki_f': [128, 128], 'trx_b': [128, 256], 'trt_b': [128, 128], 'mki_b': [128, 128],
    'sel8': [8, 1024], 'lbl_col': [128, 2], 'lbl_row': [2, 128], 'selw': [128, 2], 'memT': [1024, 256],
}
B_INPUTS = {'w_out': [1024, 1024], 'xq': [1024, 1024], 'xk': [1024, 1024], 'xv': [1024, 1024], 'xo': [1024, 1024],
            'ln1_g': [1024], 'ln1_b': [1024], 'ln2_g': [1024], 'ln2_b': [1024], 'ln3_g': [1024], 'ln3_b': [1024]}
B_DENSE = {'ffn_wg': [1024, 2816], 'ffn_wu': [1024, 2816], 'ffn_wd': [2816, 1024]}
B_MOE = {'router': [1024, 8], 'moe_wg': [8, 1024, 3584], 'moe_wu': [8, 1024, 3584], 'moe_wd': [8, 3584, 1024]}
DEPTH = 2


def phase_a(k, io, layer, xsrc, out_view, PS):
    with ExitStack() as sa:
        old = k.es
        k.es = sa
        c = consts_a(k, io)
        xT = k.sb([128, 8, SEQ + 2 * PAD], BF16)
        load_xT(k, xT, xsrc)
        mt = [k.sb([128, 2, 512], BF16) for _ in range(2)]
        for m_ in mt:
            k.memset(m_.v, 0.0)

        def gd_dest(blk, done=False):
            m_ = mt[blk % 2]
            if not done:
                return (m_[:, 0, :], m_[0:64, 1, :])
            k.dma(out_view(0, 128, blk * 512, (blk + 1) * 512), m_[:, 0, :], q='pool')
            k.dma(out_view(128, 192, blk * 512, (blk + 1) * 512), m_[0:64, 1, :], q='pool')

        def hg_dest(blk, done=False):
            m_ = mt[blk % 2]
            if not done:
                return m_[:, 0, :]
            k.dma(out_view(256, 384, blk * 512, (blk + 1) * 512), m_[:, 0, :])

        mtq = [k.sb([128, 2, 512], BF16) for _ in range(2)]

        def gq_dest(blk, done=False):
            m_ = mtq[blk % 2]
            if not done:
                return [m_[0:64, 0, :], m_[64:128, 0, :], m_[64:128, 1, :]]
            k.dma(out_view(384, 512, blk * 512, (blk + 1) * 512), m_[:, 0, :])
            k.dma(out_view(192, 256, blk * 512, (blk + 1) * 512), m_[64:128, 1, :])

        gdn(k, io, xT, c, PS, layer, gd_dest)
        with ExitStack() as sg:
            old2 = k.es
            k.es = sg
            attn = gqa_prepare(k, io, xT, c, PS, gq_dest)
            gen = attn([PS[0], PS[1], PS[4]])

            def tick(n):
                for _ in range(n):
                    next(gen, None)
            hgrn(k, io, xT, c, PS, layer, hg_dest, tick=tick)
            for _ in gen:
                pass
            barrier(k)
            k.es = old2
        barrier(k)
        k.es = old


def build_fused():
    nc = bass.Bass("TRN2", target_bir_lowering=False)
    names = []
    with ExitStack() as es:
        k = K(nc, es)

        def inp(n, shape, dt=F32):
            names.append(n)
            return k.dram(n, shape, dt, kind='ExternalInput')
        cst = {n: inp(n, s) for n, s in A_CONSTS.items()}
        xT0 = inp('xT0', [1024, SEQ])
        x0 = inp('x0', [TOK, 1024])
        out_final = k.dram('out', [TOK, 1024], F32, kind='ExternalOutput')
        PS = [k.ps([128, 512]) for _ in range(8)]
        x_res = x0
        xg = None
        for l in range(DEPTH):
            moe = (l % 2 == 1)
            last = (l == DEPTH - 1)
            ioa = dict(cst)
            for n, s in A_INPUTS.items():
                ioa[n] = inp('%s_l%d' % (n, l), s)
            if l == 0:
                xsrc = lambda kc, hh: xT0[kc * 128:(kc + 1) * 128, hh * 2048:(hh + 1) * 2048]
            else:
                xsrc = (lambda g: (lambda kc, hh: g[kc // 4][hh * 512 + (kc % 4) * 128:hh * 512 + (kc % 4 + 1) * 128, :]))(xg)
            mixh = [k.dram('mixh%d_l%d' % (j, l), [256, SEQ], BF16) for j in range(2)]
            mixf = [k.dram('mixf%d_l%d' % (j, l), [512, SEQ], BF16) for j in range(2)]
            with nc.named_scope('A%d' % l):
                phase_a(k, ioa, l, xsrc, lambda r0, r1, c0, c1: mixh[r0 // 256][r0 % 256:r0 % 256 + (r1 - r0), c0:c1], PS)
            with nc.named_scope('cc_mix%d' % l):
                for j in range(2):
                    k.allgather_pairs(mixf[j], mixh[j])
            iob = {'ident': cst['ident'], 'memT': cst['memT'], 'sel8': cst['sel8'], 'x': x_res}
            spec = dict(B_INPUTS)
            spec.update(B_MOE if moe else B_DENSE)
            for n, s in spec.items():
                iob[n] = inp('%s_l%d' % (n, l), s)
            with ExitStack() as sb_:
                old = k.es
                k.es = sb_

                def mix_loader(bufA, mixf=mixf):
                    with ExitStack() as sl:
                        o2 = k.es
                        k.es = sl
                        bufA2 = k.sb([128, 8, TOK], BF16)
                        selw = k.sb([128, 2])
                        k.dma(selw.v, cst['selw'].v)
                        for th, buf in enumerate([bufA, bufA2]):
                            for kc in range(8):
                                hf, i = kc // 4, kc % 4
                                r0 = hf * 256 + (i % 2) * 128
                                k.dma(buf[:, kc, :], mixf[i // 2][r0:r0 + 128, th * TOK:(th + 1) * TOK])
                        for kc in range(8):
                            k.ts(bufA2[:, kc, :], bufA2[:, kc, :], selw[:, 1:2], None, op0=ALU.mult)
                            k.stt(bufA[:, kc, :], bufA[:, kc, :], selw[:, 0:1], bufA2[:, kc, :], ALU.mult, ALU.add)
                        barrier(k)
                        k.es = o2
                iob['mix_loader'] = mix_loader
                if last:
                    iob['out'] = out_final
                else:
                    x_next = k.dram('x3_l%d' % l, [TOK, 1024])
                    x3T = [k.dram('x3T%d_l%d' % (j, l), [512, TOK], BF16) for j in range(2)]
                    iob['out'] = x_next

                    def outT_writer(tt, oT_, x3T=x3T):
                        for j in range(2):
                            k.dma(V(x3T[j], x3T[j].h[:, tt * 128:(tt + 1) * 128].rearrange("(kc p) t -> p kc t", p=128)),
                                  oT_[:, j * 4:(j + 1) * 4, :], q='pool')
                    iob['outT'] = outT_writer
                with nc.named_scope('B%d' % l):
                    phase_b(k, sb_, iob, moe, PS=PS, tag='_l%d' % l)
                barrier(k)
                k.es = old
            if not last:
                xg = [k.dram('xg%d_l%d' % (j, l), [1024, TOK], BF16) for j in range(2)]
                for j in range(2):
                    k.allgather_pairs(xg[j], x3T[j])
                x_res = x_next
        k.finish()
    return nc, names


def mix_perm():
    oa = lambda h: list(range(h * 64, (h + 1) * 64))
    ob = lambda h: list(range(384 + h * 64, 384 + (h + 1) * 64))
    oc = lambda h: list(range(768 + h * 64, 768 + (h + 1) * 64))
    p = []
    for hf in range(2):
        p += oa(3 * hf) + oa(3 * hf + 1) + oa(3 * hf + 2) + ob(3 * hf + 2) + oc(2 * hf) + oc(2 * hf + 1) + ob(3 * hf) + ob(3 * hf + 1)
    return np.array(p)


def kernel(**inp):
    inp = {n: np.asarray(v) for n, v in inp.items()}
    x = np.ascontiguousarray(inp['x'], dtype=np.float32)
    B = x.shape[0]
    C = host_consts()
    C.update(scan_consts())
    C.update(gdn_consts())
    C['sel8'] = np.repeat(np.eye(8, dtype=np.float32), 128, axis=1)
    perm = mix_perm()
    nc, names = build_fused()
    cores = list(range(8))
    shared = {}
    for l in range(DEPTH):
        for n in ['xq', 'xk', 'xv', 'xo', 'ln1_g', 'ln1_b', 'ln2_g', 'ln2_b', 'ln3_g', 'ln3_b']:
            shared['%s_l%d' % (n, l)] = inp[n][l]
        shared['w_out_l%d' % l] = inp['w_out'][l][perm]
        if l % 2 == 1:
            shared['router_l%d' % l] = inp['moe_router'][l // 2]
            for n in ['moe_wg', 'moe_wu', 'moe_wd']:
                shared['%s_l%d' % (n, l)] = inp[n][l // 2]
        else:
            for n in ['ffn_wg', 'ffn_wu', 'ffn_wd']:
                shared['%s_l%d' % (n, l)] = inp[n][l // 2]
    shared = {n: np.ascontiguousarray(v, dtype=np.float32) for n, v in shared.items()}
    per_hf = []
    for hf in range(2):
        m = {}
        for l in range(DEPTH):
            w = {}
            w.update(gdn_w(inp['w_in'][l], inp['conv_w'][l], inp['gdn_a_log'][l], inp['gdn_dt_bias'][l], inp['gdn_norm_w'][l], hf))
            hw = hgrn_w(inp['w_in'][l], inp['hgrn_lb_logits'], inp['hgrn_norm_w'][l], hf)
            m['lbl_col'] = hw.pop('lbl_col'); m['lbl_row'] = hw.pop('lbl_row')
            w.update(hw)
            w.update(gqa_w(inp['w_in'][l], inp['q_norm_w'][l], inp['k_norm_w'][l], hf))
            for n, v in w.items():
                m['%s_l%d' % (n, l)] = np.ascontiguousarray(v, dtype=np.float32)
        per_hf.append(m)
    maps = []
    for core in cores:
        b, r = core // 2, core % 2
        m = dict(C)
        m.update(shared)
        m.update(per_hf[r])
        m['xT0'] = np.ascontiguousarray(x[b].T)
        m['x0'] = np.ascontiguousarray(x[b, r * TOK:(r + 1) * TOK])
        m['memT'] = np.ascontiguousarray(inp['mem'][b].T.astype(np.float32))
        sw = np.zeros((128, 2), np.float32); sw[:, r] = 1.0
        m['selw'] = sw
        maps.append({n: m[n] for n in names})
    res = run_bass_kernel_spmd(nc, maps, core_ids=cores)
    out = np.empty((B, SEQ, 1024), np.float32)
    for core in cores:
        b, r = core // 2, core % 2
        out[b, r * TOK:(r + 1) * TOK] = res.results[core]['out']
    return out
```

```python
import os
import numpy as np
from contextlib import ExitStack
import concourse.bass as bass
import concourse.mybir as mybir
from concourse.bass_utils import run_bass_kernel_spmd

F32 = mybir.dt.float32
BF16 = mybir.dt.bfloat16
AF = mybir.ActivationFunctionType
ALU = mybir.AluOpType
AX = mybir.AxisListType


class T:
    def __init__(self, h, name):
        self.h = h
        self.name = name
        self.lw = None
        self.rd = {}
        self.psum = False

    def __getitem__(self, idx):
        return V(self, self.h[idx])

    @property
    def v(self):
        return V(self, self.h[:])


class V:
    def __init__(self, t, ap):
        self.t = getattr(t, 't', t)
        self.ap = ap

    def __getitem__(self, idx):
        return V(self.t, self.ap[idx])


class K:
    NRING = 6

    def __init__(self, nc, es):
        self.nc = nc
        self.es = es
        self.es0 = es
        self.eng = {'pe': nc.tensor, 'dve': nc.vector, 'act': nc.scalar,
                    'pool': nc.gpsimd, 'sp': nc.sync}
        self.sem = {e: es.enter_context(nc.semaphore('s_' + e)) for e in self.eng}
        self.cnt = {e: 0 for e in self.eng}
        self.known = {e: {} for e in self.eng}
        self.ring = {}
        for q in ('sp', 'pool', 'act'):
            self.ring[q] = [[es.enter_context(nc.semaphore('d_%s%d' % (q, i))), 0]
                            for i in range(self.NRING)]
        self.ring_i = {q: 0 for q in self.ring}
        self.nalloc = 0
        self.ninstr = 0

    def sb(self, shape, dt=F32, name=None):
        self.nalloc += 1
        name = name or 'sb%d' % self.nalloc
        h = self.es.enter_context(self.nc.sbuf_tensor(name, list(shape), dt))
        return T(h, name)

    def ps(self, shape, dt=F32, name=None):
        self.nalloc += 1
        name = name or 'ps%d' % self.nalloc
        h = self.es.enter_context(self.nc.psum_tensor(name, list(shape), dt))
        t = T(h, name)
        t.psum = True
        return t

    def dram(self, name, shape, dt=F32, kind=None):
        if kind is None:
            h = self.nc.dram_tensor(name, list(shape), dt)
        else:
            h = self.nc.dram_tensor(name, list(shape), dt, kind=kind)
        return T(h, name)

    def _wait(self, e, ev):
        if ev is None:
            return
        sem, val = ev[0], ev[1]
        kn = self.known[e]
        if kn.get(sem.name, 0) >= val:
            return
        self.eng[e].wait_ge(sem, val)
        kn[sem.name] = val
        self.ninstr += 1

    def _pre(self, e, reads, writes):
        for v in reads:
            t = v.t
            if t.lw is not None:
                if not (e == 'pe' and t.lw[2] == 'pe'):
                    self._wait(e, t.lw)
            if t.psum:
                for src, ev in t.rd.items():
                    if src != e:
                        self._wait(e, ev)
        for v in writes:
            t = v.t
            if t.lw is not None and not (e == 'pe' and t.lw[2] == 'pe'):
                self._wait(e, t.lw)
            for src, ev in t.rd.items():
                if not (e == 'pe' and src == 'pe'):
                    self._wait(e, ev)

    def _post(self, src, ev3, reads, writes):
        for v in writes:
            v.t.lw = ev3
            v.t.rd = {}
        for v in reads:
            if v.t.lw is ev3:
                continue
            v.t.rd[src] = (ev3[0], ev3[1])

    def emit(self, e, fn, reads, writes):
        reads = [r for r in reads if isinstance(r, V)]
        writes = [w for w in writes if isinstance(w, V)]
        self._pre(e, reads, writes)
        ins = fn(self.eng[e])
        self.cnt[e] += 1
        ins.then_inc(self.sem[e], 1)
        self.ninstr += 1
        ev = (self.sem[e], self.cnt[e], e)
        self._post(e, ev, reads, writes)
        return ins

    def dma(self, out, in_, q='sp', **kw):
        ring = self.ring[q]
        i = self.ring_i[q]
        self.ring_i[q] = (i + 1) % len(ring)
        slot = ring[i]
        sem, uses = slot
        if uses > 0:
            self._wait(q, (sem, 16 * uses))
        self._pre(q, [in_], [out])
        ins = self.eng[q].dma_start(out=out.ap, in_=in_.ap, **kw)
        ins.then_inc(sem, 16)
        slot[1] = uses + 1
        self.ninstr += 1
        src = 'dma_' + sem.name
        ev = (sem, 16 * (uses + 1), src)
        self._post(src, ev, [in_], [out])
        return ins

    def allgather_pairs(self, out_t, in_t):
        self.ncc = getattr(self, 'ncc', 0) + 1
        sem = self.es0.enter_context(self.nc.semaphore('cc%d' % self.ncc))
        self._pre('pool', [in_t.v], [out_t.v])
        ins = self.eng['pool'].collective_compute(
            "AllGather", ALU.bypass, replica_groups=[[0, 1], [2, 3], [4, 5], [6, 7]],
            ins=[in_t.h.ap().opt()], outs=[out_t.h.ap().opt()])
        ins.then_inc(sem, 1)
        self.ninstr += 1
        ev = (sem, 1, 'cc%d' % self.ncc)
        self._post(ev[2], ev, [in_t.v], [out_t.v])
        self.ccsems = getattr(self, 'ccsems', []) + [sem]

    def finish(self, e='sp'):
        for q, ring in self.ring.items():
            for sem, uses in ring:
                if uses > 0:
                    self._wait(e, (sem, 16 * uses))

    @staticmethod
    def _a(x):
        return x.ap if isinstance(x, V) else x

    def mm(self, out, lhsT, rhs, start=True, stop=True, **kw):
        tp = kw.get('tile_position')
        rows = (tp[0] if tp else 0, lhsT.ap.shape[0])
        t = out.t
        if t.lw is not None and t.lw[2] == 'pe' and getattr(t, 'pe_rows', rows) != rows:
            self._wait('pe', t.lw)
        t.pe_rows = rows
        return self.emit('pe', lambda g: g.matmul(out.ap, lhsT.ap, rhs.ap, start=start, stop=stop, **kw),
                         [lhsT, rhs], [out])

    def tr(self, out, in_, ident):
        rows = (0, in_.ap.shape[0])
        t = out.t
        if t.lw is not None and t.lw[2] == 'pe' and getattr(t, 'pe_rows', rows) != rows:
            self._wait('pe', t.lw)
        t.pe_rows = rows
        return self.emit('pe', lambda g: g.transpose(out.ap, in_.ap, ident.ap), [in_, ident], [out])

    def act(self, out, in_, func, bias=0.0, scale=1.0, accum=None, e='act'):
        a = self._a
        kw = {}
        if accum is not None:
            kw['accum_out'] = accum.ap
        return self.emit('act', lambda g: g.activation(out.ap, in_.ap, func, bias=a(bias), scale=a(scale), **kw),
                         [in_, bias, scale], [out] + ([accum] if accum is not None else []))

    def tt(self, out, a_, b_, op, e='dve'):
        return self.emit(e, lambda g: g.tensor_tensor(out.ap, a_.ap, b_.ap, op), [a_, b_], [out])

    def ts(self, out, in_, s1, s2=None, op0=ALU.mult, op1=None, e='dve', accum=None):
        a = self._a
        kw = {}
        if op1 is not None:
            kw['op1'] = op1
        if accum is not None:
            kw['accum_out'] = accum.ap
        return self.emit(e, lambda g: g.tensor_scalar(out.ap, in_.ap, a(s1), a(s2), op0, **kw),
                         [in_, s1, s2], [out] + ([accum] if accum is not None else []))

    def stt(self, out, in0, scalar, in1, op0, op1, e='dve'):
        a = self._a
        return self.emit(e, lambda g: g.scalar_tensor_tensor(out.ap, in0.ap, a(scalar), in1.ap, op0, op1),
                         [in0, scalar, in1], [out])

    def copy(self, out, in_, e='dve'):
        if e == 'act':
            return self.emit('act', lambda g: g.copy(out.ap, in_.ap), [in_], [out])
        return self.emit(e, lambda g: g.tensor_copy(out.ap, in_.ap), [in_], [out])

    def memset(self, out, val, e='dve'):
        return self.emit(e, lambda g: g.memset(out.ap, val), [], [out])

    def recip(self, out, in_):
        return self.emit('dve', lambda g: g.reciprocal(out.ap, in_.ap), [in_], [out])

    def reduce(self, out, in_, op, axis=AX.X, e='dve'):
        return self.emit(e, lambda g: g.tensor_reduce(out.ap, in_.ap, axis, op), [in_], [out])

    def bn_stats(self, out, in_):
        return self.emit('dve', lambda g: g.bn_stats(out.ap, in_.ap), [in_], [out])

    def bn_aggr(self, out, in_):
        return self.emit('dve', lambda g: g.bn_aggr(out.ap, in_.ap), [in_], [out])

    def max8(self, out, in_):
        return self.emit('dve', lambda g: g.max(out.ap, in_.ap), [in_], [out])


ALPHA = float((2 * 2) ** 0.25)
LN_EPS = 1e-5
NEXP = 8
NT = 16
TOK = 2048


def barrier(k):
    for e in k.eng:
        for f in k.eng:
            if f != e and k.cnt[f] > 0:
                k._wait(e, (k.sem[f], k.cnt[f]))
        for q, ring in k.ring.items():
            for sem, uses in ring:
                if uses > 0:
                    k._wait(e, (sem, 16 * uses))


def load_w_rows(k, dst, src_ap, nk, q='pool'):
    for kc in range(nk):
        k.dma(dst[:, kc, :], V(src_ap.t, src_ap.ap[kc * 128:(kc + 1) * 128, :]), q=q)


def layer_norm_tile(k, t, g_bc, b_bc, out, small):
    st, mv, rstd, nmr = small['st'], small['mv'], small['rstd'], small['nmr']
    k.bn_stats(st[:, 0, :], t[:, 0:512])
    k.bn_stats(st[:, 1, :], t[:, 512:1024])
    k.bn_aggr(mv.v, st.v)
    k.act(rstd.v, mv[:, 1:2], AF.Sqrt, bias=small['eps'].v)
    k.recip(rstd.v, rstd.v)
    k.stt(nmr.v, mv[:, 0:1], -1.0, rstd.v, ALU.mult, ALU.mult)
    k.act(t.v, t.v, AF.Identity, bias=nmr.v, scale=rstd.v)
    k.tt(t.v, t.v, g_bc.v, ALU.mult)
    k.tt(out.v, t.v, b_bc.v, ALU.add)


def proj_ln(k, es, srcT, w_d, res_d, g_d, b_d, out_d, outT, ident, PS, outT32=None, out_q='pool'):
    with ExitStack() as s2:
        k2 = k
        old = k.es
        k.es = s2
        w = k.sb([128, 8, 1024], BF16)
        g_bc = k.sb([128, 1024]); b_bc = k.sb([128, 1024])
        small = dict(st=k.sb([128, 2, 6]), mv=k.sb([128, 2]), rstd=k.sb([128, 1]), nmr=k.sb([128, 1]), eps=k.sb([128, 1]))
        k.memset(small['eps'].v, LN_EPS)
        xt = [k.sb([128, 1024]) for _ in range(2)]
        tt_ = [k.sb([128, 1024]) for _ in range(2)]
        ot = [k.sb([128, 1024]) for _ in range(2)]
        load_w_rows(k, w, w_d.v, 8)
        k.dma(g_bc.v, V(g_d, g_d.h[:].partition_broadcast(128)))
        k.dma(b_bc.v, V(b_d, b_d.h[:].partition_broadcast(128)))
        def mm_stage(tt):
            k.dma(xt[tt % 2].v, res_d[tt * 128:(tt + 1) * 128, :])
            for nt in range(2):
                p = PS[(tt % 2) * 4 + nt]
                for kc in range(8):
                    k.mm(p.v, srcT[:, kc, tt * 128:(tt + 1) * 128], w[:, kc, nt * 512:(nt + 1) * 512],
                         start=(kc == 0), stop=(kc == 7))

        def ln_stage(tt):
            x_ = xt[tt % 2]; t_ = tt_[tt % 2]; o_ = ot[tt % 2]
            for nt in range(2):
                p = PS[(tt % 2) * 4 + nt]
                k.stt(t_[:, nt * 512:(nt + 1) * 512], x_[:, nt * 512:(nt + 1) * 512], ALPHA, p.v, ALU.mult, ALU.add)
            layer_norm_tile(k, t_, g_bc, b_bc, o_, small)
            if out_d is not None:
                k.dma(out_d[tt * 128:(tt + 1) * 128, :], o_.v, q=out_q)

        def tr_stage(tt):
            o_ = ot[tt % 2]
            for half in range(2):
                p = PS[(tt % 2) * 4 + 2 + half]
                for j in range(4):
                    kc = half * 4 + j
                    k.tr(p[:, j * 128:(j + 1) * 128], o_[:, kc * 128:(kc + 1) * 128], ident.v)
                k.copy(outT[:, half * 4:half * 4 + 4, tt * 128:(tt + 1) * 128],
                       V(p, p.h[:].rearrange("p (j t) -> p j t", j=4)), e='act')
                if outT32 is not None:
                    k.copy(outT32[tt % 2][:, half * 4:half * 4 + 4, :],
                           V(p, p.h[:].rearrange("p (j t) -> p j t", j=4)), e='dve')

        mm_stage(0)
        for tt in range(NT):
            ln_stage(tt)
            if tt + 1 < NT:
                mm_stage(tt + 1)
            tr_stage(tt)
            if outT32 is not None and tt >= 1:
                outT32[2](tt - 1, outT32[(tt - 1) % 2])
        if outT32 is not None:
            outT32[2](NT - 1, outT32[(NT - 1) % 2])
        barrier(k)
        k.es = old


def cross_attn(k, es, x1T, memT_d, xq_d, xk_d, xv_d, attnT, PS, ones_bf):
    with ExitStack() as s2:
        old = k.es
        k.es = s2
        memT = k.sb([128, 8, 256], BF16)
        load_w_rows(k, memT, memT_d.v, 8)
        kT = k.sb([128, 8, 256], BF16)
        Vm = k.sb([128, 2, 1024], BF16)
        qT = k.sb([128, 8, TOK], BF16)
        with ExitStack() as s3:
            k.es = s3
            wk = k.sb([128, 8, 1024], BF16)
            wv = k.sb([128, 8, 1024], BF16)
            wq = k.sb([128, 8, 1024], BF16)
            load_w_rows(k, wk, xk_d.v, 8)
            load_w_rows(k, wv, xv_d.v, 8)
            load_w_rows(k, wq, xq_d.v, 8)
            for mt in range(8):
                p = PS[mt % 2]
                for kc in range(8):
                    k.mm(p[:, 0:256], wk[:, kc, mt * 128:(mt + 1) * 128], memT[:, kc, :], start=(kc == 0), stop=(kc == 7))
                k.copy(kT[:, mt, :], p[:, 0:256], e='act')
            for m in range(2):
                for nt in range(2):
                    p = PS[2 + nt]
                    for kc in range(8):
                        k.mm(p.v, memT[:, kc, m * 128:(m + 1) * 128], wv[:, kc, nt * 512:(nt + 1) * 512],
                             start=(kc == 0), stop=(kc == 7))
                    k.copy(Vm[:, m, nt * 512:(nt + 1) * 512], p.v, e='dve')
            i = 0
            for mt in range(8):
                for n in range(4):
                    p = PS[4 + i % 4]; i += 1
                    for kc in range(8):
                        k.mm(p.v, wq[:, kc, mt * 128:(mt + 1) * 128], x1T[:, kc, n * 512:(n + 1) * 512],
                             start=(kc == 0), stop=(kc == 7))
                    k.copy(qT[:, mt, n * 512:(n + 1) * 512], p.v, e=('act' if i % 2 else 'dve'))
            barrier(k)
        k.es = s2
        pT = [[k.sb([128, 512], BF16) for _ in range(2)] for _ in range(2)]
        rs = [k.sb([128, 512]) for _ in range(2)]
        its = [(h, n) for h in range(4) for n in range(4)]

        def scores(i):
            h, n = its[i]
            pp = pT[i % 2]
            for m in range(2):
                p = PS[(i % 2) * 5 + m]
                for dc in range(2):
                    k.mm(p.v, kT[:, 2 * h + dc, m * 128:(m + 1) * 128], qT[:, 2 * h + dc, n * 512:(n + 1) * 512],
                         start=(dc == 0), stop=(dc == 1))
                k.act(pp[m].v, p.v, AF.Exp, scale=1.0 / 16.0)

        scores(0)
        for i, (h, n) in enumerate(its):
            pp = pT[i % 2]; r_ = rs[i % 2]
            if i + 1 < len(its):
                scores(i + 1)
            ps_ = PS[2]
            for m in range(2):
                k.mm(ps_.v, ones_bf.v, pp[m].v, start=(m == 0), stop=(m == 1))
            k.recip(r_.v, ps_.v)
            for dc in range(2):
                p = PS[3 + dc]
                for m in range(2):
                    k.mm(p.v, Vm[:, m, (2 * h + dc) * 128:(2 * h + dc + 1) * 128], pp[m].v, start=(m == 0), stop=(m == 1))
                k.tt(attnT[:, 2 * h + dc, n * 512:(n + 1) * 512], p.v, r_.v, ALU.mult)
        barrier(k)
        k.es = old


def ffn(k, es, x2T, experts, acc, PS, gateT=None, sel=None):
    with ExitStack() as s2:
        old = k.es
        k.es = s2
        G = 4
        wgt = [k.sb([128, 8, 128], BF16) for _ in range(2)]
        wut = [k.sb([128, 8, 128], BF16) for _ in range(2)]
        wdt = [k.sb([128, G, 1024], BF16) for _ in range(2)]
        hT = [k.sb([128, G, TOK], BF16) for _ in range(1)]
        sg = [k.sb([128, 512]) for _ in range(2)]
        first = True
        wi = 0
        gi = 0
        for e, (wg_d, wu_d, wd_d) in enumerate(experts):
            F = wg_d.h.shape[1]
            nch = F // 128
            for g0 in range(0, nch, G):
                gn = min(G, nch - g0)
                wd_ = wdt[gi % 2]; gi += 1
                h_ = hT[0]
                for j in range(gn):
                    k.dma(wd_[:, j, :], wd_d[(g0 + j) * 128:(g0 + j + 1) * 128, :], q='pool')
                for j in range(gn):
                    mt = g0 + j
                    wg_ = wgt[wi % 2]; wu_ = wut[wi % 2]; wi += 1
                    k.dma(wg_.v, V(wg_d, wg_d.h[:, mt * 128:(mt + 1) * 128].rearrange("(kc p) m -> p kc m", p=128)), q='pool')
                    k.dma(wu_.v, V(wu_d, wu_d.h[:, mt * 128:(mt + 1) * 128].rearrange("(kc p) m -> p kc m", p=128)), q='pool')
                    for n in range(4):
                        pg = PS[n % 2]; pu = PS[2 + n % 2]; s_ = sg[n % 2]
                        for kc in range(8):
                            k.mm(pg.v, wg_[:, kc, :], x2T[:, kc, n * 512:(n + 1) * 512], start=(kc == 0), stop=(kc == 7))
                        for kc in range(8):
                            k.mm(pu.v, wu_[:, kc, :], x2T[:, kc, n * 512:(n + 1) * 512], start=(kc == 0), stop=(kc == 7))
                        k.act(s_.v, pg.v, AF.Silu)
                        k.tt(h_[:, j, n * 512:(n + 1) * 512], s_.v, pu.v, ALU.mult)
                for tt in range(NT):
                    for nt in range(2):
                        pd = PS[4 + (tt * 2 + nt) % 2]
                        for j in range(gn):
                            k.mm(pd.v, h_[:, j, tt * 128:(tt + 1) * 128], wd_[:, j, nt * 512:(nt + 1) * 512],
                                 start=(j == 0), stop=(j == gn - 1))
                        a_ = acc[:, tt, nt * 512:(nt + 1) * 512]
                        if gateT is None:
                            if first:
                                k.copy(a_, pd.v, e='act')
                            else:
                                k.tt(a_, a_, pd.v, ALU.add)
                        else:
                            g_ = gateT[:, tt, e:e + 1]
                            if first:
                                k.ts(a_, pd.v, g_, None, op0=ALU.mult)
                            else:
                                k.stt(a_, pd.v, g_, a_, ALU.mult, ALU.add)
                first = False
        barrier(k)
        k.es = old


def final_ln(k, es, acc, res_d, g_d, b_d, out_d, outT_d, ident, PS):
    with ExitStack() as s2:
        old = k.es
        k.es = s2
        g_bc = k.sb([128, 1024]); b_bc = k.sb([128, 1024])
        small = dict(st=k.sb([128, 2, 6]), mv=k.sb([128, 2]), rstd=k.sb([128, 1]), nmr=k.sb([128, 1]), eps=k.sb([128, 1]))
        k.memset(small['eps'].v, LN_EPS)
        xt = [k.sb([128, 1024]) for _ in range(2)]
        tt_ = [k.sb([128, 1024]) for _ in range(2)]
        ot = [k.sb([128, 1024]) for _ in range(2)]
        oT = [k.sb([128, 8, 128], BF16) for _ in range(2)]
        k.dma(g_bc.v, V(g_d, g_d.h[:].partition_broadcast(128)))
        k.dma(b_bc.v, V(b_d, b_d.h[:].partition_broadcast(128)))
        for tt in range(NT):
            x_ = xt[tt % 2]; t_ = tt_[tt % 2]; o_ = ot[tt % 2]
            k.dma(x_.v, res_d[tt * 128:(tt + 1) * 128, :])
            k.stt(t_.v, x_.v, ALPHA, acc[:, tt, :], ALU.mult, ALU.add)
            layer_norm_tile(k, t_, g_bc, b_bc, o_, small)
            k.dma(out_d[tt * 128:(tt + 1) * 128, :], o_.v, q='pool')
            if outT_d is not None:
                oT_ = oT[tt % 2]
                for half in range(2):
                    p = PS[2 + half]
                    for j in range(4):
                        kc = half * 4 + j
                        k.tr(p[:, j * 128:(j + 1) * 128], o_[:, kc * 128:(kc + 1) * 128], ident.v)
                    k.copy(oT_[:, half * 4:half * 4 + 4, :], V(p, p.h[:].rearrange("p (j t) -> p j t", j=4)), e='act')
                if callable(outT_d):
                    outT_d(tt, oT_)
                else:
                    k.dma(V(outT_d, outT_d.h[:, tt * 128:(tt + 1) * 128].rearrange("(kc p) t -> p kc t", p=128)), oT_.v)
        barrier(k)
        k.es = old


def moe_gate_tile(k, lg_ps, gate_tok, tt, tmp):
    lg, mx, nm1, ex, selm, den = tmp['lg'], tmp['mx'], tmp['nm1'], tmp['ex'], tmp['sel'], tmp['den']
    k.copy(lg.v, lg_ps, e='dve')
    k.max8(mx.v, lg.v)
    k.ts(nm1.v, mx[:, 0:1], -1.0, None, op0=ALU.mult)
    k.act(ex.v, lg.v, AF.Exp, bias=nm1.v)
    k.ts(selm.v, lg.v, mx[:, 1:2], None, op0=ALU.is_ge)
    k.tt(ex.v, ex.v, selm.v, ALU.mult)
    k.reduce(den.v, ex.v, ALU.add)
    k.recip(den.v, den.v)
    k.ts(gate_tok[:, tt, :], ex.v, den.v, None, op0=ALU.mult)


def phase_b(k, es, io, moe, PS=None, tag=''):
    ident = k.sb([128, 128])
    ones_bf = k.sb([128, 128], BF16)
    k.dma(ident.v, io['ident'].v)
    k.memset(ones_bf.v, 1.0)
    if PS is None:
        PS = [k.ps([128, 512]) for _ in range(8)]
    x1_d = io.get('x1_dbg') or k.dram('x1_scr' + tag, [TOK, 1024])
    x2_d = io.get('x2_dbg') or k.dram('x2_scr' + tag, [TOK, 1024])
    bufB = k.sb([128, 8, TOK], BF16)
    gateT = None
    outT32 = None
    if moe:
        gateT = k.sb([128, NT, 8])
        wr = k.sb([128, 8, 8])
        k.dma(wr.v, V(io['router'], io['router'].h[:].rearrange("(kc p) e -> p kc e", p=128)))
        sel = k.sb([8, 8 * 128])
        k.dma(sel.v, io['sel8'].v)
        tmp = dict(lg=k.sb([128, 8]), mx=k.sb([128, 8]), nm1=k.sb([128, 1]), ex=k.sb([128, 8]), sel=k.sb([128, 8]),
                   den=k.sb([128, 1]), gt=k.sb([128, 8]))
        x32 = [k.sb([128, 8, 128]) for _ in range(2)]

        def route(tt, xT32):
            p = PS[6]
            for kc in range(8):
                k.mm(p[:, 0:8], xT32[:, kc, :], wr[:, kc, :], start=(kc == 0), stop=(kc == 7))
            moe_gate_tile(k, p[:, 0:8], gateT, tt, tmp)
        outT32 = [x32[0], x32[1], route]
    sA = ExitStack()
    old_es = k.es
    k.es = sA
    bufA = k.sb([128, 8, TOK], BF16)
    if io.get('mix_loader') is not None:
        with k.nc.named_scope('mixload' + tag):
            io['mix_loader'](bufA)
    else:
        for kc in range(8):
            k.dma(bufA[:, kc, :], io['mixT'][kc * 128:(kc + 1) * 128, :])
    with k.nc.named_scope('projln1' + tag):
        proj_ln(k, es, bufA, io['w_out'], io['x'], io['ln1_g'], io['ln1_b'], x1_d, bufB, ident, PS)
    with k.nc.named_scope('xattn' + tag):
        cross_attn(k, es, bufB, io['memT'], io['xq'], io['xk'], io['xv'], bufA, PS, ones_bf)
    with k.nc.named_scope('projln2' + tag):
        proj_ln(k, es, bufA, io['xo'], x1_d, io['ln2_g'], io['ln2_b'], x2_d, bufB, ident, PS, outT32=outT32)
    barrier(k)
    sA.close()
    k.es = old_es
    acc = k.sb([128, NT, 1024])
    if moe:
        experts = [(V(io['moe_wg'], io['moe_wg'].h[e]), V(io['moe_wu'], io['moe_wu'].h[e]), V(io['moe_wd'], io['moe_wd'].h[e])) for e in range(NEXP)]
        experts = [tuple(Tsub(v) for v in ex) for ex in experts]
        ffn(k, es, bufB, experts, acc, PS, gateT=gateT, sel=sel)
    else:
        ffn(k, es, bufB, [(io['ffn_wg'], io['ffn_wu'], io['ffn_wd'])], acc, PS)
    with k.nc.named_scope('finalln' + tag):
        final_ln(k, es, acc, x2_d, io['ln3_g'], io['ln3_b'], io['out'], io.get('outT'), ident, PS)


class Tsub:
    def __init__(self, v):
        self.t = v.t
        self.h = v.ap
        self.name = v.t.name

    def __getitem__(self, idx):
        return V(self.t, self.h[idx])

    @property
    def v(self):
        return V(self.t, self.h)


SEQ = 4096
NBLK = 8
RMS_EPS = 1e-6
PAD = 2

O_AQ, O_AK, O_AV, O_AZ, O_AB, O_AD = 0, 384, 768, 1152, 1536, 1548
O_BQ, O_BK, O_BV = 1560, 1944, 2072
O_CF, O_CI, O_CQ, O_CG = 2200, 2712, 2968, 3224


def mmt(k, out, lhsT, rhs, start=True, stop=True, tp=None):
    if tp is None or tp == (0, 0):
        return k.mm(out, lhsT, rhs, start=start, stop=stop)
    return k.mm(out, lhsT, rhs, start=start, stop=stop, tile_position=tp)


def load_xT(k, xT, src_fn):
    k.memset(xT[:, :, 0:PAD], 0.0)
    k.memset(xT[:, :, PAD + SEQ:PAD + SEQ + PAD], 0.0)
    for kc in range(8):
        for hh in range(2):
            k.dma(xT[:, kc, PAD + hh * 2048:PAD + (hh + 1) * 2048], src_fn(kc, hh), q='pool')


def consts_a(k, io):
    c = {}
    for n in ['ident', 'bd64', 'rt128']:
        c[n] = k.sb([128, 128])
        k.dma(c[n].v, io[n].v)
    c['ones_bf'] = k.sb([128, 128], BF16)
    k.memset(c['ones_bf'].v, 1.0)
    c['eps'] = k.sb([128, 1])
    k.memset(c['eps'].v, RMS_EPS)
    return c


def rope_norm(k, ps, nwcol, cos_, sin_, out, c, W, PSr):
    xs, sq, rn, xn, t1 = W['xs'], W['sq'], W['rn'], W['xn'], W['t1']
    k.copy(xs.v, ps.v, e='act')
    k.tt(sq.v, xs.v, xs.v, ALU.mult)
    k.mm(PSr[0].v, c['bd64'].v, sq.v)
    k.act(rn.v, PSr[0].v, AF.Sqrt, bias=c['eps'].v, scale=1.0 / 64.0)
    k.recip(rn.v, rn.v)
    k.stt(xn.v, xs.v, nwcol, rn.v, ALU.mult, ALU.mult)
    k.mm(PSr[1].v, c['rt128'].v, xn.v)
    k.tt(t1.v, xn.v, cos_.v, ALU.mult)
    k.tt(sq.v, PSr[1].v, sin_.v, ALU.mult)
    k.tt(out, t1.v, sq.v, ALU.add)


def gqa_prepare(k, io, xT, c, PS, mix_units):
    wg = k.sb([128, 8, 384], BF16)
    wbv = k.sb([128, 8, 64], BF16)
    load_w_rows(k, wg, io['wg'].v, 8)
    load_w_rows(k, wbv, io['wbv'].v, 8)
    gnw = k.sb([128, 3])
    k.dma(gnw.v, io['gnw'].v)
    kT = k.sb([128, SEQ], BF16)
    Vsb = k.sb([128, 32, 128], BF16)
    cos_ = [k.sb([128, 512]) for _ in range(2)]
    sin_ = [k.sb([128, 512]) for _ in range(2)]
    W = dict(xs=k.sb([128, 512]), sq=k.sb([128, 512]), rn=k.sb([128, 512]), xn=k.sb([128, 512]), t1=k.sb([128, 512]))
    qT = [k.sb([128, 512], BF16) for _ in range(2)]
    pT = [k.sb([128, 512], BF16) for _ in range(3)]
    pT4 = [k.sb([128, 512], BF16) for _ in range(3)]
    rs = k.sb([128, 512])
    pacc = k.sb([128, 512])
    pacc2 = k.sb([128, 512])
    ones32 = k.sb([128, 64])
    k.memset(ones32.v, 1.0)
    for blk in range(NBLK):
        s = blk * 512
        cs, sn = cos_[blk % 2], sin_[blk % 2]
        k.dma(cs.v, io['cos'][:, s:s + 512])
        k.dma(sn.v, io['sin'][:, s:s + 512])
        p = PS[0]
        for kc in range(8):
            k.mm(p.v, wg[:, kc, 128:256], xT[:, kc, PAD + s:PAD + s + 512], start=(kc == 0), stop=(kc == 7))
        rope_norm(k, p, gnw[:, 1:2], cs, sn, kT[:, s:s + 512], c, W, PS[1:3])
        for t in range(4):
            ti = blk * 4 + t
            pv = PS[3 + t % 2]
            for kc in range(8):
                k.mm(pv[:, 0:64], xT[:, kc, PAD + ti * 128:PAD + (ti + 1) * 128], wbv[:, kc, :], start=(kc == 0), stop=(kc == 7))
            k.copy(Vsb[:, ti, 0:64], pv[:, 0:64], e='act')
            k.copy(Vsb[:, ti, 64:128], pv[:, 0:64], e='dve')

    def attn(PSa):
        for blk in range(NBLK):
            s = blk * 512
            cs, sn = cos_[blk % 2], sin_[blk % 2]
            k.dma(cs.v, io['cos'][:, s:s + 512])
            k.dma(sn.v, io['sin'][:, s:s + 512])
            for qi, (c0, nwc) in enumerate([(0, 0), (256, 2)]):
                p = PSa[0]
                for kc in range(8):
                    k.mm(p.v, wg[:, kc, c0:c0 + 128], xT[:, kc, PAD + s:PAD + s + 512], start=(kc == 0), stop=(kc == 7))
                rope_norm(k, p, gnw[:, nwc:nwc + 1], cs, sn, qT[qi].v, c, W, [PSa[1], PSa[0]])
                yield
            dests = mix_units(blk)
            for h in range(3):
                r = 64 if h == 1 else 0
                qsrc = qT[1] if h == 2 else qT[0]
                po = 64 if h >= 1 else 0
                pv = PSa[2][po:po + 64, :]

                GB = 0
                if GB and len(PSa) >= 7:
                    banks = [PSa[3:3 + GB], PSa[0:2] + PSa[6:7]] if GB == 3 else None
                    banks = [[PSa[0], PSa[1], PSa[3]], [PSa[4], PSa[5], PSa[6]]]
                    ngrp = 32 // GB + (1 if 32 % GB else 0)

                    def sgroup(g):
                        for j in range(GB):
                            kt = g * GB + j
                            if kt < 32:
                                mmt(k, banks[g % 2][j].v, kT[r:r + 64, kt * 128:(kt + 1) * 128], qsrc[r:r + 64, :], tp=(r, 0))
                    pTg = [pT[0], pT[1], pT[2], pT4[0], pT4[1], pT4[2]]
                    sgroup(0)
                    for g in range(ngrp):
                        for j in range(GB):
                            kt = g * GB + j
                            if kt < 32:
                                k.act(pTg[(g % 2) * 3 + j].v, banks[g % 2][j].v, AF.Exp, scale=0.125)
                        if g + 1 < ngrp:
                            sgroup(g + 1)
                        for j in range(GB):
                            kt = g * GB + j
                            if kt < 32:
                                p_ = pTg[(g % 2) * 3 + j]
                                k.mm(PSa[2].v, Vsb[:, kt, :], p_.v, start=(kt == 0), stop=(kt == 31))
                                eng_, acc_ = ('dve', pacc) if kt % 2 == 0 else ('pool', pacc2)
                                if kt < 2:
                                    k.copy(acc_.v, p_.v, e=eng_)
                                else:
                                    k.tt(acc_.v, acc_.v, p_.v, ALU.add, e=eng_)
                        yield
                else:
                    def scores(kt):
                        mmt(k, PSa[kt % 2].v, kT[r:r + 64, kt * 128:(kt + 1) * 128], qsrc[r:r + 64, :], tp=(r, 0))
                    scores(0)
                    for kt in range(32):
                        p_ = pT[kt % 3]
                        k.act(p_.v, PSa[kt % 2].v, AF.Exp, scale=0.125)
                        if kt + 1 < 32:
                            scores(kt + 1)
                        k.mm(PSa[2].v, Vsb[:, kt, :], p_.v, start=(kt == 0), stop=(kt == 31))
                        eng_, acc_ = ('dve', pacc) if kt % 2 == 0 else ('pool', pacc2)
                        if kt < 2:
                            k.copy(acc_.v, p_.v, e=eng_)
                        else:
                            k.tt(acc_.v, acc_.v, p_.v, ALU.add, e=eng_)
                        yield
                sm = PSa[0][po:po + 64, :]
                mmt(k, sm, ones32[:, 0:64], pacc.v, start=True, stop=False, tp=(0, po))
                mmt(k, sm, ones32[:, 0:64], pacc2.v, start=False, stop=True, tp=(0, po))
                k.recip(rs[po:po + 64, :], sm)
                k.tt(dests[h], pv, rs[po:po + 64, :], ALU.mult)
                yield
            mix_units(blk, done=True)
    return attn


def gqa(k, io, xT, c, PS, mix_units):
    with ExitStack() as s2:
        old = k.es
        k.es = s2
        attn = gqa_prepare(k, io, xT, c, PS, mix_units)
        for _ in attn([PS[5], PS[6], PS[3], PS[4], PS[0], PS[1], PS[2]]):
            pass
        barrier(k)
        k.es = old


def hgrn(k, io, xT, c, PS, layer, mix_dest, tick=None):
    if tick is None:
        tick = lambda n: None
    with ExitStack() as s2:
        old = k.es
        k.es = s2
        wfm = k.sb([128, 8, 512], BF16)
        wtm = k.sb([128, 8, 384], BF16)
        load_w_rows(k, wfm, io['wh_fm'].v, 8)
        load_w_rows(k, wtm, io['wh_tm'].v, 8)
        hnw = k.sb([128, 1]); k.dma(hnw.v, io['hnw'].v)
        lb_col = k.sb([128, 1]); oml_col = k.sb([128, 1]); lb_row = k.sb([128, 128]); oml_row = k.sb([128, 128])
        if layer == 0:
            k.memset(lb_col.v, 0.0); k.memset(lb_row.v, 0.0)
        else:
            lc = k.sb([128, 2]); lr = k.sb([128, 2, 128])
            k.dma(lc.v, io['lbl_col'].v)
            k.dma(lr.v, V(io['lbl_row'], io['lbl_row'].h[:].partition_broadcast(128)))
            k.tt(lb_col.v, lc[:, 1:2], lc[:, 0:1], ALU.subtract)
            k.act(lb_col.v, lb_col.v, AF.Sigmoid)
            k.tt(lb_row.v, lr[:, 1, :], lr[:, 0, :], ALU.subtract)
            k.act(lb_row.v, lb_row.v, AF.Sigmoid)
        k.ts(oml_col.v, lb_col.v, -1.0, 1.0, op0=ALU.mult, op1=ALU.add)
        k.ts(oml_row.v, lb_row.v, -1.0, 1.0, op0=ALU.mult, op1=ALU.add)
        ofw_d = k.dram('hg_ofw_l%d' % layer, [128, SEQ])
        fT = k.sb([128, 512]); kTf = k.sb([128, 512]); qs = k.sb([128, 512]); gate = k.sb([128, 512])
        obuf = k.sb([128, 512]); ofw = k.sb([128, 512])
        S = k.sb([128, 64])
        W = {n: [k.sb([128, 128]) for _ in range(2)] for n in ['ftok', 'logf', 'ktok', 'vtok', 'kitok', 'E', 'Einv', 'qd', 'ki', 'at0', 'at1']}
        sc = [k.sb([128, 6]) for _ in range(2)]
        Sm = [k.sb([128, 64]) for _ in range(2)]
        tmpu = [k.sb([128, 64]) for _ in range(2)]
        sq = k.sb([128, 512]); rn = k.sb([128, 512])
        for d in range(2):
            sfx = 'f' if d == 0 else 'b'
            trx = k.sb([128, 132]); trt = k.sb([128, 128]); mki = k.sb([128, 128])
            k.dma(trx.v, io['trx_' + sfx][:, 0:132]); k.dma(trt.v, io['trt_' + sfx].v); k.dma(mki.v, io['mki_' + sfx].v)
            k.memset(S.v, 0.0)
            blks = range(NBLK) if d == 0 else range(NBLK - 1, -1, -1)
            it = 0
            for blk in blks:
                s = blk * 512
                p = PS[2]
                for kc in range(8):
                    k.mm(p.v, wfm[:, kc, d * 128:(d + 1) * 128], xT[:, kc, PAD + s:PAD + s + 512], start=(kc == 0), stop=(kc == 7))
                k.act(fT.v, p.v, AF.Sigmoid)
                k.ts(fT.v, fT.v, oml_col.v, lb_col.v, op0=ALU.mult, op1=ALU.add)
                k.ts(kTf.v, fT.v, -1.0, 1.0, op0=ALU.mult, op1=ALU.add)
                p = PS[3]
                for kc in range(8):
                    k.mm(p.v, wfm[:, kc, 256:384], xT[:, kc, PAD + s:PAD + s + 512], start=(kc == 0), stop=(kc == 7))
                k.act(qs.v, p.v, AF.Silu)
                if d == 1:
                    p = PS[2]
                    for kc in range(8):
                        k.mm(p.v, wfm[:, kc, 384:512], xT[:, kc, PAD + s:PAD + s + 512], start=(kc == 0), stop=(kc == 7))
                    k.act(gate.v, p.v, AF.Sigmoid)
                    k.dma(ofw.v, ofw_d[:, s:s + 512])
                tiles = list(range(4)) if d == 0 else list(range(3, -1, -1))

                def Gst(t, par):
                    w = {n: W[n][par] for n in W}
                    sc_ = sc[par]
                    tc = slice(t * 128, (t + 1) * 128)
                    tok0 = PAD + s + t * 128
                    ptm = PS[2]
                    c0 = 0 if d == 0 else 128
                    for kc in range(8):
                        k.mm(ptm[:, 0:256], xT[:, kc, tok0:tok0 + 128], wtm[:, kc, c0:c0 + 256], start=(kc == 0), stop=(kc == 7))
                    yield
                    cf_ps = ptm[:, 0:128] if d == 0 else ptm[:, 128:256]
                    ci_ps = ptm[:, 128:256] if d == 0 else ptm[:, 0:128]
                    k.act(w['ftok'].v, cf_ps, AF.Exp, scale=-1.0)
                    k.copy(w['vtok'].v, ci_ps, e='act')
                    k.ts(w['ftok'].v, w['ftok'].v, 1.0, None, op0=ALU.add)
                    k.recip(w['ftok'].v, w['ftok'].v)
                    yield
                    k.tt(w['ftok'].v, w['ftok'].v, oml_row.v, ALU.mult)
                    k.tt(w['ftok'].v, w['ftok'].v, lb_row.v, ALU.add)
                    k.act(w['logf'].v, w['ftok'].v, AF.Ln)
                    k.ts(w['ktok'].v, w['ftok'].v, -1.0, 1.0, op0=ALU.mult, op1=ALU.add)
                    tick(2)
                    yield
                    pc1 = PS[3]
                    k.mm(pc1[:, 0:128], trt.v, w['logf'].v)
                    pc2 = V(PS[3], PS[3].h[:, 256:512])
                    k.mm(pc2[:, 0:132], w['logf'].v, trx.v)
                    yield
                    k.act(w['kitok'].v, pc1[:, 0:128], AF.Exp, scale=-1.0)
                    k.tt(w['kitok'].v, w['kitok'].v, w['ktok'].v, ALU.mult)
                    k.act(w['E'].v, pc2[:, 0:128], AF.Exp)
                    k.act(w['Einv'].v, pc2[:, 0:128], AF.Exp, scale=-1.0)
                    k.copy(sc_[:, 0:4], pc2[:, 128:132], e='act')
                    yield
                    k.tt(sc_[:, 4:6], sc_[:, 2:4], sc_[:, 0:2], ALU.subtract)
                    k.act(sc_.v, sc_.v, AF.Exp)
                    k.stt(w['qd'].v, qs[:, tc], 0.125, w['E'].v, ALU.mult, ALU.mult)
                    k.tt(w['ki'].v, kTf[:, tc], w['Einv'].v, ALU.mult)
                    tick(2)
                    yield
                    atm = [w['at0'], w['at1']]
                    for hh in range(2):
                        ph = hh * 64
                        pa_ = PS[5]
                        mmt(k, pa_[:, 0:128], w['ki'][ph:ph + 64, :], w['qd'][ph:ph + 64, :], tp=(ph, 0))
                        k.tt(atm[hh].v, pa_[:, 0:128], mki.v, ALU.mult)
                        yield
                    tick(2)

                def Sst(t, par):
                    w = {n: W[n][par] for n in W}
                    sc_ = sc[par]
                    tc = slice(t * 128, (t + 1) * 128)
                    atm = [w['at0'], w['at1']]
                    pso = PS[6]
                    chunks = (0, 1) if d == 0 else (1, 0)
                    for ci_, cc in enumerate(chunks):
                        pc_ = cc * 64
                        cols = slice(pc_, pc_ + 64)
                        sm_ = Sm[ci_]; tu = tmpu[ci_]
                        k.ts(sm_.v, S.v, sc_[:, cc:cc + 1], None, op0=ALU.mult)
                        yield
                        psu = PS[7]
                        for hh in range(2):
                            ph = hh * 64
                            mmt(k, pso[ph:ph + 64, cols], sm_[ph:ph + 64, :], w['qd'][ph:ph + 64, cols], start=True, stop=False, tp=(ph, ph))
                            mmt(k, pso[ph:ph + 64, cols], w['vtok'][pc_:pc_ + 64, ph:ph + 64], atm[hh][pc_:pc_ + 64, cols], start=False, stop=True, tp=(pc_, ph))
                            mmt(k, psu[ph:ph + 64, 0:64], w['kitok'][pc_:pc_ + 64, ph:ph + 64], w['vtok'][pc_:pc_ + 64, ph:ph + 64], tp=(pc_, ph))
                        yield
                        k.ts(tu.v, psu[:, 0:64], sc_[:, 4 + cc:5 + cc], None, op0=ALU.mult)
                        k.stt(S.v, S.v, sc_[:, 2 + cc:3 + cc], tu.v, ALU.mult, ALU.add)
                        tick(3)
                        yield
                    if d == 0:
                        k.copy(obuf[:, tc], pso[:, 0:128], e='act')
                    else:
                        k.tt(obuf[:, tc], pso[:, 0:128], ofw[:, tc], ALU.add)

                def merged(gens):
                    alive = list(gens)
                    while alive:
                        for g_ in list(alive):
                            try:
                                next(g_)
                            except StopIteration:
                                alive.remove(g_)

                merged([Gst(tiles[0], it % 2)])
                for ti, t in enumerate(tiles):
                    gens = [Sst(t, it % 2)]
                    if ti + 1 < len(tiles):
                        gens.append(Gst(tiles[ti + 1], (it + 1) % 2))
                    merged(gens)
                    it += 1
                if d == 0:
                    k.dma(ofw_d[:, s:s + 512], obuf.v)
                else:
                    if io.get('hg_dbg') is not None:
                        k.dma(io['hg_dbg'][:, s:s + 512], obuf.v)
                    k.tt(sq.v, obuf.v, obuf.v, ALU.mult)
                    pn = PS[2]
                    k.mm(pn.v, c['bd64'].v, sq.v)
                    k.act(rn.v, pn.v, AF.Sqrt, bias=c['eps'].v, scale=1.0 / 64.0)
                    k.recip(rn.v, rn.v)
                    k.stt(sq.v, obuf.v, hnw.v, rn.v, ALU.mult, ALU.mult)
                    k.tt(mix_dest(blk), sq.v, gate.v, ALU.mult)
                    mix_dest(blk, done=True)
        barrier(k)
        k.es = old


def gdn(k, io, xT, c, PS, layer, mix_dest):
    with k.nc.named_scope('gdn%d' % layer):
        return _gdn(k, io, xT, c, PS, layer, mix_dest)


def _gdn(k, io, xT, c, PS, layer, mix_dest):
    with ExitStack() as s2:
        old = k.es
        k.es = s2
        wcs = [k.sb([128, 8, 640], BF16) for _ in range(5)]
        with ExitStack() as s3:
            k.es = s3
            cwb = k.sb([128, 5, 640])
            k.dma(cwb.v, V(io['cw'], io['cw'].h[:].partition_broadcast(128)))
            stg = [k.sb([128, 640]) for _ in range(2)]
            for kc in range(8):
                st = stg[kc % 2]
                k.dma(st.v, io['wc'][kc * 128:(kc + 1) * 128, :])
                for j in range(5):
                    k.tt(wcs[j][:, kc, :], st.v, cwb[:, j, :], ALU.mult)
            barrier(k)
        k.es = s2
        wz = k.sb([128, 8, 256], BF16)
        wgt = k.sb([128, 8, 12], BF16)
        load_w_rows(k, wz, io['wz'].v, 8)
        load_w_rows(k, wgt, io['wgt'].v, 8)
        gpar = k.sb([128, 12])
        k.dma(gpar.v, V(io['gpar'], io['gpar'].h[:].partition_broadcast(128)))
        negA = k.sb([128, 6])
        k.act(negA.v, gpar[:, 0:6], AF.Exp)
        k.ts(negA.v, negA.v, -1.0, None, op0=ALU.mult)
        gnorm = k.sb([128, 1]); k.dma(gnorm.v, io['gnorm'].v)
        sel3 = k.sb([3, 384]); k.dma(sel3.v, io['sel3'].v)
        ofw_d = [k.dram('gd_ofw%d_l%d' % (i, layer), [128, SEQ]) for i in range(2)]
        cstash = [k.dram('gd_cs%d_l%d' % (i, layer), [128, SEQ]) for i in range(5)]
        cs = [k.sb([128, 512]) for _ in range(5)]
        zs = [k.sb([128, 512]) for _ in range(2)]
        sq = k.sb([128, 512]); rn = k.sb([128, 512])
        obuf = [k.sb([128, 512]) for _ in range(2)]
        ofw = [k.sb([128, 512]) for _ in range(2)]
        k.memset(obuf[1].v, 0.0)
        S = [k.sb([128, 64]) for _ in range(3)]
        TOK = k.sb([128, 4, 128])
        gsm = {n: k.sb([128, 6]) for n in ['bd', 'gcl', 'x1']}
        gcol = {n: k.sb([128, 3]) for n in ['beta', 'nbeta', 'g', 'ngc', 'bg', 'kdc', 't']}
        gT = k.sb([3, 256])
        H = [{n: k.sb([128, 128]) for n in ['D', 'DT', 'at', 'tmp', 'tmp2']} for _ in range(3)]
        identbf = k.sb([128, 128], BF16)
        k.copy(identbf.v, c['ident'].v)
        for h in range(3):
            for n in ['T0', 'T1']:
                H[h][n] = k.sb([128, 384], BF16)
            for n in ['vb', 'kbg']:
                H[h][n] = k.sb([128, 64], BF16)
            for n in ['kdec', 'u', 'vnew']:
                H[h][n] = k.sb([128, 64])
            H[h]['wT'] = k.sb([128, 128]); H[h]['qd'] = k.sb([128, 128]); H[h]['egb'] = k.sb([128, 128]); H[h]['ecd0'] = k.sb([128, 2]); H[h]['ecd1'] = k.sb([128, 2])
        RB = [0, 64, 0]
        QT = [cs[0], cs[0], cs[2]]
        KT = [cs[1], cs[1], cs[3]]
        KTOK = [(0, 0), (0, 64), (3, 0)]
        VTOK = [(1, 0), (1, 64), (2, 64)]
        PO = [0, 64, 0]
        for d in range(2):
            sfx = 'f' if d == 0 else 'b'
            tb = k.sb([128, 256]); negs = k.sb([128, 128]); negi = k.sb([128, 128])
            k.dma(tb.v, io['tb_' + sfx].v); k.dma(negs.v, io['negs_' + sfx].v); k.dma(negi.v, io['negi_' + sfx].v)
            for h in range(3):
                k.memset(S[h].v, 0.0)
            blks = range(NBLK) if d == 0 else range(NBLK - 1, -1, -1)
            for blk in blks:
                s = blk * 512
                if d == 0:
                    for ct in range(5):
                        p = PS[ct % 2]
                        n_ = 0
                        for j in range(5):
                            for kc in range(8):
                                k.mm(p.v, wcs[j][:, kc, ct * 128:(ct + 1) * 128], xT[:, kc, s + j:s + j + 512], start=(n_ == 0), stop=(n_ == 39))
                                n_ += 1
                        k.act(cs[ct].v, p.v, AF.Silu)
                    for ct, scl, rows in [(0, 0.125, 128), (1, 1.0, 128), (2, 0.125, 64), (3, 1.0, 128)]:
                        k.tt(sq.v, cs[ct].v, cs[ct].v, ALU.mult)
                        pn = PS[2]
                        k.mm(pn.v, c['bd64'].v, sq.v)
                        k.act(rn.v, pn.v, AF.Sqrt, bias=c['eps'].v)
                        k.recip(rn.v, rn.v)
                        k.stt(cs[ct][0:rows, :], cs[ct][0:rows, :], scl, rn[0:rows, :], ALU.mult, ALU.mult)
                    for ct in range(5):
                        k.dma(cstash[ct][:, s:s + 512], cs[ct].v, q='pool')
                else:
                    for ct in range(5):
                        k.dma(cs[ct].v, cstash[ct][:, s:s + 512])
                if d == 1:
                    for zi in range(2):
                        p = PS[zi]
                        for kc in range(8):
                            k.mm(p.v, wz[:, kc, zi * 128:(zi + 1) * 128], xT[:, kc, PAD + s:PAD + s + 512], start=(kc == 0), stop=(kc == 7))
                        k.act(zs[zi].v, p.v, AF.Silu)
                    for i in range(2):
                        k.dma(ofw[i].v, ofw_d[i][:, s:s + 512])
                tiles = list(range(4)) if d == 0 else list(range(3, -1, -1))

                def Gstage(t, par):
                    tc = slice(t * 128, (t + 1) * 128)
                    tok0 = PAD + s + t * 128
                    ptr = PS[3]
                    for si, src in enumerate([cs[1], cs[4], cs[2], cs[3]]):
                        k.tr(ptr[:, si * 128:(si + 1) * 128], src[:, tc], c['ident'].v)
                    pg = PS[4]
                    for kc in range(8):
                        k.mm(pg[:, 0:6], xT[:, kc, tok0:tok0 + 128], wgt[:, kc, d * 6:(d + 1) * 6], start=(kc == 0), stop=(kc == 7))
                    yield
                    k.copy(gsm['bd'].v, pg[:, 0:6], e='dve')
                    k.copy(TOK.v, V(ptr, ptr.h[:].rearrange("p (a b) -> p a b", a=4)), e='dve')
                    yield
                    k.act(gcol['beta'].v, gsm['bd'][:, 0:3], AF.Exp, scale=-1.0)
                    k.tt(gcol['t'].v, gsm['bd'][:, 3:6], gpar[:, 6 + d * 3:9 + d * 3], ALU.add)
                    k.act(gcol['t'].v, gcol['t'].v, AF.Exp)
                    k.ts(gcol['beta'].v, gcol['beta'].v, 1.0, None, op0=ALU.add)
                    k.recip(gcol['beta'].v, gcol['beta'].v)
                    yield
                    k.act(gcol['t'].v, gcol['t'].v, AF.Ln, bias=1.0)
                    k.tt(gcol['g'].v, gcol['t'].v, negA[:, d * 3:(d + 1) * 3], ALU.mult)
                    yield
                    k.mm(pg[:, 8:11], tb[:, 0:128], gcol['g'].v)
                    k.mm(pg[:, 11:14], tb[:, 128:256], gcol['g'].v)
                    pg2 = PS[5]
                    k.mm(pg2[0:3, 0:256], gcol['g'].v, tb.v)
                    yield
                    k.copy(gsm['gcl'].v, pg[:, 8:14], e='dve')
                    k.copy(gT.v, pg2[0:3, 0:256], e='act')
                    k.ts(gcol['ngc'].v, gsm['gcl'][:, 0:3], -1.0, None, op0=ALU.mult)
                    k.ts(gcol['nbeta'].v, gcol['beta'].v, -1.0, None, op0=ALU.mult)
                    k.act(gcol['bg'].v, gsm['gcl'][:, 0:3], AF.Exp)
                    k.tt(gcol['kdc'].v, gsm['gcl'][:, 3:6], gsm['gcl'][:, 0:3], ALU.subtract)
                    yield
                    k.tt(gcol['bg'].v, gcol['bg'].v, gcol['beta'].v, ALU.mult)
                    k.act(gcol['kdc'].v, gcol['kdc'].v, AF.Exp)
                    for h in range(3):
                        pb = PS[h]
                        k.mm(pb.v[:, 256:512], sel3[:, h * 128:(h + 1) * 128], gT.v)
                    yield
                    for h in range(3):
                        hh = H[h]; rb = RB[h]; pb = PS[h]
                        k.tt(hh['tmp'].v, negs.v, pb[:, 256:384], ALU.subtract)
                        k.tt(hh['tmp2'].v, pb[:, 256:384], negi.v, ALU.add)
                        k.act(hh['egb'][rb:rb + 64, :], pb[rb:rb + 64, 256:384], AF.Exp)
                        k.act(hh['ecd%d' % par][rb:rb + 64, 0:1], pb[rb:rb + 64, 384:385], AF.Exp)
                        k.act(hh['ecd%d' % par][rb:rb + 64, 1:2], pb[rb:rb + 64, 448:449], AF.Exp)
                        yield
                    for h in range(3):
                        hh = H[h]
                        k.act(hh['D'].v, hh['tmp'].v, AF.Exp, bias=gsm['gcl'][:, h:h + 1])
                        k.act(hh['DT'].v, hh['tmp2'].v, AF.Exp, bias=gcol['ngc'][:, h:h + 1])
                    yield

                def Mstage(t):
                    tc = slice(t * 128, (t + 1) * 128)
                    for h in range(3):
                        hh = H[h]; rb = RB[h]; ph_ = PS[h]
                        kT_ = KT[h][rb:rb + 64, tc]; qT_ = QT[h][rb:rb + 64, tc]
                        mmt(k, ph_[:, 0:128], kT_, kT_, tp=(rb, 0))
                        mmt(k, ph_[:, 128:256], kT_, qT_, tp=(rb, 0))
                        k.stt(hh['T0'][:, 0:128], ph_[:, 0:128], gcol['nbeta'][:, h:h + 1], hh['D'].v, ALU.mult, ALU.mult)
                        k.tt(hh['at'].v, ph_[:, 128:256], hh['DT'].v, ALU.mult)
                        k.tt(hh['qd'][rb:rb + 64, :], qT_, hh['egb'][rb:rb + 64, :], ALU.mult)
                        ks, ko = KTOK[h]; vs, vo = VTOK[h]
                        k.ts(hh['vb'].v, TOK[:, vs, vo:vo + 64], gcol['beta'][:, h:h + 1], None, op0=ALU.mult)
                        k.ts(hh['kbg'].v, TOK[:, ks, ko:ko + 64], gcol['bg'][:, h:h + 1], None, op0=ALU.mult)
                        k.ts(hh['kdec'].v, TOK[:, ks, ko:ko + 64], gcol['kdc'][:, h:h + 1], None, op0=ALU.mult)
                    for h in range(3):
                        hh = H[h]; ph_ = PS[h]
                        k.mm(ph_[:, 128:256], hh['T0'][:, 0:128], identbf.v)
                        k.copy(hh['T0'][:, 128:256], ph_[:, 128:256], e='act')
                    Tc, Tn = 'T0', 'T1'
                    for lvl in range(6):
                        for h in range(3):
                            hh = H[h]; ph_ = PS[h]
                            if lvl == 0:
                                k.mm(ph_[:, 128:256], hh[Tc][:, 0:128], hh[Tc][:, 128:256])
                            elif lvl <= 3:
                                k.mm(ph_[:, 128:384], hh[Tc][:, 0:128], hh[Tc][:, 128:384])
                            else:
                                k.mm(ph_[:, 256:384], hh[Tc][:, 0:128], hh[Tc][:, 256:384])
                            if lvl <= 4:
                                k.mm(ph_[:, 0:128], hh[Tc][:, 128:256], hh[Tc][:, 0:128])
                        for h in range(3):
                            hh = H[h]; ph_ = PS[h]
                            if lvl <= 3:
                                k.copy(hh[Tn][:, 0:256], ph_[:, 0:256], e='act')
                            elif lvl == 4:
                                k.copy(hh[Tn][:, 0:128], ph_[:, 0:128], e='act')
                            if lvl == 0:
                                k.tt(hh[Tn][:, 256:384], hh[Tc][:, 128:256], c['ident'].v, ALU.add)
                            else:
                                k.tt(hh[Tn][:, 256:384], hh[Tc][:, 256:384], ph_[:, 256:384], ALU.add)
                        Tc, Tn = Tn, Tc
                    for h in range(3):
                        hh = H[h]; rb = RB[h]; ph_ = PS[h]
                        X_ = hh[Tc][:, 256:384]
                        k.mm(ph_[:, 384:448], X_, hh['vb'].v)
                        mmt(k, ph_[rb:rb + 64, 0:128], hh['kbg'].v, X_, tp=(0, rb))
                        k.copy(hh['u'].v, ph_[:, 384:448], e='act')
                        k.copy(hh['wT'][rb:rb + 64, :], ph_[rb:rb + 64, 0:128], e='dve')

                def Sstage(t, par):
                    tc = slice(t * 128, (t + 1) * 128)
                    pso = PS[7]; psx = PS[6]
                    chunks = (0, 1) if d == 0 else (1, 0)
                    for cc in chunks:
                        pc_ = cc * 64
                        cols = slice(pc_, pc_ + 64)
                        for h in (0, 2, 1):
                            hh = H[h]; rb = RB[h]
                            mmt(k, psx[pc_:pc_ + 64, h * 64:(h + 1) * 64], hh['wT'][rb:rb + 64, cols], S[h][rb:rb + 64, :], tp=(rb, pc_))
                        yield
                        for h in (0, 2, 1):
                            hh = H[h]
                            k.tt(hh['vnew'][pc_:pc_ + 64, :], hh['u'][pc_:pc_ + 64, :], psx[pc_:pc_ + 64, h * 64:(h + 1) * 64], ALU.subtract)
                        yield
                        for h in (0, 2, 1):
                            hh = H[h]; rb = RB[h]; po = PO[h]
                            oc_ = slice((128 if h == 2 else 0) + pc_, (128 if h == 2 else 0) + pc_ + 64)
                            mmt(k, pso[po:po + 64, oc_], S[h][rb:rb + 64, :], hh['qd'][rb:rb + 64, cols], start=True, stop=False, tp=(rb, po))
                            mmt(k, pso[po:po + 64, oc_], hh['vnew'][pc_:pc_ + 64, :], hh['at'][pc_:pc_ + 64, cols], start=False, stop=True, tp=(pc_, po))
                            mmt(k, psx[rb:rb + 64, 256 + h * 64:256 + (h + 1) * 64], hh['kdec'][pc_:pc_ + 64, :], hh['vnew'][pc_:pc_ + 64, :], tp=(pc_, rb))
                        yield
                        for h in (0, 2, 1):
                            hh = H[h]; rb = RB[h]
                            k.stt(S[h][rb:rb + 64, :], S[h][rb:rb + 64, :], hh['ecd%d' % par][rb:rb + 64, cc:cc + 1],
                                  psx[rb:rb + 64, 256 + h * 64:256 + (h + 1) * 64], ALU.mult, ALU.add)
                        yield
                    if d == 0:
                        k.copy(obuf[0][:, tc], pso[:, 0:128], e='act')
                        k.copy(obuf[1][0:64, tc], pso[0:64, 128:256], e='act')
                    else:
                        k.tt(obuf[0][:, tc], pso[:, 0:128], ofw[0][:, tc], ALU.add)
                        k.tt(obuf[1][0:64, tc], pso[0:64, 128:256], ofw[1][0:64, tc], ALU.add)

                def merged(gens):
                    alive = list(gens)
                    while alive:
                        for g_ in list(alive):
                            try:
                                next(g_)
                            except StopIteration:
                                alive.remove(g_)

                merged([Gstage(tiles[0], 0)])
                for ti, t in enumerate(tiles):
                    Mstage(t)
                    gens = [Sstage(t, ti % 2)]
                    if ti + 1 < len(tiles):
                        gens.append(Gstage(tiles[ti + 1], (ti + 1) % 2))
                    merged(gens)
                if d == 0:
                    for i in range(2):
                        k.dma(ofw_d[i][:, s:s + 512], obuf[i].v, q='pool')
                else:
                    if io.get('gd_dbg') is not None:
                        for i in range(2):
                            k.dma(io['gd_dbg'][i * 128:(i + 1) * 128, s:s + 512], obuf[i].v)
                    dst = mix_dest(blk)
                    for i in range(2):
                        rows = 128 if i == 0 else 64
                        k.tt(sq.v, obuf[i].v, obuf[i].v, ALU.mult)
                        pn = PS[2]
                        k.mm(pn.v, c['bd64'].v, sq.v)
                        k.act(rn.v, pn.v, AF.Sqrt, bias=c['eps'].v, scale=1.0 / 64.0)
                        k.recip(rn.v, rn.v)
                        k.stt(sq[0:rows, :], obuf[i][0:rows, :], gnorm[0:rows, :], rn[0:rows, :], ALU.mult, ALU.mult)
                        k.tt(dst[i], sq[0:rows, :], zs[i][0:rows, :], ALU.mult)
                    mix_dest(blk, done=True)
        barrier(k)
        k.es = old


O_AQ, O_AK, O_AV, O_AZ, O_AB, O_AD = 0, 384, 768, 1152, 1536, 1548
O_BQ, O_BK, O_BV = 1560, 1944, 2072
O_CF, O_CI, O_CQ, O_CG = 2200, 2712, 2968, 3224

def host_consts():
    c = {}
    c['ident'] = np.eye(128, dtype=np.float32)
    bd = np.zeros((128,128), np.float32); bd[:64,:64]=1; bd[64:,64:]=1
    c['bd64'] = bd
    rt = np.zeros((128,128), np.float32)
    for b in range(4):
        for i in range(16):
            rt[b*32+i+16, b*32+i] = -1.0
            rt[b*32+i, b*32+i+16] = 1.0
    c['rt128'] = rt
    s = np.arange(4096)
    pos = np.stack([(s//64).astype(np.float32), (s%64).astype(np.float32)], 0)
    inv = (np.float32(10000.0) ** (-np.arange(0, 32, 2, dtype=np.float32) / np.float32(32))).astype(np.float32)
    d = np.arange(64)
    ang = (pos[d//32][:, :] * inv[d%16][:, None]).astype(np.float32)
    c['cos'] = np.ascontiguousarray(np.concatenate([np.cos(ang), np.cos(ang)], 0).astype(np.float32))
    c['sin'] = np.ascontiguousarray(np.concatenate([np.sin(ang), np.sin(ang)], 0).astype(np.float32))
    return c

def gqa_w(w_in_l, qw, kw, hf):
    z64 = np.zeros((1024,64), np.float32)
    bq = lambda h: w_in_l[:, O_BQ+h*64:O_BQ+(h+1)*64]
    bk = w_in_l[:, O_BK+hf*64:O_BK+(hf+1)*64]
    wg = np.concatenate([bq(3*hf), bq(3*hf+1), bk, bk, bq(3*hf+2), z64], 1)
    wbv = w_in_l[:, O_BV+hf*64:O_BV+(hf+1)*64]
    gnw = np.stack([np.concatenate([qw,qw]), np.concatenate([kw,kw]), np.concatenate([qw, np.zeros(64,np.float32)])], 1)
    return dict(wg=np.ascontiguousarray(wg), wbv=np.ascontiguousarray(wbv), gnw=np.ascontiguousarray(gnw.astype(np.float32)))

def scan_consts():
    c = {}
    idx = np.arange(128); ch = idx // 64
    same = ch[:, None] == ch[None, :]
    for sfx in ('f', 'b'):
        if sfx == 'f':
            tri = same & (idx[None, :] <= idx[:, None])
            mid = ch * 64 + 31
        else:
            tri = same & (idx[None, :] >= idx[:, None])
            mid = ch * 64 + 32
        tri = tri.astype(np.float32)
        trirel = tri - tri[mid, :]
        cext = np.zeros((128, 4), np.float32)
        for cc in range(2):
            cext[:, cc] = tri[cc * 64 + (31 if sfx == 'f' else 32), :]
            cext[:, 2 + cc] = (ch == cc).astype(np.float32)
        c['trt_' + sfx] = np.ascontiguousarray(trirel.T)
        c['trx_' + sfx] = np.ascontiguousarray(np.concatenate([trirel.T, cext, np.zeros((128, 124), np.float32)], 1))
        c['mki_' + sfx] = np.ascontiguousarray(tri.T)
        c['tria_' + sfx] = np.ascontiguousarray(tri.T)
    return c

def hgrn_w(w_in_l, lbl, hnw, hf):
    hs = [2*hf, 2*hf+1]
    def cols(o): return np.concatenate([w_in_l[:, o+h*64:o+(h+1)*64] for h in hs], 1)
    wfm = np.concatenate([cols(O_CF), cols(O_CF+256), cols(O_CQ), cols(O_CG)], 1)
    wtm = np.concatenate([cols(O_CF), cols(O_CI), cols(O_CF+256)], 1)
    ch = np.concatenate([np.arange(h*64,(h+1)*64) for h in hs])
    return dict(wh_fm=np.ascontiguousarray(wfm), wh_tm=np.ascontiguousarray(wtm),
                lbl_col=np.ascontiguousarray(lbl[:, ch].T.astype(np.float32)), lbl_row=np.ascontiguousarray(lbl[:, ch].astype(np.float32)),
                hnw=np.ascontiguousarray(np.concatenate([hnw,hnw])[:,None].astype(np.float32)))

def gdn_consts():
    c = {}
    idx = np.arange(128); ch = idx // 64
    same = ch[:, None] == ch[None, :]
    bd = same.astype(np.float32)
    for sfx in ('f', 'b'):
        if sfx == 'f':
            incl = same & (idx[None, :] <= idx[:, None])
            strict = same & (idx[None, :] < idx[:, None])
        else:
            incl = same & (idx[None, :] >= idx[:, None])
            strict = same & (idx[None, :] > idx[:, None])
        c['tb_' + sfx] = np.ascontiguousarray(np.concatenate([incl.T.astype(np.float32), bd], 1))
        c['negs_' + sfx] = np.where(strict, 0.0, -30000.0).astype(np.float32)
        c['negi_' + sfx] = np.where(incl.T, 0.0, -30000.0).astype(np.float32)
    c['sel3'] = np.repeat(np.eye(3, dtype=np.float32), 128, axis=1)
    return c

def gdn_w(w_in_l, conv_w_l, a_log_l, dt_bias_l, gnw, hf):
    hs = [3*hf, 3*hf+1, 3*hf+2]
    q = lambda h: O_AQ + h*64; kk = lambda h: O_AK + h*64; v = lambda h: O_AV + h*64
    chans = [q(hs[0]), q(hs[1]), kk(hs[0]), kk(hs[1]), q(hs[2]), v(hs[2]), kk(hs[2]), None, v(hs[0]), v(hs[1])]
    wc = np.zeros((1024, 640), np.float32); cw = np.zeros((5, 640), np.float32)
    for u, o in enumerate(chans):
        if o is None: continue
        wc[:, u*64:(u+1)*64] = w_in_l[:, o:o+64]
        cw[:, u*64:(u+1)*64] = conv_w_l[:, o:o+64]
    wz = np.zeros((1024, 256), np.float32)
    for u, h in enumerate(hs):
        wz[:, u*64:(u+1)*64] = w_in_l[:, O_AZ + h*64:O_AZ + (h+1)*64]
    cols = [O_AB + 0*6 + h for h in hs] + [O_AD + 0*6 + h for h in hs] + [O_AB + 6 + h for h in hs] + [O_AD + 6 + h for h in hs]
    wgt = np.ascontiguousarray(w_in_l[:, cols])
    gpar = np.concatenate([a_log_l[0, hs], a_log_l[1, hs], dt_bias_l[0, hs], dt_bias_l[1, hs]]).astype(np.float32)
    return dict(wc=wc, cw=cw, wz=wz, wgt=wgt, gpar=gpar, gnorm=np.concatenate([gnw, gnw])[:, None].astype(np.float32))


A_INPUTS = {
    'wc': [1024, 640], 'cw': [5, 640], 'wz': [1024, 256], 'wgt': [1024, 12], 'gpar': [12], 'gnorm': [128, 1],
    'wh_fm': [1024, 512], 'wh_tm': [1024, 384], 'hnw': [128, 1], 'wg': [1024, 384], 'wbv': [1024, 64], 'gnw': [128, 3],
}
A_CONSTS = {
    'ident': [128, 128], 'bd64': [128, 128], 'rt128': [128, 128], 'cos': [128, SEQ], 'sin': [128, SEQ], 'sel3': [3, 384],
    'tb_f': [128, 256], 'negs_f': [128, 128], 'negi_f': [128, 128], 'tb_b': [128, 256], 'negs_b': [128, 128], 'negi_b': [128, 128],
    'trx_f': [128, 256], 'trt_f': [128, 128], 'mki_f': [128, 128], 'trx_b': [128, 256], 'trt_b': [128, 128], 'mki_b': [128, 128],
    'sel8': [8, 1024], 'lbl_col': [128, 2], 'lbl_row': [2, 128], 'selw': [128, 2], 'memT': [1024, 256],
}
B_INPUTS = {'w_out': [1024, 1024], 'xq': [1024, 1024], 'xk': [1024, 1024], 'xv': [1024, 1024], 'xo': [1024, 1024],
            'ln1_g': [1024], 'ln1_b': [1024], 'ln2_g': [1024], 'ln2_b': [1024], 'ln3_g': [1024], 'ln3_b': [1024]}
B_DENSE = {'ffn_wg': [1024, 2816], 'ffn_wu': [1024, 2816], 'ffn_wd': [2816, 1024]}
B_MOE = {'router': [1024, 8], 'moe_wg': [8, 1024, 3584], 'moe_wu': [8, 1024, 3584], 'moe_wd': [8, 3584, 1024]}
DEPTH = 2


def phase_a(k, io, layer, xsrc, out_view, PS):
    with ExitStack() as sa:
        old = k.es
        k.es = sa
        c = consts_a(k, io)
        xT = k.sb([128, 8, SEQ + 2 * PAD], BF16)
        load_xT(k, xT, xsrc)
        mt = [k.sb([128, 2, 512], BF16) for _ in range(2)]
        for m_ in mt:
            k.memset(m_.v, 0.0)

        def gd_dest(blk, done=False):
            m_ = mt[blk % 2]
            if not done:
                return (m_[:, 0, :], m_[0:64, 1, :])
            k.dma(out_view(0, 128, blk * 512, (blk + 1) * 512), m_[:, 0, :], q='pool')
            k.dma(out_view(128, 192, blk * 512, (blk + 1) * 512), m_[0:64, 1, :], q='pool')

        def hg_dest(blk, done=False):
            m_ = mt[blk % 2]
            if not done:
                return m_[:, 0, :]
            k.dma(out_view(256, 384, blk * 512, (blk + 1) * 512), m_[:, 0, :])

        mtq = [k.sb([128, 2, 512], BF16) for _ in range(2)]

        def gq_dest(blk, done=False):
            m_ = mtq[blk % 2]
            if not done:
                return [m_[0:64, 0, :], m_[64:128, 0, :], m_[64:128, 1, :]]
            k.dma(out_view(384, 512, blk * 512, (blk + 1) * 512), m_[:, 0, :])
            k.dma(out_view(192, 256, blk * 512, (blk + 1) * 512), m_[64:128, 1, :])

        gdn(k, io, xT, c, PS, layer, gd_dest)
        with ExitStack() as sg:
            old2 = k.es
            k.es = sg
            attn = gqa_prepare(k, io, xT, c, PS, gq_dest)
            gen = attn([PS[0], PS[1], PS[4]])

            def tick(n):
                for _ in range(n):
                    next(gen, None)
            hgrn(k, io, xT, c, PS, layer, hg_dest, tick=tick)
            for _ in gen:
                pass
            barrier(k)
            k.es = old2
        barrier(k)
        k.es = old


def build_fused():
    nc = bass.Bass("TRN2", target_bir_lowering=False)
    names = []
    with ExitStack() as es:
        k = K(nc, es)

        def inp(n, shape, dt=F32):
            names.append(n)
            return k.dram(n, shape, dt, kind='ExternalInput')
        cst = {n: inp(n, s) for n, s in A_CONSTS.items()}
        xT0 = inp('xT0', [1024, SEQ])
        x0 = inp('x0', [TOK, 1024])
        out_final = k.dram('out', [TOK, 1024], F32, kind='ExternalOutput')
        PS = [k.ps([128, 512]) for _ in range(8)]
        x_res = x0
        xg = None
        for l in range(DEPTH):
            moe = (l % 2 == 1)
            last = (l == DEPTH - 1)
            ioa = dict(cst)
            for n, s in A_INPUTS.items():
                ioa[n] = inp('%s_l%d' % (n, l), s)
            if l == 0:
                xsrc = lambda kc, hh: xT0[kc * 128:(kc + 1) * 128, hh * 2048:(hh + 1) * 2048]
            else:
                xsrc = (lambda g: (lambda kc, hh: g[kc // 4][hh * 512 + (kc % 4) * 128:hh * 512 + (kc % 4 + 1) * 128, :]))(xg)
            mixh = [k.dram('mixh%d_l%d' % (j, l), [256, SEQ], BF16) for j in range(2)]
            mixf = [k.dram('mixf%d_l%d' % (j, l), [512, SEQ], BF16) for j in range(2)]
            with nc.named_scope('A%d' % l):
                phase_a(k, ioa, l, xsrc, lambda r0, r1, c0, c1: mixh[r0 // 256][r0 % 256:r0 % 256 + (r1 - r0), c0:c1], PS)
            with nc.named_scope('cc_mix%d' % l):
                for j in range(2):
                    k.allgather_pairs(mixf[j], mixh[j])
            iob = {'ident': cst['ident'], 'memT': cst['memT'], 'sel8': cst['sel8'], 'x': x_res}
            spec = dict(B_INPUTS)
            spec.update(B_MOE if moe else B_DENSE)
            for n, s in spec.items():
                iob[n] = inp('%s_l%d' % (n, l), s)
            with ExitStack() as sb_:
                old = k.es
                k.es = sb_

                def mix_loader(bufA, mixf=mixf):
                    with ExitStack() as sl:
                        o2 = k.es
                        k.es = sl
                        bufA2 = k.sb([128, 8, TOK], BF16)
                        selw = k.sb([128, 2])
                        k.dma(selw.v, cst['selw'].v)
                        for th, buf in enumerate([bufA, bufA2]):
                            for kc in range(8):
                                hf, i = kc // 4, kc % 4
                                r0 = hf * 256 + (i % 2) * 128
                                k.dma(buf[:, kc, :], mixf[i // 2][r0:r0 + 128, th * TOK:(th + 1) * TOK])
                        for kc in range(8):
                            k.ts(bufA2[:, kc, :], bufA2[:, kc, :], selw[:, 1:2], None, op0=ALU.mult)
                            k.stt(bufA[:, kc, :], bufA[:, kc, :], selw[:, 0:1], bufA2[:, kc, :], ALU.mult, ALU.add)
                        barrier(k)
                        k.es = o2
                iob['mix_loader'] = mix_loader
                if last:
                    iob['out'] = out_final
                else:
                    x_next = k.dram('x3_l%d' % l, [TOK, 1024])
                    x3T = [k.dram('x3T%d_l%d' % (j, l), [512, TOK], BF16) for j in range(2)]
                    iob['out'] = x_next

                    def outT_writer(tt, oT_, x3T=x3T):
                        for j in range(2):
                            k.dma(V(x3T[j], x3T[j].h[:, tt * 128:(tt + 1) * 128].rearrange("(kc p) t -> p kc t", p=128)),
                                  oT_[:, j * 4:(j + 1) * 4, :], q='pool')
                    iob['outT'] = outT_writer
                with nc.named_scope('B%d' % l):
                    phase_b(k, sb_, iob, moe, PS=PS, tag='_l%d' % l)
                barrier(k)
                k.es = old
            if not last:
                xg = [k.dram('xg%d_l%d' % (j, l), [1024, TOK], BF16) for j in range(2)]
                for j in range(2):
                    k.allgather_pairs(xg[j], x3T[j])
                x_res = x_next
        k.finish()
    return nc, names


def mix_perm():
    oa = lambda h: list(range(h * 64, (h + 1) * 64))
    ob = lambda h: list(range(384 + h * 64, 384 + (h + 1) * 64))
    oc = lambda h: list(range(768 + h * 64, 768 + (h + 1) * 64))
    p = []
    for hf in range(2):
        p += oa(3 * hf) + oa(3 * hf + 1) + oa(3 * hf + 2) + ob(3 * hf + 2) + oc(2 * hf) + oc(2 * hf + 1) + ob(3 * hf) + ob(3 * hf + 1)
    return np.array(p)


def kernel(**inp):
    inp = {n: np.asarray(v) for n, v in inp.items()}
    x = np.ascontiguousarray(inp['x'], dtype=np.float32)
    B = x.shape[0]
    C = host_consts()
    C.update(scan_consts())
    C.update(gdn_consts())
    C['sel8'] = np.repeat(np.eye(8, dtype=np.float32), 128, axis=1)
    perm = mix_perm()
    nc, names = build_fused()
    cores = list(range(8))
    shared = {}
    for l in range(DEPTH):
        for n in ['xq', 'xk', 'xv', 'xo', 'ln1_g', 'ln1_b', 'ln2_g', 'ln2_b', 'ln3_g', 'ln3_b']:
            shared['%s_l%d' % (n, l)] = inp[n][l]
        shared['w_out_l%d' % l] = inp['w_out'][l][perm]
        if l % 2 == 1:
            shared['router_l%d' % l] = inp['moe_router'][l // 2]
            for n in ['moe_wg', 'moe_wu', 'moe_wd']:
                shared['%s_l%d' % (n, l)] = inp[n][l // 2]
        else:
            for n in ['ffn_wg', 'ffn_wu', 'ffn_wd']:
                shared['%s_l%d' % (n, l)] = inp[n][l // 2]
    shared = {n: np.ascontiguousarray(v, dtype=np.float32) for n, v in shared.items()}
    per_hf = []
    for hf in range(2):
        m = {}
        for l in range(DEPTH):
            w = {}
            w.update(gdn_w(inp['w_in'][l], inp['conv_w'][l], inp['gdn_a_log'][l], inp['gdn_dt_bias'][l], inp['gdn_norm_w'][l], hf))
            hw = hgrn_w(inp['w_in'][l], inp['hgrn_lb_logits'], inp['hgrn_norm_w'][l], hf)
            m['lbl_col'] = hw.pop('lbl_col'); m['lbl_row'] = hw.pop('lbl_row')
            w.update(hw)
            w.update(gqa_w(inp['w_in'][l], inp['q_norm_w'][l], inp['k_norm_w'][l], hf))
            for n, v in w.items():
                m['%s_l%d' % (n, l)] = np.ascontiguousarray(v, dtype=np.float32)
        per_hf.append(m)
    maps = []
    for core in cores:
        b, r = core // 2, core % 2
        m = dict(C)
        m.update(shared)
        m.update(per_hf[r])
        m['xT0'] = np.ascontiguousarray(x[b].T)
        m['x0'] = np.ascontiguousarray(x[b, r * TOK:(r + 1) * TOK])
        m['memT'] = np.ascontiguousarray(inp['mem'][b].T.astype(np.float32))
        sw = np.zeros((128, 2), np.float32); sw[:, r] = 1.0
        m['selw'] = sw
        maps.append({n: m[n] for n in names})
    res = run_bass_kernel_spmd(nc, maps, core_ids=cores)
    out = np.empty((B, SEQ, 1024), np.float32)
    for core in cores:
        b, r = core // 2, core % 2
        out[b, r * TOK:(r + 1) * TOK] = res.results[core]['out']
    return out
```

```python
import os
import numpy as np
from contextlib import ExitStack
import concourse.bass as bass
import concourse.mybir as mybir
from concourse.bass_utils import run_bass_kernel_spmd

F32 = mybir.dt.float32
BF16 = mybir.dt.bfloat16
AF = mybir.ActivationFunctionType
ALU = mybir.AluOpType
AX = mybir.AxisListType


class T:
    def __init__(self, h, name):
        self.h = h
        self.name = name
        self.lw = None
        self.rd = {}
        self.psum = False

    def __getitem__(self, idx):
        return V(self, self.h[idx])

    @property
    def v(self):
        return V(self, self.h[:])


class V:
    def __init__(self, t, ap):
        self.t = getattr(t, 't', t)
        self.ap = ap

    def __getitem__(self, idx):
        return V(self.t, self.ap[idx])


class K:
    NRING = 6

    def __init__(self, nc, es):
        self.nc = nc
        self.es = es
        self.es0 = es
        self.eng = {'pe': nc.tensor, 'dve': nc.vector, 'act': nc.scalar,
                    'pool': nc.gpsimd, 'sp': nc.sync}
        self.sem = {e: es.enter_context(nc.semaphore('s_' + e)) for e in self.eng}
        self.cnt = {e: 0 for e in self.eng}
        self.known = {e: {} for e in self.eng}
        self.ring = {}
        for q in ('sp', 'pool', 'act'):
            self.ring[q] = [[es.enter_context(nc.semaphore('d_%s%d' % (q, i))), 0]
                            for i in range(self.NRING)]
        self.ring_i = {q: 0 for q in self.ring}
        self.nalloc = 0
        self.ninstr = 0

    def sb(self, shape, dt=F32, name=None):
        self.nalloc += 1
        name = name or 'sb%d' % self.nalloc
        h = self.es.enter_context(self.nc.sbuf_tensor(name, list(shape), dt))
        return T(h, name)

    def ps(self, shape, dt=F32, name=None):
        self.nalloc += 1
        name = name or 'ps%d' % self.nalloc
        h = self.es.enter_context(self.nc.psum_tensor(name, list(shape), dt))
        t = T(h, name)
        t.psum = True
        return t

    def dram(self, name, shape, dt=F32, kind=None):
        if kind is None:
            h = self.nc.dram_tensor(name, list(shape), dt)
        else:
            h = self.nc.dram_tensor(name, list(shape), dt, kind=kind)
        return T(h, name)

    def _wait(self, e, ev):
        if ev is None:
            return
        sem, val = ev[0], ev[1]
        kn = self.known[e]
        if kn.get(sem.name, 0) >= val:
            return
        self.eng[e].wait_ge(sem, val)
        kn[sem.name] = val
        self.ninstr += 1
        snap = ev[-1] if isinstance(ev[-1], dict) else None
        if snap:
            for n_, v_ in snap.items():
                if kn.get(n_, 0) < v_:
                    kn[n_] = v_

    def _pre(self, e, reads, writes):
        for v in reads:
            t = v.t
            if t.lw is not None:
                if not (e == 'pe' and t.lw[2] == 'pe'):
                    self._wait(e, t.lw)
            if t.psum:
                for src, ev in t.rd.items():
                    if src != e:
                        self._wait(e, ev)
        for v in writes:
            t = v.t
            if t.lw is not None and not (e == 'pe' and t.lw[2] == 'pe'):
                self._wait(e, t.lw)
            for src, ev in t.rd.items():
                if not (e == 'pe' and src == 'pe'):
                    self._wait(e, ev)

    def _post(self, src, ev3, reads, writes):
        for v in writes:
            v.t.lw = ev3
            v.t.rd = {}
        for v in reads:
            if v.t.lw is ev3:
                continue
            v.t.rd[src] = (ev3[0], ev3[1], ev3[3])

    def emit(self, e, fn, reads, writes):
        reads = [r for r in reads if isinstance(r, V)]
        writes = [w for w in writes if isinstance(w, V)]
        self._pre(e, reads, writes)
        ins = fn(self.eng[e])
        self.cnt[e] += 1
        ins.then_inc(self.sem[e], 1)
        self.ninstr += 1
        snap = dict(self.known[e])
        snap[self.sem[e].name] = self.cnt[e]
        ev = (self.sem[e], self.cnt[e], e, snap)
        self._post(e, ev, reads, writes)
        return ins

    def dma(self, out, in_, q='sp', **kw):
        ring = self.ring[q]
        i = self.ring_i[q]
        self.ring_i[q] = (i + 1) % len(ring)
        slot = ring[i]
        sem, uses = slot
        if uses > 0:
            self._wait(q, (sem, 16 * uses))
        self._pre(q, [in_], [out])
        ins = self.eng[q].dma_start(out=out.ap, in_=in_.ap, **kw)
        ins.then_inc(sem, 16)
        slot[1] = uses + 1
        self.ninstr += 1
        src = 'dma_' + sem.name
        ev = (sem, 16 * (uses + 1), src, dict(self.known[q]))
        self._post(src, ev, [in_], [out])
        return ins

    def allgather_pairs(self, out_t, in_t):
        self.ncc = getattr(self, 'ncc', 0) + 1
        sem = self.es0.enter_context(self.nc.semaphore('cc%d' % self.ncc))
        self._pre('pool', [in_t.v], [out_t.v])
        ins = self.eng['pool'].collective_compute(
            "AllGather", ALU.bypass, replica_groups=[[0, 1], [2, 3], [4, 5], [6, 7]],
            ins=[in_t.h.ap().opt()], outs=[out_t.h.ap().opt()])
        ins.then_inc(sem, 1)
        self.ninstr += 1
        ev = (sem, 1, 'cc%d' % self.ncc, dict(self.known['pool']))
        self._post(ev[2], ev, [in_t.v], [out_t.v])
        self.ccsems = getattr(self, 'ccsems', []) + [sem]

    def finish(self, e='sp'):
        for q, ring in self.ring.items():
            for sem, uses in ring:
                if uses > 0:
                    self._wait(e, (sem, 16 * uses))

    @staticmethod
    def _a(x):
        return x.ap if isinstance(x, V) else x

    def mm(self, out, lhsT, rhs, start=True, stop=True, **kw):
        tp = kw.get('tile_position')
        rows = (tp[0] if tp else 0, lhsT.ap.shape[0])
        t = out.t
        if t.lw is not None and t.lw[2] == 'pe' and getattr(t, 'pe_rows', rows) != rows:
            self._wait('pe', t.lw)
        t.pe_rows = rows
        return self.emit('pe', lambda g: g.matmul(out.ap, lhsT.ap, rhs.ap, start=start, stop=stop, **kw),
                         [lhsT, rhs], [out])

    def tr(self, out, in_, ident):
        rows = (0, in_.ap.shape[0])
        t = out.t
        if t.lw is not None and t.lw[2] == 'pe' and getattr(t, 'pe_rows', rows) != rows:
            self._wait('pe', t.lw)
        t.pe_rows = rows
        return self.emit('pe', lambda g: g.transpose(out.ap, in_.ap, ident.ap), [in_, ident], [out])

    def act(self, out, in_, func, bias=0.0, scale=1.0, accum=None, e='act'):
        a = self._a
        kw = {}
        if accum is not None:
            kw['accum_out'] = accum.ap
        return self.emit('act', lambda g: g.activation(out.ap, in_.ap, func, bias=a(bias), scale=a(scale), **kw),
                         [in_, bias, scale], [out] + ([accum] if accum is not None else []))

    def tt(self, out, a_, b_, op, e='dve'):
        return self.emit(e, lambda g: g.tensor_tensor(out.ap, a_.ap, b_.ap, op), [a_, b_], [out])

    def ts(self, out, in_, s1, s2=None, op0=ALU.mult, op1=None, e='dve', accum=None):
        a = self._a
        kw = {}
        if op1 is not None:
            kw['op1'] = op1
        if accum is not None:
            kw['accum_out'] = accum.ap
        return self.emit(e, lambda g: g.tensor_scalar(out.ap, in_.ap, a(s1), a(s2), op0, **kw),
                         [in_, s1, s2], [out] + ([accum] if accum is not None else []))

    def stt(self, out, in0, scalar, in1, op0, op1, e='dve'):
        a = self._a
        return self.emit(e, lambda g: g.scalar_tensor_tensor(out.ap, in0.ap, a(scalar), in1.ap, op0, op1),
                         [in0, scalar, in1], [out])

    def copy(self, out, in_, e='dve'):
        if e == 'act':
            return self.emit('act', lambda g: g.copy(out.ap, in_.ap), [in_], [out])
        return self.emit(e, lambda g: g.tensor_copy(out.ap, in_.ap), [in_], [out])

    def memset(self, out, val, e='dve'):
        return self.emit(e, lambda g: g.memset(out.ap, val), [], [out])

    def recip(self, out, in_):
        return self.emit('dve', lambda g: g.reciprocal(out.ap, in_.ap), [in_], [out])

    def reduce(self, out, in_, op, axis=AX.X, e='dve'):
        return self.emit(e, lambda g: g.tensor_reduce(out.ap, in_.ap, axis, op), [in_], [out])

    def bn_stats(self, out, in_):
        return self.emit('dve', lambda g: g.bn_stats(out.ap, in_.ap), [in_], [out])

    def bn_aggr(self, out, in_):
        return self.emit('dve', lambda g: g.bn_aggr(out.ap, in_.ap), [in_], [out])

    def max8(self, out, in_):
        return self.emit('dve', lambda g: g.max(out.ap, in_.ap), [in_], [out])


ALPHA = float((2 * 2) ** 0.25)
LN_EPS = 1e-5
NEXP = 8
NT = 16
TOK = 2048


def barrier(k):
    for e in k.eng:
        for f in k.eng:
            if f != e and k.cnt[f] > 0:
                k._wait(e, (k.sem[f], k.cnt[f]))
        for q, ring in k.ring.items():
            for sem, uses in ring:
                if uses > 0:
                    k._wait(e, (sem, 16 * uses))


def load_w_rows(k, dst, src_ap, nk, q='pool'):
    for kc in range(nk):
        k.dma(dst[:, kc, :], V(src_ap.t, src_ap.ap[kc * 128:(kc + 1) * 128, :]), q=q)


def layer_norm_tile(k, t, g_bc, b_bc, out, small):
    st, mv, rstd, nmr = small['st'], small['mv'], small['rstd'], small['nmr']
    k.bn_stats(st[:, 0, :], t[:, 0:512])
    k.bn_stats(st[:, 1, :], t[:, 512:1024])
    k.bn_aggr(mv.v, st.v)
    k.act(rstd.v, mv[:, 1:2], AF.Sqrt, bias=small['eps'].v)
    k.recip(rstd.v, rstd.v)
    k.stt(nmr.v, mv[:, 0:1], -1.0, rstd.v, ALU.mult, ALU.mult)
    k.act(t.v, t.v, AF.Identity, bias=nmr.v, scale=rstd.v)
    k.tt(t.v, t.v, g_bc.v, ALU.mult)
    k.tt(out.v, t.v, b_bc.v, ALU.add)


def proj_ln(k, es, srcT, w_d, res_d, g_d, b_d, out_d, outT, ident, PS, outT32=None, out_q='pool'):
    with ExitStack() as s2:
        k2 = k
        old = k.es
        k.es = s2
        w = k.sb([128, 8, 1024], BF16)
        g_bc = k.sb([128, 1024]); b_bc = k.sb([128, 1024])
        small = dict(st=k.sb([128, 2, 6]), mv=k.sb([128, 2]), rstd=k.sb([128, 1]), nmr=k.sb([128, 1]), eps=k.sb([128, 1]))
        k.memset(small['eps'].v, LN_EPS)
        xt = [k.sb([128, 1024]) for _ in range(2)]
        tt_ = [k.sb([128, 1024]) for _ in range(2)]
        ot = [k.sb([128, 1024]) for _ in range(2)]
        load_w_rows(k, w, w_d.v, 8)
        k.dma(g_bc.v, V(g_d, g_d.h[:].partition_broadcast(128)))
        k.dma(b_bc.v, V(b_d, b_d.h[:].partition_broadcast(128)))
        def mm_stage(tt):
            k.dma(xt[tt % 2].v, res_d[tt * 128:(tt + 1) * 128, :])
            for nt in range(2):
                p = PS[(tt % 2) * 4 + nt]
                for kc in range(8):
                    k.mm(p.v, srcT[:, kc, tt * 128:(tt + 1) * 128], w[:, kc, nt * 512:(nt + 1) * 512],
                         start=(kc == 0), stop=(kc == 7))

        def ln_stage(tt):
            x_ = xt[tt % 2]; t_ = tt_[tt % 2]; o_ = ot[tt % 2]
            for nt in range(2):
                p = PS[(tt % 2) * 4 + nt]
                k.stt(t_[:, nt * 512:(nt + 1) * 512], x_[:, nt * 512:(nt + 1) * 512], ALPHA, p.v, ALU.mult, ALU.add)
            layer_norm_tile(k, t_, g_bc, b_bc, o_, small)
            if out_d is not None:
                k.dma(out_d[tt * 128:(tt + 1) * 128, :], o_.v, q=out_q)

        def tr_stage(tt):
            o_ = ot[tt % 2]
            for half in range(2):
                p = PS[(tt % 2) * 4 + 2 + half]
                for j in range(4):
                    kc = half * 4 + j
                    k.tr(p[:, j * 128:(j + 1) * 128], o_[:, kc * 128:(kc + 1) * 128], ident.v)
                k.copy(outT[:, half * 4:half * 4 + 4, tt * 128:(tt + 1) * 128],
                       V(p, p.h[:].rearrange("p (j t) -> p j t", j=4)), e='act')
                if outT32 is not None:
                    k.copy(outT32[tt % 2][:, half * 4:half * 4 + 4, :],
                           V(p, p.h[:].rearrange("p (j t) -> p j t", j=4)), e='dve')

        mm_stage(0)
        for tt in range(NT):
            ln_stage(tt)
            if tt + 1 < NT:
                mm_stage(tt + 1)
            tr_stage(tt)
            if outT32 is not None and tt >= 1:
                outT32[2](tt - 1, outT32[(tt - 1) % 2])
        if outT32 is not None:
            outT32[2](NT - 1, outT32[(NT - 1) % 2])
        barrier(k)
        k.es = old


def cross_attn(k, es, x1T, memT_d, xq_d, xk_d, xv_d, attnT, PS, ones_bf):
    with ExitStack() as s2:
        old = k.es
        k.es = s2
        memT = k.sb([128, 8, 256], BF16)
        load_w_rows(k, memT, memT_d.v, 8)
        kT = k.sb([128, 8, 256], BF16)
        Vm = k.sb([128, 2, 1024], BF16)
        qT = k.sb([128, 8, TOK], BF16)
        with ExitStack() as s3:
            k.es = s3
            wk = k.sb([128, 8, 1024], BF16)
            wv = k.sb([128, 8, 1024], BF16)
            wq = k.sb([128, 8, 1024], BF16)
            load_w_rows(k, wk, xk_d.v, 8)
            load_w_rows(k, wv, xv_d.v, 8)
            load_w_rows(k, wq, xq_d.v, 8)
            for mt in range(8):
                p = PS[mt % 2]
                for kc in range(8):
                    k.mm(p[:, 0:256], wk[:, kc, mt * 128:(mt + 1) * 128], memT[:, kc, :], start=(kc == 0), stop=(kc == 7))
                k.copy(kT[:, mt, :], p[:, 0:256], e='act')
            for m in range(2):
                for nt in range(2):
                    p = PS[2 + nt]
                    for kc in range(8):
                        k.mm(p.v, memT[:, kc, m * 128:(m + 1) * 128], wv[:, kc, nt * 512:(nt + 1) * 512],
                             start=(kc == 0), stop=(kc == 7))
                    k.copy(Vm[:, m, nt * 512:(nt + 1) * 512], p.v, e='dve')
            i = 0
            for mt in range(8):
                for n in range(4):
                    p = PS[4 + i % 4]; i += 1
                    for kc in range(8):
                        k.mm(p.v, wq[:, kc, mt * 128:(mt + 1) * 128], x1T[:, kc, n * 512:(n + 1) * 512],
                             start=(kc == 0), stop=(kc == 7))
                    k.copy(qT[:, mt, n * 512:(n + 1) * 512], p.v, e=('act' if i % 2 else 'dve'))
            barrier(k)
        k.es = s2
        pT = [[k.sb([128, 512], BF16) for _ in range(2)] for _ in range(2)]
        rs = [k.sb([128, 512]) for _ in range(2)]
        its = [(h, n) for h in range(4) for n in range(4)]

        def scores(i):
            h, n = its[i]
            pp = pT[i % 2]
            for m in range(2):
                p = PS[(i % 2) * 5 + m]
                for dc in range(2):
                    k.mm(p.v, kT[:, 2 * h + dc, m * 128:(m + 1) * 128], qT[:, 2 * h + dc, n * 512:(n + 1) * 512],
                         start=(dc == 0), stop=(dc == 1))
                k.act(pp[m].v, p.v, AF.Exp, scale=1.0 / 16.0)

        scores(0)
        for i, (h, n) in enumerate(its):
            pp = pT[i % 2]; r_ = rs[i % 2]
            if i + 1 < len(its):
                scores(i + 1)
            ps_ = PS[2]
            for m in range(2):
                k.mm(ps_.v, ones_bf.v, pp[m].v, start=(m == 0), stop=(m == 1))
            k.recip(r_.v, ps_.v)
            for dc in range(2):
                p = PS[3 + dc]
                for m in range(2):
                    k.mm(p.v, Vm[:, m, (2 * h + dc) * 128:(2 * h + dc + 1) * 128], pp[m].v, start=(m == 0), stop=(m == 1))
                k.tt(attnT[:, 2 * h + dc, n * 512:(n + 1) * 512], p.v, r_.v, ALU.mult)
        barrier(k)
        k.es = old


def ffn(k, es, x2T, experts, acc, PS, gateT=None, sel=None):
    with ExitStack() as s2:
        old = k.es
        k.es = s2
        G = 4
        wgt = [k.sb([128, 8, 128], BF16) for _ in range(2)]
        wut = [k.sb([128, 8, 128], BF16) for _ in range(2)]
        wdt = [k.sb([128, G, 1024], BF16) for _ in range(2)]
        hT = [k.sb([128, G, TOK], BF16) for _ in range(1)]
        sg = [k.sb([128, 512]) for _ in range(2)]
        first = True
        wi = 0
        gi = 0
        for e, (wg_d, wu_d, wd_d) in enumerate(experts):
            F = wg_d.h.shape[1]
            nch = F // 128
            for g0 in range(0, nch, G):
                gn = min(G, nch - g0)
                wd_ = wdt[gi % 2]; gi += 1
                h_ = hT[0]
                for j in range(gn):
                    k.dma(wd_[:, j, :], wd_d[(g0 + j) * 128:(g0 + j + 1) * 128, :], q='pool')
                for j in range(gn):
                    mt = g0 + j
                    wg_ = wgt[wi % 2]; wu_ = wut[wi % 2]; wi += 1
                    k.dma(wg_.v, V(wg_d, wg_d.h[:, mt * 128:(mt + 1) * 128].rearrange("(kc p) m -> p kc m", p=128)), q='pool')
                    k.dma(wu_.v, V(wu_d, wu_d.h[:, mt * 128:(mt + 1) * 128].rearrange("(kc p) m -> p kc m", p=128)), q='pool')
                    for n in range(4):
                        pg = PS[n % 2]; pu = PS[2 + n % 2]; s_ = sg[n % 2]
                        for kc in range(8):
                            k.mm(pg.v, wg_[:, kc, :], x2T[:, kc, n * 512:(n + 1) * 512], start=(kc == 0), stop=(kc == 7))
                        for kc in range(8):
                            k.mm(pu.v, wu_[:, kc, :], x2T[:, kc, n * 512:(n + 1) * 512], start=(kc == 0), stop=(kc == 7))
                        k.act(s_.v, pg.v, AF.Silu)
                        k.tt(h_[:, j, n * 512:(n + 1) * 512], s_.v, pu.v, ALU.mult)
                for tt in range(NT):
                    for nt in range(2):
                        pd = PS[4 + (tt * 2 + nt) % 2]
                        for j in range(gn):
                            k.mm(pd.v, h_[:, j, tt * 128:(tt + 1) * 128], wd_[:, j, nt * 512:(nt + 1) * 512],
                                 start=(j == 0), stop=(j == gn - 1))
                        a_ = acc[:, tt, nt * 512:(nt + 1) * 512]
                        if gateT is None:
                            if first:
                                k.copy(a_, pd.v, e='act')
                            else:
                                k.tt(a_, a_, pd.v, ALU.add)
                        else:
                            g_ = gateT[:, tt, e:e + 1]
                            if first:
                                k.ts(a_, pd.v, g_, None, op0=ALU.mult)
                            else:
                                k.stt(a_, pd.v, g_, a_, ALU.mult, ALU.add)
                first = False
        barrier(k)
        k.es = old


def final_ln(k, es, acc, res_d, g_d, b_d, out_d, outT_d, ident, PS):
    with ExitStack() as s2:
        old = k.es
        k.es = s2
        g_bc = k.sb([128, 1024]); b_bc = k.sb([128, 1024])
        small = dict(st=k.sb([128, 2, 6]), mv=k.sb([128, 2]), rstd=k.sb([128, 1]), nmr=k.sb([128, 1]), eps=k.sb([128, 1]))
        k.memset(small['eps'].v, LN_EPS)
        xt = [k.sb([128, 1024]) for _ in range(2)]
        tt_ = [k.sb([128, 1024]) for _ in range(2)]
        ot = [k.sb([128, 1024]) for _ in range(2)]
        oT = [k.sb([128, 8, 128], BF16) for _ in range(2)]
        k.dma(g_bc.v, V(g_d, g_d.h[:].partition_broadcast(128)))
        k.dma(b_bc.v, V(b_d, b_d.h[:].partition_broadcast(128)))
        for tt in range(NT):
            x_ = xt[tt % 2]; t_ = tt_[tt % 2]; o_ = ot[tt % 2]
            k.dma(x_.v, res_d[tt * 128:(tt + 1) * 128, :])
            k.stt(t_.v, x_.v, ALPHA, acc[:, tt, :], ALU.mult, ALU.add)
            layer_norm_tile(k, t_, g_bc, b_bc, o_, small)
            k.dma(out_d[tt * 128:(tt + 1) * 128, :], o_.v, q='pool')
            if outT_d is not None:
                oT_ = oT[tt % 2]
                for half in range(2):
                    p = PS[2 + half]
                    for j in range(4):
                        kc = half * 4 + j
                        k.tr(p[:, j * 128:(j + 1) * 128], o_[:, kc * 128:(kc + 1) * 128], ident.v)
                    k.copy(oT_[:, half * 4:half * 4 + 4, :], V(p, p.h[:].rearrange("p (j t) -> p j t", j=4)), e='act')
                if callable(outT_d):
                    outT_d(tt, oT_)
                else:
                    k.dma(V(outT_d, outT_d.h[:, tt * 128:(tt + 1) * 128].rearrange("(kc p) t -> p kc t", p=128)), oT_.v)
        barrier(k)
        k.es = old


def moe_gate_tile(k, lg_ps, gate_tok, tt, tmp):
    lg, mx, nm1, ex, selm, den = tmp['lg'], tmp['mx'], tmp['nm1'], tmp['ex'], tmp['sel'], tmp['den']
    k.copy(lg.v, lg_ps, e='dve')
    k.max8(mx.v, lg.v)
    k.ts(nm1.v, mx[:, 0:1], -1.0, None, op0=ALU.mult)
    k.act(ex.v, lg.v, AF.Exp, bias=nm1.v)
    k.ts(selm.v, lg.v, mx[:, 1:2], None, op0=ALU.is_ge)
    k.tt(ex.v, ex.v, selm.v, ALU.mult)
    k.reduce(den.v, ex.v, ALU.add)
    k.recip(den.v, den.v)
    k.ts(gate_tok[:, tt, :], ex.v, den.v, None, op0=ALU.mult)


def phase_b(k, es, io, moe, PS=None, tag=''):
    ident = k.sb([128, 128])
    ones_bf = k.sb([128, 128], BF16)
    k.dma(ident.v, io['ident'].v)
    k.memset(ones_bf.v, 1.0)
    if PS is None:
        PS = [k.ps([128, 512]) for _ in range(8)]
    x1_d = io.get('x1_dbg') or k.dram('x1_scr' + tag, [TOK, 1024])
    x2_d = io.get('x2_dbg') or k.dram('x2_scr' + tag, [TOK, 1024])
    bufB = k.sb([128, 8, TOK], BF16)
    gateT = None
    outT32 = None
    if moe:
        gateT = k.sb([128, NT, 8])
        wr = k.sb([128, 8, 8])
        k.dma(wr.v, V(io['router'], io['router'].h[:].rearrange("(kc p) e -> p kc e", p=128)))
        sel = k.sb([8, 8 * 128])
        k.dma(sel.v, io['sel8'].v)
        tmp = dict(lg=k.sb([128, 8]), mx=k.sb([128, 8]), nm1=k.sb([128, 1]), ex=k.sb([128, 8]), sel=k.sb([128, 8]),
                   den=k.sb([128, 1]), gt=k.sb([128, 8]))
        x32 = [k.sb([128, 8, 128]) for _ in range(2)]

        def route(tt, xT32):
            p = PS[6]
            for kc in range(8):
                k.mm(p[:, 0:8], xT32[:, kc, :], wr[:, kc, :], start=(kc == 0), stop=(kc == 7))
            moe_gate_tile(k, p[:, 0:8], gateT, tt, tmp)
        outT32 = [x32[0], x32[1], route]
    sA = ExitStack()
    old_es = k.es
    k.es = sA
    bufA = k.sb([128, 8, TOK], BF16)
    if io.get('mix_loader') is not None:
        with k.nc.named_scope('mixload' + tag):
            io['mix_loader'](bufA)
    else:
        for kc in range(8):
            k.dma(bufA[:, kc, :], io['mixT'][kc * 128:(kc + 1) * 128, :])
    with k.nc.named_scope('projln1' + tag):
        proj_ln(k, es, bufA, io['w_out'], io['x'], io['ln1_g'], io['ln1_b'], x1_d, bufB, ident, PS)
    with k.nc.named_scope('xattn' + tag):
        cross_attn(k, es, bufB, io['memT'], io['xq'], io['xk'], io['xv'], bufA, PS, ones_bf)
    with k.nc.named_scope('projln2' + tag):
        proj_ln(k, es, bufA, io['xo'], x1_d, io['ln2_g'], io['ln2_b'], x2_d, bufB, ident, PS, outT32=outT32)
    barrier(k)
    sA.close()
    k.es = old_es
    acc = k.sb([128, NT, 1024])
    if moe:
        experts = [(V(io['moe_wg'], io['moe_wg'].h[e]), V(io['moe_wu'], io['moe_wu'].h[e]), V(io['moe_wd'], io['moe_wd'].h[e])) for e in range(NEXP)]
        experts = [tuple(Tsub(v) for v in ex) for ex in experts]
        ffn(k, es, bufB, experts, acc, PS, gateT=gateT, sel=sel)
    else:
        ffn(k, es, bufB, [(io['ffn_wg'], io['ffn_wu'], io['ffn_wd'])], acc, PS)
    with k.nc.named_scope('finalln' + tag):
        final_ln(k, es, acc, x2_d, io['ln3_g'], io['ln3_b'], io['out'], io.get('outT'), ident, PS)


class Tsub:
    def __init__(self, v):
        self.t = v.t
        self.h = v.ap
        self.name = v.t.name

    def __getitem__(self, idx):
        return V(self.t, self.h[idx])

    @property
    def v(self):
        return V(self.t, self.h)


SEQ = 4096
NBLK = 8
RMS_EPS = 1e-6
PAD = 2

O_AQ, O_AK, O_AV, O_AZ, O_AB, O_AD = 0, 384, 768, 1152, 1536, 1548
O_BQ, O_BK, O_BV = 1560, 1944, 2072
O_CF, O_CI, O_CQ, O_CG = 2200, 2712, 2968, 3224


def mmt(k, out, lhsT, rhs, start=True, stop=True, tp=None):
    if tp is None or tp == (0, 0):
        return k.mm(out, lhsT, rhs, start=start, stop=stop)
    return k.mm(out, lhsT, rhs, start=start, stop=stop, tile_position=tp)


def load_xT(k, xT, src_fn):
    k.memset(xT[:, :, 0:PAD], 0.0)
    k.memset(xT[:, :, PAD + SEQ:PAD + SEQ + PAD], 0.0)
    for kc in range(8):
        for hh in range(2):
            k.dma(xT[:, kc, PAD + hh * 2048:PAD + (hh + 1) * 2048], src_fn(kc, hh), q='pool')


def consts_a(k, io):
    c = {}
    for n in ['ident', 'bd64', 'rt128']:
        c[n] = k.sb([128, 128])
        k.dma(c[n].v, io[n].v)
    c['ones_bf'] = k.sb([128, 128], BF16)
    k.memset(c['ones_bf'].v, 1.0)
    c['eps'] = k.sb([128, 1])
    k.memset(c['eps'].v, RMS_EPS)
    return c


def rope_norm(k, ps, nwcol, cos_, sin_, out, c, W, PSr):
    xs, sq, rn, xn, t1 = W['xs'], W['sq'], W['rn'], W['xn'], W['t1']
    k.copy(xs.v, ps.v, e='act')
    k.tt(sq.v, xs.v, xs.v, ALU.mult)
    k.mm(PSr[0].v, c['bd64'].v, sq.v)
    k.act(rn.v, PSr[0].v, AF.Sqrt, bias=c['eps'].v, scale=1.0 / 64.0)
    k.recip(rn.v, rn.v)
    k.stt(xn.v, xs.v, nwcol, rn.v, ALU.mult, ALU.mult)
    k.mm(PSr[1].v, c['rt128'].v, xn.v)
    k.tt(t1.v, xn.v, cos_.v, ALU.mult)
    k.tt(sq.v, PSr[1].v, sin_.v, ALU.mult)
    k.tt(out, t1.v, sq.v, ALU.add)


def gqa_prepare(k, io, xT, c, PS, mix_units):
    wg = k.sb([128, 8, 384], BF16)
    wbv = k.sb([128, 8, 64], BF16)
    load_w_rows(k, wg, io['wg'].v, 8)
    load_w_rows(k, wbv, io['wbv'].v, 8)
    gnw = k.sb([128, 3])
    k.dma(gnw.v, io['gnw'].v)
    kT = k.sb([128, SEQ], BF16)
    Vsb = k.sb([128, 32, 128], BF16)
    cos_ = [k.sb([128, 512]) for _ in range(2)]
    sin_ = [k.sb([128, 512]) for _ in range(2)]
    W = dict(xs=k.sb([128, 512]), sq=k.sb([128, 512]), rn=k.sb([128, 512]), xn=k.sb([128, 512]), t1=k.sb([128, 512]))
    qT = [k.sb([128, 512], BF16) for _ in range(2)]
    pT = [k.sb([128, 512], BF16) for _ in range(3)]
    pT4 = [k.sb([128, 512], BF16) for _ in range(3)]
    rs = k.sb([128, 512])
    pacc = k.sb([128, 512])
    pacc2 = k.sb([128, 512])
    ones32 = k.sb([128, 64])
    k.memset(ones32.v, 1.0)
    for blk in range(NBLK):
        s = blk * 512
        cs, sn = cos_[blk % 2], sin_[blk % 2]
        k.dma(cs.v, io['cos'][:, s:s + 512])
        k.dma(sn.v, io['sin'][:, s:s + 512])
        p = PS[0]
        for kc in range(8):
            k.mm(p.v, wg[:, kc, 128:256], xT[:, kc, PAD + s:PAD + s + 512], start=(kc == 0), stop=(kc == 7))
        rope_norm(k, p, gnw[:, 1:2], cs, sn, kT[:, s:s + 512], c, W, PS[1:3])
        for t in range(4):
            ti = blk * 4 + t
            pv = PS[3 + t % 2]
            for kc in range(8):
                k.mm(pv[:, 0:64], xT[:, kc, PAD + ti * 128:PAD + (ti + 1) * 128], wbv[:, kc, :], start=(kc == 0), stop=(kc == 7))
            k.copy(Vsb[:, ti, 0:64], pv[:, 0:64], e='act')
            k.copy(Vsb[:, ti, 64:128], pv[:, 0:64], e='dve')

    def attn(PSa):
        for blk in range(NBLK):
            s = blk * 512
            cs, sn = cos_[blk % 2], sin_[blk % 2]
            k.dma(cs.v, io['cos'][:, s:s + 512])
            k.dma(sn.v, io['sin'][:, s:s + 512])
            for qi, (c0, nwc) in enumerate([(0, 0), (256, 2)]):
                p = PSa[0]
                for kc in range(8):
                    k.mm(p.v, wg[:, kc, c0:c0 + 128], xT[:, kc, PAD + s:PAD + s + 512], start=(kc == 0), stop=(kc == 7))
                rope_norm(k, p, gnw[:, nwc:nwc + 1], cs, sn, qT[qi].v, c, W, [PSa[1], PSa[0]])
                yield
            dests = mix_units(blk)
            for h in range(3):
                r = 64 if h == 1 else 0
                qsrc = qT[1] if h == 2 else qT[0]
                po = 64 if h >= 1 else 0
                pv = PSa[2][po:po + 64, :]

                GB = 0
                if GB and len(PSa) >= 7:
                    banks = [PSa[3:3 + GB], PSa[0:2] + PSa[6:7]] if GB == 3 else None
                    banks = [[PSa[0], PSa[1], PSa[3]], [PSa[4], PSa[5], PSa[6]]]
                    ngrp = 32 // GB + (1 if 32 % GB else 0)

                    def sgroup(g):
                        for j in range(GB):
                            kt = g * GB + j
                            if kt < 32:
                                mmt(k, banks[g % 2][j].v, kT[r:r + 64, kt * 128:(kt + 1) * 128], qsrc[r:r + 64, :], tp=(r, 0))
                    pTg = [pT[0], pT[1], pT[2], pT4[0], pT4[1], pT4[2]]
                    sgroup(0)
                    for g in range(ngrp):
                        for j in range(GB):
                            kt = g * GB + j
                            if kt < 32:
                                k.act(pTg[(g % 2) * 3 + j].v, banks[g % 2][j].v, AF.Exp, scale=0.125)
                        if g + 1 < ngrp:
                            sgroup(g + 1)
                        for j in range(GB):
                            kt = g * GB + j
                            if kt < 32:
                                p_ = pTg[(g % 2) * 3 + j]
                                k.mm(PSa[2].v, Vsb[:, kt, :], p_.v, start=(kt == 0), stop=(kt == 31))
                                eng_, acc_ = ('dve', pacc) if kt % 2 == 0 else ('pool', pacc2)
                                if kt < 2:
                                    k.copy(acc_.v, p_.v, e=eng_)
                                else:
                                    k.tt(acc_.v, acc_.v, p_.v, ALU.add, e=eng_)
                        yield
                else:
                    def scores(kt):
                        mmt(k, PSa[kt % 2].v, kT[r:r + 64, kt * 128:(kt + 1) * 128], qsrc[r:r + 64, :], tp=(r, 0))
                    scores(0)
                    for kt in range(32):
                        p_ = pT[kt % 3]
                        k.act(p_.v, PSa[kt % 2].v, AF.Exp, scale=0.125)
                        if kt + 1 < 32:
                            scores(kt + 1)
                        k.mm(PSa[2].v, Vsb[:, kt, :], p_.v, start=(kt == 0), stop=(kt == 31))
                        eng_, acc_ = ('dve', pacc) if kt % 2 == 0 else ('pool', pacc2)
                        if kt < 2:
                            k.copy(acc_.v, p_.v, e=eng_)
                        else:
                            k.tt(acc_.v, acc_.v, p_.v, ALU.add, e=eng_)
                        yield
                sm = PSa[0][po:po + 64, :]
                mmt(k, sm, ones32[:, 0:64], pacc.v, start=True, stop=False, tp=(0, po))
                mmt(k, sm, ones32[:, 0:64], pacc2.v, start=False, stop=True, tp=(0, po))
                k.recip(rs[po:po + 64, :], sm)
                k.tt(dests[h], pv, rs[po:po + 64, :], ALU.mult)
                yield
            mix_units(blk, done=True)
    return attn


def gqa(k, io, xT, c, PS, mix_units):
    with ExitStack() as s2:
        old = k.es
        k.es = s2
        attn = gqa_prepare(k, io, xT, c, PS, mix_units)
        for _ in attn([PS[5], PS[6], PS[3], PS[4], PS[0], PS[1], PS[2]]):
            pass
        barrier(k)
        k.es = old


def hgrn(k, io, xT, c, PS, layer, mix_dest, tick=None):
    if tick is None:
        tick = lambda n: None
    with ExitStack() as s2:
        old = k.es
        k.es = s2
        wfm = k.sb([128, 8, 512], BF16)
        wtm = k.sb([128, 8, 384], BF16)
        load_w_rows(k, wfm, io['wh_fm'].v, 8)
        load_w_rows(k, wtm, io['wh_tm'].v, 8)
        hnw = k.sb([128, 1]); k.dma(hnw.v, io['hnw'].v)
        lb_col = k.sb([128, 1]); oml_col = k.sb([128, 1]); lb_row = k.sb([128, 128]); oml_row = k.sb([128, 128])
        if layer == 0:
            k.memset(lb_col.v, 0.0); k.memset(lb_row.v, 0.0)
        else:
            lc = k.sb([128, 2]); lr = k.sb([128, 2, 128])
            k.dma(lc.v, io['lbl_col'].v)
            k.dma(lr.v, V(io['lbl_row'], io['lbl_row'].h[:].partition_broadcast(128)))
            k.tt(lb_col.v, lc[:, 1:2], lc[:, 0:1], ALU.subtract)
            k.act(lb_col.v, lb_col.v, AF.Sigmoid)
            k.tt(lb_row.v, lr[:, 1, :], lr[:, 0, :], ALU.subtract)
            k.act(lb_row.v, lb_row.v, AF.Sigmoid)
        k.ts(oml_col.v, lb_col.v, -1.0, 1.0, op0=ALU.mult, op1=ALU.add)
        k.ts(oml_row.v, lb_row.v, -1.0, 1.0, op0=ALU.mult, op1=ALU.add)
        ofw_d = k.dram('hg_ofw_l%d' % layer, [128, SEQ])
        fT = k.sb([128, 512]); kTf = k.sb([128, 512]); qs = k.sb([128, 512]); gate = k.sb([128, 512])
        obuf = k.sb([128, 512]); ofw = k.sb([128, 512])
        S = k.sb([128, 64])
        W = {n: [k.sb([128, 128]) for _ in range(2)] for n in ['ftok', 'logf', 'ktok', 'vtok', 'kitok', 'E', 'Einv', 'qd', 'ki', 'at0', 'at1']}
        sc = [k.sb([128, 6]) for _ in range(2)]
        Sm = [k.sb([128, 64]) for _ in range(2)]
        tmpu = [k.sb([128, 64]) for _ in range(2)]
        sq = k.sb([128, 512]); rn = k.sb([128, 512])
        for d in range(2):
            sfx = 'f' if d == 0 else 'b'
            trx = k.sb([128, 132]); trt = k.sb([128, 128]); mki = k.sb([128, 128])
            k.dma(trx.v, io['trx_' + sfx][:, 0:132]); k.dma(trt.v, io['trt_' + sfx].v); k.dma(mki.v, io['mki_' + sfx].v)
            k.memset(S.v, 0.0)
            blks = range(NBLK) if d == 0 else range(NBLK - 1, -1, -1)
            it = 0
            for blk in blks:
                s = blk * 512
                p = PS[2]
                for kc in range(8):
                    k.mm(p.v, wfm[:, kc, d * 128:(d + 1) * 128], xT[:, kc, PAD + s:PAD + s + 512], start=(kc == 0), stop=(kc == 7))
                k.act(fT.v, p.v, AF.Sigmoid)
                k.ts(fT.v, fT.v, oml_col.v, lb_col.v, op0=ALU.mult, op1=ALU.add)
                k.ts(kTf.v, fT.v, -1.0, 1.0, op0=ALU.mult, op1=ALU.add)
                p = PS[3]
                for kc in range(8):
                    k.mm(p.v, wfm[:, kc, 256:384], xT[:, kc, PAD + s:PAD + s + 512], start=(kc == 0), stop=(kc == 7))
                k.act(qs.v, p.v, AF.Silu)
                if d == 1:
                    p = PS[2]
                    for kc in range(8):
                        k.mm(p.v, wfm[:, kc, 384:512], xT[:, kc, PAD + s:PAD + s + 512], start=(kc == 0), stop=(kc == 7))
                    k.act(gate.v, p.v, AF.Sigmoid)
                    k.dma(ofw.v, ofw_d[:, s:s + 512])
                tiles = list(range(4)) if d == 0 else list(range(3, -1, -1))

                def Gst(t, par):
                    w = {n: W[n][par] for n in W}
                    sc_ = sc[par]
                    tc = slice(t * 128, (t + 1) * 128)
                    tok0 = PAD + s + t * 128
                    ptm = PS[2]
                    c0 = 0 if d == 0 else 128
                    for kc in range(8):
                        k.mm(ptm[:, 0:256], xT[:, kc, tok0:tok0 + 128], wtm[:, kc, c0:c0 + 256], start=(kc == 0), stop=(kc == 7))
                    yield
                    cf_ps = ptm[:, 0:128] if d == 0 else ptm[:, 128:256]
                    ci_ps = ptm[:, 128:256] if d == 0 else ptm[:, 0:128]
                    k.act(w['ftok'].v, cf_ps, AF.Exp, scale=-1.0)
                    k.copy(w['vtok'].v, ci_ps, e='act')
                    k.ts(w['ftok'].v, w['ftok'].v, 1.0, None, op0=ALU.add)
                    k.recip(w['ftok'].v, w['ftok'].v)
                    yield
                    k.tt(w['ftok'].v, w['ftok'].v, oml_row.v, ALU.mult)
                    k.tt(w['ftok'].v, w['ftok'].v, lb_row.v, ALU.add)
                    k.act(w['logf'].v, w['ftok'].v, AF.Ln)
                    k.ts(w['ktok'].v, w['ftok'].v, -1.0, 1.0, op0=ALU.mult, op1=ALU.add)
                    tick(2)
                    yield
                    pc1 = PS[3]
                    k.mm(pc1[:, 0:128], trt.v, w['logf'].v)
                    pc2 = V(PS[3], PS[3].h[:, 256:512])
                    k.mm(pc2[:, 0:132], w['logf'].v, trx.v)
                    yield
                    k.act(w['kitok'].v, pc1[:, 0:128], AF.Exp, scale=-1.0)
                    k.tt(w['kitok'].v, w['kitok'].v, w['ktok'].v, ALU.mult)
                    k.act(w['E'].v, pc2[:, 0:128], AF.Exp)
                    k.act(w['Einv'].v, pc2[:, 0:128], AF.Exp, scale=-1.0)
                    k.copy(sc_[:, 0:4], pc2[:, 128:132], e='act')
                    yield
                    k.tt(sc_[:, 4:6], sc_[:, 2:4], sc_[:, 0:2], ALU.subtract)
                    k.act(sc_.v, sc_.v, AF.Exp)
                    k.stt(w['qd'].v, qs[:, tc], 0.125, w['E'].v, ALU.mult, ALU.mult)
                    k.tt(w['ki'].v, kTf[:, tc], w['Einv'].v, ALU.mult)
                    tick(2)
                    yield
                    atm = [w['at0'], w['at1']]
                    for hh in range(2):
                        ph = hh * 64
                        pa_ = PS[5]
                        mmt(k, pa_[:, 0:128], w['ki'][ph:ph + 64, :], w['qd'][ph:ph + 64, :], tp=(ph, 0))
                        k.tt(atm[hh].v, pa_[:, 0:128], mki.v, ALU.mult)
                        yield
                    tick(2)

                def Sst(t, par):
                    w = {n: W[n][par] for n in W}
                    sc_ = sc[par]
                    tc = slice(t * 128, (t + 1) * 128)
                    atm = [w['at0'], w['at1']]
                    pso = PS[6]
                    chunks = (0, 1) if d == 0 else (1, 0)
                    for ci_, cc in enumerate(chunks):
                        pc_ = cc * 64
                        cols = slice(pc_, pc_ + 64)
                        sm_ = Sm[ci_]; tu = tmpu[ci_]
                        k.ts(sm_.v, S.v, sc_[:, cc:cc + 1], None, op0=ALU.mult)
                        yield
                        psu = PS[7]
                        for hh in range(2):
                            ph = hh * 64
                            mmt(k, pso[ph:ph + 64, cols], sm_[ph:ph + 64, :], w['qd'][ph:ph + 64, cols], start=True, stop=False, tp=(ph, ph))
                            mmt(k, pso[ph:ph + 64, cols], w['vtok'][pc_:pc_ + 64, ph:ph + 64], atm[hh][pc_:pc_ + 64, cols], start=False, stop=True, tp=(pc_, ph))
                            mmt(k, psu[ph:ph + 64, 0:64], w['kitok'][pc_:pc_ + 64, ph:ph + 64], w['vtok'][pc_:pc_ + 64, ph:ph + 64], tp=(pc_, ph))
                        yield
                        k.ts(tu.v, psu[:, 0:64], sc_[:, 4 + cc:5 + cc], None, op0=ALU.mult)
                        k.stt(S.v, S.v, sc_[:, 2 + cc:3 + cc], tu.v, ALU.mult, ALU.add)
                        tick(3)
                        yield
                    if d == 0:
                        k.copy(obuf[:, tc], pso[:, 0:128], e='act')
                    else:
                        k.tt(obuf[:, tc], pso[:, 0:128], ofw[:, tc], ALU.add)

                def merged(gens):
                    alive = list(gens)
                    while alive:
                        for g_ in list(alive):
                            try:
                                next(g_)
                            except StopIteration:
                                alive.remove(g_)

                merged([Gst(tiles[0], it % 2)])
                for ti, t in enumerate(tiles):
                    gens = [Sst(t, it % 2)]
                    if ti + 1 < len(tiles):
                        gens.append(Gst(tiles[ti + 1], (it + 1) % 2))
                    merged(gens)
                    it += 1
                if d == 0:
                    k.dma(ofw_d[:, s:s + 512], obuf.v)
                else:
                    if io.get('hg_dbg') is not None:
                        k.dma(io['hg_dbg'][:, s:s + 512], obuf.v)
                    k.tt(sq.v, obuf.v, obuf.v, ALU.mult)
                    pn = PS[2]
                    k.mm(pn.v, c['bd64'].v, sq.v)
                    k.act(rn.v, pn.v, AF.Sqrt, bias=c['eps'].v, scale=1.0 / 64.0)
                    k.recip(rn.v, rn.v)
                    k.stt(sq.v, obuf.v, hnw.v, rn.v, ALU.mult, ALU.mult)
                    k.tt(mix_dest(blk), sq.v, gate.v, ALU.mult)
                    mix_dest(blk, done=True)
        barrier(k)
        k.es = old


def gdn(k, io, xT, c, PS, layer, mix_dest):
    with k.nc.named_scope('gdn%d' % layer):
        return _gdn(k, io, xT, c, PS, layer, mix_dest)


def _gdn(k, io, xT, c, PS, layer, mix_dest):
    with ExitStack() as s2:
        old = k.es
        k.es = s2
        wcs = [k.sb([128, 8, 640], BF16) for _ in range(5)]
        with ExitStack() as s3:
            k.es = s3
            cwb = k.sb([128, 5, 640])
            k.dma(cwb.v, V(io['cw'], io['cw'].h[:].partition_broadcast(128)))
            stg = [k.sb([128, 640]) for _ in range(2)]
            for kc in range(8):
                st = stg[kc % 2]
                k.dma(st.v, io['wc'][kc * 128:(kc + 1) * 128, :])
                for j in range(5):
                    k.tt(wcs[j][:, kc, :], st.v, cwb[:, j, :], ALU.mult)
            barrier(k)
        k.es = s2
        wz = k.sb([128, 8, 256], BF16)
        wgt = k.sb([128, 8, 12], BF16)
        load_w_rows(k, wz, io['wz'].v, 8)
        load_w_rows(k, wgt, io['wgt'].v, 8)
        gpar = k.sb([128, 12])
        k.dma(gpar.v, V(io['gpar'], io['gpar'].h[:].partition_broadcast(128)))
        negA = k.sb([128, 6])
        k.act(negA.v, gpar[:, 0:6], AF.Exp)
        k.ts(negA.v, negA.v, -1.0, None, op0=ALU.mult)
        gnorm = k.sb([128, 1]); k.dma(gnorm.v, io['gnorm'].v)
        sel3 = k.sb([3, 384]); k.dma(sel3.v, io['sel3'].v)
        ofw_d = [k.dram('gd_ofw%d_l%d' % (i, layer), [128, SEQ]) for i in range(2)]
        cstash = [k.dram('gd_cs%d_l%d' % (i, layer), [128, SEQ]) for i in range(5)]
        cs = [k.sb([128, 512]) for _ in range(5)]
        zs = [k.sb([128, 512]) for _ in range(2)]
        sq = k.sb([128, 512]); rn = k.sb([128, 512])
        obuf = [k.sb([128, 512]) for _ in range(2)]
        ofw = [k.sb([128, 512]) for _ in range(2)]
        k.memset(obuf[1].v, 0.0)
        S = [k.sb([128, 64]) for _ in range(3)]
        TOK = k.sb([128, 4, 128])
        gsm = {n: k.sb([128, 6]) for n in ['bd', 'gcl', 'x1']}
        gcol = {n: k.sb([128, 3]) for n in ['beta', 'nbeta', 'g', 'ngc', 'bg', 'kdc', 't']}
        gT = k.sb([3, 256])
        H = [{n: k.sb([128, 128]) for n in ['D', 'DT', 'at', 'tmp', 'tmp2']} for _ in range(3)]
        identbf = k.sb([128, 128], BF16)
        k.copy(identbf.v, c['ident'].v)
        for h in range(3):
            for n in ['T0', 'T1']:
                H[h][n] = k.sb([128, 384], BF16)
            for n in ['vb', 'kbg']:
                H[h][n] = k.sb([128, 64], BF16)
            for n in ['kdec', 'u', 'vnew']:
                H[h][n] = k.sb([128, 64])
            H[h]['wT'] = k.sb([128, 128]); H[h]['qd'] = k.sb([128, 128]); H[h]['egb'] = k.sb([128, 128]); H[h]['ecd0'] = k.sb([128, 2]); H[h]['ecd1'] = k.sb([128, 2])
        RB = [0, 64, 0]
        QT = [cs[0], cs[0], cs[2]]
        KT = [cs[1], cs[1], cs[3]]
        KTOK = [(0, 0), (0, 64), (3, 0)]
        VTOK = [(1, 0), (1, 64), (2, 64)]
        PO = [0, 64, 0]
        for d in range(2):
            sfx = 'f' if d == 0 else 'b'
            tb = k.sb([128, 256]); negs = k.sb([128, 128]); negi = k.sb([128, 128])
            k.dma(tb.v, io['tb_' + sfx].v); k.dma(negs.v, io['negs_' + sfx].v); k.dma(negi.v, io['negi_' + sfx].v)
            for h in range(3):
                k.memset(S[h].v, 0.0)
            blks = range(NBLK) if d == 0 else range(NBLK - 1, -1, -1)
            for blk in blks:
                s = blk * 512
                if d == 0:
                    for ct in range(5):
                        p = PS[ct % 2]
                        n_ = 0
                        for j in range(5):
                            for kc in range(8):
                                k.mm(p.v, wcs[j][:, kc, ct * 128:(ct + 1) * 128], xT[:, kc, s + j:s + j + 512], start=(n_ == 0), stop=(n_ == 39))
                                n_ += 1
                        k.act(cs[ct].v, p.v, AF.Silu)
                    for ct, scl, rows in [(0, 0.125, 128), (1, 1.0, 128), (2, 0.125, 64), (3, 1.0, 128)]:
                        k.tt(sq.v, cs[ct].v, cs[ct].v, ALU.mult)
                        pn = PS[2]
                        k.mm(pn.v, c['bd64'].v, sq.v)
                        k.act(rn.v, pn.v, AF.Sqrt, bias=c['eps'].v)
                        k.recip(rn.v, rn.v)
                        k.stt(cs[ct][0:rows, :], cs[ct][0:rows, :], scl, rn[0:rows, :], ALU.mult, ALU.mult)
                    for ct in range(5):
                        k.dma(cstash[ct][:, s:s + 512], cs[ct].v, q='pool')
                else:
                    for ct in range(5):
                        k.dma(cs[ct].v, cstash[ct][:, s:s + 512])
                if d == 1:
                    for zi in range(2):
                        p = PS[zi]
                        for kc in range(8):
                            k.mm(p.v, wz[:, kc, zi * 128:(zi + 1) * 128], xT[:, kc, PAD + s:PAD + s + 512], start=(kc == 0), stop=(kc == 7))
                        k.act(zs[zi].v, p.v, AF.Silu)
                    for i in range(2):
                        k.dma(ofw[i].v, ofw_d[i][:, s:s + 512])
                tiles = list(range(4)) if d == 0 else list(range(3, -1, -1))

                def Gstage(t, par):
                    tc = slice(t * 128, (t + 1) * 128)
                    tok0 = PAD + s + t * 128
                    ptr = PS[3]
                    for si, src in enumerate([cs[1], cs[4], cs[2], cs[3]]):
                        k.tr(ptr[:, si * 128:(si + 1) * 128], src[:, tc], c['ident'].v)
                    pg = PS[4]
                    for kc in range(8):
                        k.mm(pg[:, 0:6], xT[:, kc, tok0:tok0 + 128], wgt[:, kc, d * 6:(d + 1) * 6], start=(kc == 0), stop=(kc == 7))
                    yield
                    k.copy(gsm['bd'].v, pg[:, 0:6], e='dve')
                    k.copy(TOK.v, V(ptr, ptr.h[:].rearrange("p (a b) -> p a b", a=4)), e='dve')
                    yield
                    k.act(gcol['beta'].v, gsm['bd'][:, 0:3], AF.Exp, scale=-1.0)
                    k.tt(gcol['t'].v, gsm['bd'][:, 3:6], gpar[:, 6 + d * 3:9 + d * 3], ALU.add)
                    k.act(gcol['t'].v, gcol['t'].v, AF.Exp)
                    k.ts(gcol['beta'].v, gcol['beta'].v, 1.0, None, op0=ALU.add)
                    k.recip(gcol['beta'].v, gcol['beta'].v)
                    yield
                    k.act(gcol['t'].v, gcol['t'].v, AF.Ln, bias=1.0)
                    k.tt(gcol['g'].v, gcol['t'].v, negA[:, d * 3:(d + 1) * 3], ALU.mult)
                    yield
                    k.mm(pg[:, 8:11], tb[:, 0:128], gcol['g'].v)
                    k.mm(pg[:, 11:14], tb[:, 128:256], gcol['g'].v)
                    pg2 = PS[5]
                    k.mm(pg2[0:3, 0:256], gcol['g'].v, tb.v)
                    yield
                    k.copy(gsm['gcl'].v, pg[:, 8:14], e='dve')
                    k.copy(gT.v, pg2[0:3, 0:256], e='act')
                    k.ts(gcol['ngc'].v, gsm['gcl'][:, 0:3], -1.0, None, op0=ALU.mult)
                    k.ts(gcol['nbeta'].v, gcol['beta'].v, -1.0, None, op0=ALU.mult)
                    k.act(gcol['bg'].v, gsm['gcl'][:, 0:3], AF.Exp)
                    k.tt(gcol['kdc'].v, gsm['gcl'][:, 3:6], gsm['gcl'][:, 0:3], ALU.subtract)
                    yield
                    k.tt(gcol['bg'].v, gcol['bg'].v, gcol['beta'].v, ALU.mult)
                    k.act(gcol['kdc'].v, gcol['kdc'].v, AF.Exp)
                    for h in range(3):
                        pb = PS[h]
                        k.mm(pb.v[:, 256:512], sel3[:, h * 128:(h + 1) * 128], gT.v)
                    yield
                    for h in range(3):
                        hh = H[h]; rb = RB[h]; pb = PS[h]
                        k.tt(hh['tmp'].v, negs.v, pb[:, 256:384], ALU.subtract)
                        k.tt(hh['tmp2'].v, pb[:, 256:384], negi.v, ALU.add)
                        k.act(hh['egb'][rb:rb + 64, :], pb[rb:rb + 64, 256:384], AF.Exp)
                        k.act(hh['ecd%d' % par][rb:rb + 64, 0:1], pb[rb:rb + 64, 384:385], AF.Exp)
                        k.act(hh['ecd%d' % par][rb:rb + 64, 1:2], pb[rb:rb + 64, 448:449], AF.Exp)
                        yield
                    for h in range(3):
                        hh = H[h]
                        k.act(hh['D'].v, hh['tmp'].v, AF.Exp, bias=gsm['gcl'][:, h:h + 1])
                        k.act(hh['DT'].v, hh['tmp2'].v, AF.Exp, bias=gcol['ngc'][:, h:h + 1])
                    yield

                def Mstage(t):
                    tc = slice(t * 128, (t + 1) * 128)
                    for h in range(3):
                        hh = H[h]; rb = RB[h]; ph_ = PS[h]
                        kT_ = KT[h][rb:rb + 64, tc]; qT_ = QT[h][rb:rb + 64, tc]
                        mmt(k, ph_[:, 0:128], kT_, kT_, tp=(rb, 0))
                        mmt(k, ph_[:, 128:256], kT_, qT_, tp=(rb, 0))
                        k.stt(hh['T0'][:, 0:128], ph_[:, 0:128], gcol['nbeta'][:, h:h + 1], hh['D'].v, ALU.mult, ALU.mult)
                        k.tt(hh['at'].v, ph_[:, 128:256], hh['DT'].v, ALU.mult)
                        k.tt(hh['qd'][rb:rb + 64, :], qT_, hh['egb'][rb:rb + 64, :], ALU.mult)
                        ks, ko = KTOK[h]; vs, vo = VTOK[h]
                        k.ts(hh['vb'].v, TOK[:, vs, vo:vo + 64], gcol['beta'][:, h:h + 1], None, op0=ALU.mult)
                        k.ts(hh['kbg'].v, TOK[:, ks, ko:ko + 64], gcol['bg'][:, h:h + 1], None, op0=ALU.mult)
                        k.ts(hh['kdec'].v, TOK[:, ks, ko:ko + 64], gcol['kdc'][:, h:h + 1], None, op0=ALU.mult)
                    for h in range(3):
                        hh = H[h]; ph_ = PS[h]
                        k.mm(ph_[:, 128:256], hh['T0'][:, 0:128], identbf.v)
                        k.copy(hh['T0'][:, 128:256], ph_[:, 128:256], e='act')
                    Tc, Tn = 'T0', 'T1'
                    for lvl in range(6):
                        for h in range(3):
                            hh = H[h]; ph_ = PS[h]
                            if lvl == 0:
                                k.mm(ph_[:, 128:256], hh[Tc][:, 0:128], hh[Tc][:, 128:256])
                            elif lvl <= 3:
                                k.mm(ph_[:, 128:384], hh[Tc][:, 0:128], hh[Tc][:, 128:384])
                            else:
                                k.mm(ph_[:, 256:384], hh[Tc][:, 0:128], hh[Tc][:, 256:384])
                            if lvl <= 4:
                                k.mm(ph_[:, 0:128], hh[Tc][:, 128:256], hh[Tc][:, 0:128])
                        for h in range(3):
                            hh = H[h]; ph_ = PS[h]
                            if lvl <= 3:
                                k.copy(hh[Tn][:, 0:256], ph_[:, 0:256], e='act')
                            elif lvl == 4:
                                k.copy(hh[Tn][:, 0:128], ph_[:, 0:128], e='act')
                            if lvl == 0:
                                k.tt(hh[Tn][:, 256:384], hh[Tc][:, 128:256], c['ident'].v, ALU.add)
                            else:
                                k.tt(hh[Tn][:, 256:384], hh[Tc][:, 256:384], ph_[:, 256:384], ALU.add)
                        Tc, Tn = Tn, Tc
                    for h in range(3):
                        hh = H[h]; rb = RB[h]; ph_ = PS[h]
                        X_ = hh[Tc][:, 256:384]
                        k.mm(ph_[:, 384:448], X_, hh['vb'].v)
                        mmt(k, ph_[rb:rb + 64, 0:128], hh['kbg'].v, X_, tp=(0, rb))
                        k.copy(hh['u'].v, ph_[:, 384:448], e='act')
                        k.copy(hh['wT'][rb:rb + 64, :], ph_[rb:rb + 64, 0:128], e='dve')

                def Sstage(t, par):
                    tc = slice(t * 128, (t + 1) * 128)
                    pso = PS[7]; psx = PS[6]
                    chunks = (0, 1) if d == 0 else (1, 0)
                    for cc in chunks:
                        pc_ = cc * 64
                        cols = slice(pc_, pc_ + 64)
                        for h in (0, 2, 1):
                            hh = H[h]; rb = RB[h]
                            mmt(k, psx[pc_:pc_ + 64, h * 64:(h + 1) * 64], hh['wT'][rb:rb + 64, cols], S[h][rb:rb + 64, :], tp=(rb, pc_))
                        yield
                        for h in (0, 2, 1):
                            hh = H[h]
                            k.tt(hh['vnew'][pc_:pc_ + 64, :], hh['u'][pc_:pc_ + 64, :], psx[pc_:pc_ + 64, h * 64:(h + 1) * 64], ALU.subtract)
                        yield
                        for h in (0, 2, 1):
                            hh = H[h]; rb = RB[h]; po = PO[h]
                            oc_ = slice((128 if h == 2 else 0) + pc_, (128 if h == 2 else 0) + pc_ + 64)
                            mmt(k, pso[po:po + 64, oc_], S[h][rb:rb + 64, :], hh['qd'][rb:rb + 64, cols], start=True, stop=False, tp=(rb, po))
                            mmt(k, pso[po:po + 64, oc_], hh['vnew'][pc_:pc_ + 64, :], hh['at'][pc_:pc_ + 64, cols], start=False, stop=True, tp=(pc_, po))
                            mmt(k, psx[rb:rb + 64, 256 + h * 64:256 + (h + 1) * 64], hh['kdec'][pc_:pc_ + 64, :], hh['vnew'][pc_:pc_ + 64, :], tp=(pc_, rb))
                        yield
                        for h in (0, 2, 1):
                            hh = H[h]; rb = RB[h]
                            k.stt(S[h][rb:rb + 64, :], S[h][rb:rb + 64, :], hh['ecd%d' % par][rb:rb + 64, cc:cc + 1],
                                  psx[rb:rb + 64, 256 + h * 64:256 + (h + 1) * 64], ALU.mult, ALU.add)
                        yield
                    if d == 0:
                        k.copy(obuf[0][:, tc], pso[:, 0:128], e='act')
                        k.copy(obuf[1][0:64, tc], pso[0:64, 128:256], e='act')
                    else:
                        k.tt(obuf[0][:, tc], pso[:, 0:128], ofw[0][:, tc], ALU.add)
                        k.tt(obuf[1][0:64, tc], pso[0:64, 128:256], ofw[1][0:64, tc], ALU.add)

                def merged(gens):
                    alive = list(gens)
                    while alive:
                        for g_ in list(alive):
                            try:
                                next(g_)
                            except StopIteration:
                                alive.remove(g_)

                merged([Gstage(tiles[0], 0)])
                for ti, t in enumerate(tiles):
                    Mstage(t)
                    gens = [Sstage(t, ti % 2)]
                    if ti + 1 < len(tiles):
                        gens.append(Gstage(tiles[ti + 1], (ti + 1) % 2))
                    merged(gens)
                if d == 0:
                    for i in range(2):
                        k.dma(ofw_d[i][:, s:s + 512], obuf[i].v, q='pool')
                else:
                    if io.get('gd_dbg') is not None:
                        for i in range(2):
                            k.dma(io['gd_dbg'][i * 128:(i + 1) * 128, s:s + 512], obuf[i].v)
                    dst = mix_dest(blk)
                    for i in range(2):
                        rows = 128 if i == 0 else 64
                        k.tt(sq.v, obuf[i].v, obuf[i].v, ALU.mult)
                        pn = PS[2]
                        k.mm(pn.v, c['bd64'].v, sq.v)
                        k.act(rn.v, pn.v, AF.Sqrt, bias=c['eps'].v, scale=1.0 / 64.0)
                        k.recip(rn.v, rn.v)
                        k.stt(sq[0:rows, :], obuf[i][0:rows, :], gnorm[0:rows, :], rn[0:rows, :], ALU.mult, ALU.mult)
                        k.tt(dst[i], sq[0:rows, :], zs[i][0:rows, :], ALU.mult)
                    mix_dest(blk, done=True)
        barrier(k)
        k.es = old


O_AQ, O_AK, O_AV, O_AZ, O_AB, O_AD = 0, 384, 768, 1152, 1536, 1548
O_BQ, O_BK, O_BV = 1560, 1944, 2072
O_CF, O_CI, O_CQ, O_CG = 2200, 2712, 2968, 3224

def host_consts():
    c = {}
    c['ident'] = np.eye(128, dtype=np.float32)
    bd = np.zeros((128,128), np.float32); bd[:64,:64]=1; bd[64:,64:]=1
    c['bd64'] = bd
    rt = np.zeros((128,128), np.float32)
    for b in range(4):
        for i in range(16):
            rt[b*32+i+16, b*32+i] = -1.0
            rt[b*32+i, b*32+i+16] = 1.0
    c['rt128'] = rt
    s = np.arange(4096)
    pos = np.stack([(s//64).astype(np.float32), (s%64).astype(np.float32)], 0)
    inv = (np.float32(10000.0) ** (-np.arange(0, 32, 2, dtype=np.float32) / np.float32(32))).astype(np.float32)
    d = np.arange(64)
    ang = (pos[d//32][:, :] * inv[d%16][:, None]).astype(np.float32)
    c['cos'] = np.ascontiguousarray(np.concatenate([np.cos(ang), np.cos(ang)], 0).astype(np.float32))
    c['sin'] = np.ascontiguousarray(np.concatenate([np.sin(ang), np.sin(ang)], 0).astype(np.float32))
    return c

def gqa_w(w_in_l, qw, kw, hf):
    z64 = np.zeros((1024,64), np.float32)
    bq = lambda h: w_in_l[:, O_BQ+h*64:O_BQ+(h+1)*64]
    bk = w_in_l[:, O_BK+hf*64:O_BK+(hf+1)*64]
    wg = np.concatenate([bq(3*hf), bq(3*hf+1), bk, bk, bq(3*hf+2), z64], 1)
    wbv = w_in_l[:, O_BV+hf*64:O_BV+(hf+1)*64]
    gnw = np.stack([np.concatenate([qw,qw]), np.concatenate([kw,kw]), np.concatenate([qw, np.zeros(64,np.float32)])], 1)
    return dict(wg=np.ascontiguousarray(wg), wbv=np.ascontiguousarray(wbv), gnw=np.ascontiguousarray(gnw.astype(np.float32)))

def scan_consts():
    c = {}
    idx = np.arange(128); ch = idx // 64
    same = ch[:, None] == ch[None, :]
    for sfx in ('f', 'b'):
        if sfx == 'f':
            tri = same & (idx[None, :] <= idx[:, None])
            mid = ch * 64 + 31
        else:
            tri = same & (idx[None, :] >= idx[:, None])
            mid = ch * 64 + 32
        tri = tri.astype(np.float32)
        trirel = tri - tri[mid, :]
        cext = np.zeros((128, 4), np.float32)
        for cc in range(2):
            cext[:, cc] = tri[cc * 64 + (31 if sfx == 'f' else 32), :]
            cext[:, 2 + cc] = (ch == cc).astype(np.float32)
        c['trt_' + sfx] = np.ascontiguousarray(trirel.T)
        c['trx_' + sfx] = np.ascontiguousarray(np.concatenate([trirel.T, cext, np.zeros((128, 124), np.float32)], 1))
        c['mki_' + sfx] = np.ascontiguousarray(tri.T)
        c['tria_' + sfx] = np.ascontiguousarray(tri.T)
    return c

def hgrn_w(w_in_l, lbl, hnw, hf):
    hs = [2*hf, 2*hf+1]
    def cols(o): return np.concatenate([w_in_l[:, o+h*64:o+(h+1)*64] for h in hs], 1)
    wfm = np.concatenate([cols(O_CF), cols(O_CF+256), cols(O_CQ), cols(O_CG)], 1)
    wtm = np.concatenate([cols(O_CF), cols(O_CI), cols(O_CF+256)], 1)
    ch = np.concatenate([np.arange(h*64,(h+1)*64) for h in hs])
    return dict(wh_fm=np.ascontiguousarray(wfm), wh_tm=np.ascontiguousarray(wtm),
                lbl_col=np.ascontiguousarray(lbl[:, ch].T.astype(np.float32)), lbl_row=np.ascontiguousarray(lbl[:, ch].astype(np.float32)),
                hnw=np.ascontiguousarray(np.concatenate([hnw,hnw])[:,None].astype(np.float32)))

def gdn_consts():
    c = {}
    idx = np.arange(128); ch = idx // 64
    same = ch[:, None] == ch[None, :]
    bd = same.astype(np.float32)
    for sfx in ('f', 'b'):
        if sfx == 'f':
            incl = same & (idx[None, :] <= idx[:, None])
            strict = same & (idx[None, :] < idx[:, None])
        else:
            incl = same & (idx[None, :] >= idx[:, None])
            strict = same & (idx[None, :] > idx[:, None])
        c['tb_' + sfx] = np.ascontiguousarray(np.concatenate([incl.T.astype(np.float32), bd], 1))
        c['negs_' + sfx] = np.where(strict, 0.0, -30000.0).astype(np.float32)
        c['negi_' + sfx] = np.where(incl.T, 0.0, -30000.0).astype(np.float32)
    c['sel3'] = np.repeat(np.eye(3, dtype=np.float32), 128, axis=1)
    return c

def gdn_w(w_in_l, conv_w_l, a_log_l, dt_bias_l, gnw, hf):
    hs = [3*hf, 3*hf+1, 3*hf+2]
    q = lambda h: O_AQ + h*64; kk = lambda h: O_AK + h*64; v = lambda h: O_AV + h*64
    chans = [q(hs[0]), q(hs[1]), kk(hs[0]), kk(hs[1]), q(hs[2]), v(hs[2]), kk(hs[2]), None, v(hs[0]), v(hs[1])]
    wc = np.zeros((1024, 640), np.float32); cw = np.zeros((5, 640), np.float32)
    for u, o in enumerate(chans):
        if o is None: continue
        wc[:, u*64:(u+1)*64] = w_in_l[:, o:o+64]
        cw[:, u*64:(u+1)*64] = conv_w_l[:, o:o+64]
    wz = np.zeros((1024, 256), np.float32)
    for u, h in enumerate(hs):
        wz[:, u*64:(u+1)*64] = w_in_l[:, O_AZ + h*64:O_AZ + (h+1)*64]
    cols = [O_AB + 0*6 + h for h in hs] + [O_AD + 0*6 + h for h in hs] + [O_AB + 6 + h for h in hs] + [O_AD + 6 + h for h in hs]
    wgt = np.ascontiguousarray(w_in_l[:, cols])
    gpar = np.concatenate([a_log_l[0, hs], a_log_l[1, hs], dt_bias_l[0, hs], dt_bias_l[1, hs]]).astype(np.float32)
    return dict(wc=wc, cw=cw, wz=wz, wgt=wgt, gpar=gpar, gnorm=np.concatenate([gnw, gnw])[:, None].astype(np.float32))


A_INPUTS = {
    'wc': [1024, 640], 'cw': [5, 640], 'wz': [1024, 256], 'wgt': [1024, 12], 'gpar': [12], 'gnorm': [128, 1],
    'wh_fm': [1024, 512], 'wh_tm': [1024, 384], 'hnw': [128, 1], 'wg': [1024, 384], 'wbv': [1024, 64], 'gnw': [128, 3],
}
A_CONSTS = {
    'ident': [128, 128], 'bd64': [128, 128], 'rt128': [128, 128], 'cos': [128, SEQ], 'sin': [128, SEQ], 'sel3': [3, 384],
    'tb_f': [128, 256], 'negs_f': [128, 128], 'negi_f': [128, 128], 'tb_b': [128, 256], 'negs_b': [128, 128], 'negi_b': [128, 128],
    'trx_f': [128, 256], 'trt_f': [128, 128], 'mki_f': [128, 128], 'trx_b': [128, 256], 'trt_b': [128, 128], 'mki_b': [128, 128],
    'sel8': [8, 1024], 'lbl_col': [128, 2], 'lbl_row': [2, 128], 'selw': [128, 2], 'memT': [1024, 256],
}
B_INPUTS = {'w_out': [1024, 1024], 'xq': [1024, 1024], 'xk': [1024, 1024], 'xv': [1024, 1024], 'xo': [1024, 1024],
            'ln1_g': [1024], 'ln1_b': [1024], 'ln2_g': [1024], 'ln2_b': [1024], 'ln3_g': [1024], 'ln3_b': [1024]}
B_DENSE = {'ffn_wg': [1024, 2816], 'ffn_wu': [1024, 2816], 'ffn_wd': [2816, 1024]}
B_MOE = {'router': [1024, 8], 'moe_wg': [8, 1024, 3584], 'moe_wu': [8, 1024, 3584], 'moe_wd': [8, 3584, 1024]}
DEPTH = 2


def phase_a(k, io, layer, xsrc, out_view, PS):
    with ExitStack() as sa:
        old = k.es
        k.es = sa
        c = consts_a(k, io)
        xT = k.sb([128, 8, SEQ + 2 * PAD], BF16)
        load_xT(k, xT, xsrc)
        mt = [k.sb([128, 2, 512], BF16) for _ in range(2)]
        for m_ in mt:
            k.memset(m_.v, 0.0)

        def gd_dest(blk, done=False):
            m_ = mt[blk % 2]
            if not done:
                return (m_[:, 0, :], m_[0:64, 1, :])
            k.dma(out_view(0, 128, blk * 512, (blk + 1) * 512), m_[:, 0, :], q='pool')
            k.dma(out_view(128, 192, blk * 512, (blk + 1) * 512), m_[0:64, 1, :], q='pool')

        def hg_dest(blk, done=False):
            m_ = mt[blk % 2]
            if not done:
                return m_[:, 0, :]
            k.dma(out_view(256, 384, blk * 512, (blk + 1) * 512), m_[:, 0, :])

        mtq = [k.sb([128, 2, 512], BF16) for _ in range(2)]

        def gq_dest(blk, done=False):
            m_ = mtq[blk % 2]
            if not done:
                return [m_[0:64, 0, :], m_[64:128, 0, :], m_[64:128, 1, :]]
            k.dma(out_view(384, 512, blk * 512, (blk + 1) * 512), m_[:, 0, :])
            k.dma(out_view(192, 256, blk * 512, (blk + 1) * 512), m_[64:128, 1, :])

        gdn(k, io, xT, c, PS, layer, gd_dest)
        with ExitStack() as sg:
            old2 = k.es
            k.es = sg
            attn = gqa_prepare(k, io, xT, c, PS, gq_dest)
            gen = attn([PS[0], PS[1], PS[4]])

            def tick(n):
                for _ in range(n):
                    next(gen, None)
            hgrn(k, io, xT, c, PS, layer, hg_dest, tick=tick)
            for _ in gen:
                pass
            barrier(k)
            k.es = old2
        barrier(k)
        k.es = old


def build_fused():
    nc = bass.Bass("TRN2", target_bir_lowering=False)
    names = []
    with ExitStack() as es:
        k = K(nc, es)

        def inp(n, shape, dt=F32):
            names.append(n)
            return k.dram(n, shape, dt, kind='ExternalInput')
        cst = {n: inp(n, s) for n, s in A_CONSTS.items()}
        xT0 = inp('xT0', [1024, SEQ])
        x0 = inp('x0', [TOK, 1024])
        out_final = k.dram('out', [TOK, 1024], F32, kind='ExternalOutput')
        PS = [k.ps([128, 512]) for _ in range(8)]
        x_res = x0
        xg = None
        for l in range(DEPTH):
            moe = (l % 2 == 1)
            last = (l == DEPTH - 1)
            ioa = dict(cst)
            for n, s in A_INPUTS.items():
                ioa[n] = inp('%s_l%d' % (n, l), s)
            if l == 0:
                xsrc = lambda kc, hh: xT0[kc * 128:(kc + 1) * 128, hh * 2048:(hh + 1) * 2048]
            else:
                xsrc = (lambda g: (lambda kc, hh: g[kc // 4][hh * 512 + (kc % 4) * 128:hh * 512 + (kc % 4 + 1) * 128, :]))(xg)
            mixh = [k.dram('mixh%d_l%d' % (j, l), [256, SEQ], BF16) for j in range(2)]
            mixf = [k.dram('mixf%d_l%d' % (j, l), [512, SEQ], BF16) for j in range(2)]
            with nc.named_scope('A%d' % l):
                phase_a(k, ioa, l, xsrc, lambda r0, r1, c0, c1: mixh[r0 // 256][r0 % 256:r0 % 256 + (r1 - r0), c0:c1], PS)
            with nc.named_scope('cc_mix%d' % l):
                for j in range(2):
                    k.allgather_pairs(mixf[j], mixh[j])
            iob = {'ident': cst['ident'], 'memT': cst['memT'], 'sel8': cst['sel8'], 'x': x_res}
            spec = dict(B_INPUTS)
            spec.update(B_MOE if moe else B_DENSE)
            for n, s in spec.items():
                iob[n] = inp('%s_l%d' % (n, l), s)
            with ExitStack() as sb_:
                old = k.es
                k.es = sb_

                def mix_loader(bufA, mixf=mixf):
                    with ExitStack() as sl:
                        o2 = k.es
                        k.es = sl
                        bufA2 = k.sb([128, 8, TOK], BF16)
                        selw = k.sb([128, 2])
                        k.dma(selw.v, cst['selw'].v)
                        for th, buf in enumerate([bufA, bufA2]):
                            for kc in range(8):
                                hf, i = kc // 4, kc % 4
                                r0 = hf * 256 + (i % 2) * 128
                                k.dma(buf[:, kc, :], mixf[i // 2][r0:r0 + 128, th * TOK:(th + 1) * TOK])
                        for kc in range(8):
                            k.ts(bufA2[:, kc, :], bufA2[:, kc, :], selw[:, 1:2], None, op0=ALU.mult)
                            k.stt(bufA[:, kc, :], bufA[:, kc, :], selw[:, 0:1], bufA2[:, kc, :], ALU.mult, ALU.add)
                        barrier(k)
                        k.es = o2
                iob['mix_loader'] = mix_loader
                if last:
                    iob['out'] = out_final
                else:
                    x_next = k.dram('x3_l%d' % l, [TOK, 1024])
                    x3T = [k.dram('x3T%d_l%d' % (j, l), [512, TOK], BF16) for j in range(2)]
                    iob['out'] = x_next

                    def outT_writer(tt, oT_, x3T=x3T):
                        for j in range(2):
                            k.dma(V(x3T[j], x3T[j].h[:, tt * 128:(tt + 1) * 128].rearrange("(kc p) t -> p kc t", p=128)),
                                  oT_[:, j * 4:(j + 1) * 4, :], q='pool')
                    iob['outT'] = outT_writer
                with nc.named_scope('B%d' % l):
                    phase_b(k, sb_, iob, moe, PS=PS, tag='_l%d' % l)
                barrier(k)
                k.es = old
            if not last:
                xg = [k.dram('xg%d_l%d' % (j, l), [1024, TOK], BF16) for j in range(2)]
                for j in range(2):
                    k.allgather_pairs(xg[j], x3T[j])
                x_res = x_next
        k.finish()
    return nc, names


def mix_perm():
    oa = lambda h: list(range(h * 64, (h + 1) * 64))
    ob = lambda h: list(range(384 + h * 64, 384 + (h + 1) * 64))
    oc = lambda h: list(range(768 + h * 64, 768 + (h + 1) * 64))
    p = []
    for hf in range(2):
        p += oa(3 * hf) + oa(3 * hf + 1) + oa(3 * hf + 2) + ob(3 * hf + 2) + oc(2 * hf) + oc(2 * hf + 1) + ob(3 * hf) + ob(3 * hf + 1)
    return np.array(p)


def kernel(**inp):
    inp = {n: np.asarray(v) for n, v in inp.items()}
    x = np.ascontiguousarray(inp['x'], dtype=np.float32)
    B = x.shape[0]
    C = host_consts()
    C.update(scan_consts())
    C.update(gdn_consts())
    C['sel8'] = np.repeat(np.eye(8, dtype=np.float32), 128, axis=1)
    perm = mix_perm()
    nc, names = build_fused()
    cores = list(range(8))
    shared = {}
    for l in range(DEPTH):
        for n in ['xq', 'xk', 'xv', 'xo', 'ln1_g', 'ln1_b', 'ln2_g', 'ln2_b', 'ln3_g', 'ln3_b']:
            shared['%s_l%d' % (n, l)] = inp[n][l]
        shared['w_out_l%d' % l] = inp['w_out'][l][perm]
        if l % 2 == 1:
            shared['router_l%d' % l] = inp['moe_router'][l // 2]
            for n in ['moe_wg', 'moe_wu', 'moe_wd']:
                shared['%s_l%d' % (n, l)] = inp[n][l // 2]
        else:
            for n in ['ffn_wg', 'ffn_wu', 'ffn_wd']:
                shared['%s_l%d' % (n, l)] = inp[n][l // 2]
    shared = {n: np.ascontiguousarray(v, dtype=np.float32) for n, v in shared.items()}
    per_hf = []
    for hf in range(2):
        m = {}
        for l in range(DEPTH):
            w = {}
            w.update(gdn_w(inp['w_in'][l], inp['conv_w'][l], inp['gdn_a_log'][l], inp['gdn_dt_bias'][l], inp['gdn_norm_w'][l], hf))
            hw = hgrn_w(inp['w_in'][l], inp['hgrn_lb_logits'], inp['hgrn_norm_w'][l], hf)
            m['lbl_col'] = hw.pop('lbl_col'); m['lbl_row'] = hw.pop('lbl_row')
            w.update(hw)
            w.update(gqa_w(inp['w_in'][l], inp['q_norm_w'][l], inp['k_norm_w'][l], hf))
            for n, v in w.items():
                m['%s_l%d' % (n, l)] = np.ascontiguousarray(v, dtype=np.float32)
        per_hf.append(m)
    maps = []
    for core in cores:
        b, r = core // 2, core % 2
        m = dict(C)
        m.update(shared)
        m.update(per_hf[r])
        m['xT0'] = np.ascontiguousarray(x[b].T)
        m['x0'] = np.ascontiguousarray(x[b, r * TOK:(r + 1) * TOK])
        m['memT'] = np.ascontiguousarray(inp['mem'][b].T.astype(np.float32))
        sw = np.zeros((128, 2), np.float32); sw[:, r] = 1.0
        m['selw'] = sw
        maps.append({n: m[n] for n in names})
    res = run_bass_kernel_spmd(nc, maps, core_ids=cores)
    out = np.empty((B, SEQ, 1024), np.float32)
    for core in cores:
        b, r = core // 2, core % 2
        out[b, r * TOK:(r + 1) * TOK] = res.results[core]['out']
    return out
```

```python
import os
import numpy as np
from contextlib import ExitStack
import concourse.bass as bass
import concourse.mybir as mybir
from concourse.bass_utils import run_bass_kernel_spmd

F32 = mybir.dt.float32
BF16 = mybir.dt.bfloat16
AF = mybir.ActivationFunctionType
ALU = mybir.AluOpType
AX = mybir.AxisListType


class T:
    def __init__(self, h, name):
        self.h = h
        self.name = name
        self.lw = None
        self.rd = {}
        self.psum = False

    def __getitem__(self, idx):
        return V(self, self.h[idx])

    @property
    def v(self):
        return V(self, self.h[:])


class V:
    def __init__(self, t, ap):
        self.t = getattr(t, 't', t)
        self.ap = ap

    def __getitem__(self, idx):
        return V(self.t, self.ap[idx])


class K:
    NRING = 6

    def __init__(self, nc, es):
        self.nc = nc
        self.es = es
        self.es0 = es
        self.eng = {'pe': nc.tensor, 'dve': nc.vector, 'act': nc.scalar,
                    'pool': nc.gpsimd, 'sp': nc.sync}
        self.sem = {e: es.enter_context(nc.semaphore('s_' + e)) for e in self.eng}
        self.cnt = {e: 0 for e in self.eng}
        self.known = {e: {} for e in self.eng}
        self.ring = {}
        for q in ('sp', 'pool', 'act'):
            self.ring[q] = [[es.enter_context(nc.semaphore('d_%s%d' % (q, i))), 0]
                            for i in range(self.NRING)]
        self.ring_i = {q: 0 for q in self.ring}
        self.nalloc = 0
        self.ninstr = 0

    def sb(self, shape, dt=F32, name=None):
        self.nalloc += 1
        name = name or 'sb%d' % self.nalloc
        h = self.es.enter_context(self.nc.sbuf_tensor(name, list(shape), dt))
        return T(h, name)

    def ps(self, shape, dt=F32, name=None):
        self.nalloc += 1
        name = name or 'ps%d' % self.nalloc
        h = self.es.enter_context(self.nc.psum_tensor(name, list(shape), dt))
        t = T(h, name)
        t.psum = True
        return t

    def dram(self, name, shape, dt=F32, kind=None):
        if kind is None:
            h = self.nc.dram_tensor(name, list(shape), dt)
        else:
            h = self.nc.dram_tensor(name, list(shape), dt, kind=kind)
        return T(h, name)

    def _wait(self, e, ev):
        if ev is None:
            return
        sem, val = ev[0], ev[1]
        kn = self.known[e]
        if kn.get(sem.name, 0) >= val:
            return
        self.eng[e].wait_ge(sem, val)
        kn[sem.name] = val
        self.ninstr += 1
        snap = ev[-1] if isinstance(ev[-1], dict) else None
        if snap:
            for n_, v_ in snap.items():
                if kn.get(n_, 0) < v_:
                    kn[n_] = v_

    def _pre(self, e, reads, writes):
        for v in reads:
            t = v.t
            if t.lw is not None:
                if not (e == 'pe' and t.lw[2] == 'pe'):
                    self._wait(e, t.lw)
            if t.psum:
                for src, ev in t.rd.items():
                    if src != e:
                        self._wait(e, ev)
        for v in writes:
            t = v.t
            if t.lw is not None and not (e == 'pe' and t.lw[2] == 'pe'):
                self._wait(e, t.lw)
            for src, ev in t.rd.items():
                if not (e == 'pe' and src == 'pe'):
                    self._wait(e, ev)

    def _post(self, src, ev3, reads, writes):
        for v in writes:
            v.t.lw = ev3
            v.t.rd = {}
        for v in reads:
            if v.t.lw is ev3:
                continue
            v.t.rd[src] = (ev3[0], ev3[1], ev3[3])

    def emit(self, e, fn, reads, writes):
        reads = [r for r in reads if isinstance(r, V)]
        writes = [w for w in writes if isinstance(w, V)]
        self._pre(e, reads, writes)
        ins = fn(self.eng[e])
        self.cnt[e] += 1
        ins.then_inc(self.sem[e], 1)
        self.ninstr += 1
        snap = dict(self.known[e])
        snap[self.sem[e].name] = self.cnt[e]
        ev = (self.sem[e], self.cnt[e], e, snap)
        self._post(e, ev, reads, writes)
        return ins

    def dma(self, out, in_, q='sp', **kw):
        ring = self.ring[q]
        i = self.ring_i[q]
        self.ring_i[q] = (i + 1) % len(ring)
        slot = ring[i]
        sem, uses = slot
        if uses > 0:
            self._wait(q, (sem, 16 * uses))
        self._pre(q, [in_], [out])
        ins = self.eng[q].dma_start(out=out.ap, in_=in_.ap, **kw)
        ins.then_inc(sem, 16)
        slot[1] = uses + 1
        self.ninstr += 1
        src = 'dma_' + sem.name
        ev = (sem, 16 * (uses + 1), src, dict(self.known[q]))
        self._post(src, ev, [in_], [out])
        return ins

    def allgather_pairs(self, out_t, in_t):
        self.ncc = getattr(self, 'ncc', 0) + 1
        sem = self.es0.enter_context(self.nc.semaphore('cc%d' % self.ncc))
        self._pre('pool', [in_t.v], [out_t.v])
        ins = self.eng['pool'].collective_compute(
            "AllGather", ALU.bypass, replica_groups=[[0, 1], [2, 3], [4, 5], [6, 7]],
            ins=[in_t.h.ap().opt()], outs=[out_t.h.ap().opt()])
        ins.then_inc(sem, 1)
        self.ninstr += 1
        ev = (sem, 1, 'cc%d' % self.ncc, dict(self.known['pool']))
        self._post(ev[2], ev, [in_t.v], [out_t.v])
        self.ccsems = getattr(self, 'ccsems', []) + [sem]

    def finish(self, e='sp'):
        for q, ring in self.ring.items():
            for sem, uses in ring:
                if uses > 0:
                    self._wait(e, (sem, 16 * uses))

    @staticmethod
    def _a(x):
        return x.ap if isinstance(x, V) else x

    def mm(self, out, lhsT, rhs, start=True, stop=True, **kw):
        tp = kw.get('tile_position')
        rows = (tp[0] if tp else 0, lhsT.ap.shape[0])
        t = out.t
        if t.lw is not None and t.lw[2] == 'pe' and getattr(t, 'pe_rows', rows) != rows:
            self._wait('pe', t.lw)
        t.pe_rows = rows
        return self.emit('pe', lambda g: g.matmul(out.ap, lhsT.ap, rhs.ap, start=start, stop=stop, **kw),
                         [lhsT, rhs], [out])

    def tr(self, out, in_, ident):
        rows = (0, in_.ap.shape[0])
        t = out.t
        if t.lw is not None and t.lw[2] == 'pe' and getattr(t, 'pe_rows', rows) != rows:
            self._wait('pe', t.lw)
        t.pe_rows = rows
        return self.emit('pe', lambda g: g.transpose(out.ap, in_.ap, ident.ap), [in_, ident], [out])

    def act(self, out, in_, func, bias=0.0, scale=1.0, accum=None, e='act'):
        a = self._a
        kw = {}
        if accum is not None:
            kw['accum_out'] = accum.ap
        return self.emit('act', lambda g: g.activation(out.ap, in_.ap, func, bias=a(bias), scale=a(scale), **kw),
                         [in_, bias, scale], [out] + ([accum] if accum is not None else []))

    def tt(self, out, a_, b_, op, e='dve'):
        return self.emit(e, lambda g: g.tensor_tensor(out.ap, a_.ap, b_.ap, op), [a_, b_], [out])

    def ts(self, out, in_, s1, s2=None, op0=ALU.mult, op1=None, e='dve', accum=None):
        a = self._a
        kw = {}
        if op1 is not None:
            kw['op1'] = op1
        if accum is not None:
            kw['accum_out'] = accum.ap
        return self.emit(e, lambda g: g.tensor_scalar(out.ap, in_.ap, a(s1), a(s2), op0, **kw),
                         [in_, s1, s2], [out] + ([accum] if accum is not None else []))

    def stt(self, out, in0, scalar, in1, op0, op1, e='dve'):
        a = self._a
        return self.emit(e, lambda g: g.scalar_tensor_tensor(out.ap, in0.ap, a(scalar), in1.ap, op0, op1),
                         [in0, scalar, in1], [out])

    def copy(self, out, in_, e='dve'):
        if e == 'act':
            return self.emit('act', lambda g: g.copy(out.ap, in_.ap), [in_], [out])
        return self.emit(e, lambda g: g.tensor_copy(out.ap, in_.ap), [in_], [out])

    def memset(self, out, val, e='dve'):
        return self.emit(e, lambda g: g.memset(out.ap, val), [], [out])

    def recip(self, out, in_):
        return self.emit('dve', lambda g: g.reciprocal(out.ap, in_.ap), [in_], [out])

    def reduce(self, out, in_, op, axis=AX.X, e='dve'):
        return self.emit(e, lambda g: g.tensor_reduce(out.ap, in_.ap, axis, op), [in_], [out])

    def bn_stats(self, out, in_):
        return self.emit('dve', lambda g: g.bn_stats(out.ap, in_.ap), [in_], [out])

    def bn_aggr(self, out, in_):
        return self.emit('dve', lambda g: g.bn_aggr(out.ap, in_.ap), [in_], [out])

    def max8(self, out, in_):
        return self.emit('dve', lambda g: g.max(out.ap, in_.ap), [in_], [out])


ALPHA = float((2 * 2) ** 0.25)
LN_EPS = 1e-5
NEXP = 8
NT = 16
TOK = 2048


def barrier(k):
    for e in k.eng:
        for f in k.eng:
            if f != e and k.cnt[f] > 0:
                k._wait(e, (k.sem[f], k.cnt[f]))
        for q, ring in k.ring.items():
            for sem, uses in ring:
                if uses > 0:
                    k._wait(e, (sem, 16 * uses))


def load_w_rows(k, dst, src_ap, nk, q='pool'):
    for kc in range(nk):
        k.dma(dst[:, kc, :], V(src_ap.t, src_ap.ap[kc * 128:(kc + 1) * 128, :]), q=q)


def layer_norm_tile(k, t, g_bc, b_bc, out, small):
    st, mv, rstd, nmr = small['st'], small['mv'], small['rstd'], small['nmr']
    k.bn_stats(st[:, 0, :], t[:, 0:512])
    k.bn_stats(st[:, 1, :], t[:, 512:1024])
    k.bn_aggr(mv.v, st.v)
    k.act(rstd.v, mv[:, 1:2], AF.Sqrt, bias=small['eps'].v)
    k.recip(rstd.v, rstd.v)
    k.stt(nmr.v, mv[:, 0:1], -1.0, rstd.v, ALU.mult, ALU.mult)
    k.act(t.v, t.v, AF.Identity, bias=nmr.v, scale=rstd.v)
    k.tt(t.v, t.v, g_bc.v, ALU.mult)
    k.tt(out.v, t.v, b_bc.v, ALU.add)


def proj_ln(k, es, srcT, w_d, res_d, g_d, b_d, out_d, outT, ident, PS, outT32=None, out_q='pool'):
    with ExitStack() as s2:
        k2 = k
        old = k.es
        k.es = s2
        w = k.sb([128, 8, 1024], BF16)
        g_bc = k.sb([128, 1024]); b_bc = k.sb([128, 1024])
        small = dict(st=k.sb([128, 2, 6]), mv=k.sb([128, 2]), rstd=k.sb([128, 1]), nmr=k.sb([128, 1]), eps=k.sb([128, 1]))
        k.memset(small['eps'].v, LN_EPS)
        xt = [k.sb([128, 1024]) for _ in range(2)]
        tt_ = [k.sb([128, 1024]) for _ in range(2)]
        ot = [k.sb([128, 1024]) for _ in range(2)]
        load_w_rows(k, w, w_d.v, 8)
        k.dma(g_bc.v, V(g_d, g_d.h[:].partition_broadcast(128)))
        k.dma(b_bc.v, V(b_d, b_d.h[:].partition_broadcast(128)))
        def mm_stage(tt):
            k.dma(xt[tt % 2].v, res_d[tt * 128:(tt + 1) * 128, :])
            for nt in range(2):
                p = PS[(tt % 2) * 4 + nt]
                for kc in range(8):
                    k.mm(p.v, srcT[:, kc, tt * 128:(tt + 1) * 128], w[:, kc, nt * 512:(nt + 1) * 512],
                         start=(kc == 0), stop=(kc == 7))

        def ln_stage(tt):
            x_ = xt[tt % 2]; t_ = tt_[tt % 2]; o_ = ot[tt % 2]
            for nt in range(2):
                p = PS[(tt % 2) * 4 + nt]
                k.stt(t_[:, nt * 512:(nt + 1) * 512], x_[:, nt * 512:(nt + 1) * 512], ALPHA, p.v, ALU.mult, ALU.add)
            layer_norm_tile(k, t_, g_bc, b_bc, o_, small)
            if out_d is not None:
                k.dma(out_d[tt * 128:(tt + 1) * 128, :], o_.v, q=out_q)

        def tr_stage(tt):
            o_ = ot[tt % 2]
            for half in range(2):
                p = PS[(tt % 2) * 4 + 2 + half]
                for j in range(4):
                    kc = half * 4 + j
                    k.tr(p[:, j * 128:(j + 1) * 128], o_[:, kc * 128:(kc + 1) * 128], ident.v)
                k.copy(outT[:, half * 4:half * 4 + 4, tt * 128:(tt + 1) * 128],
                       V(p, p.h[:].rearrange("p (j t) -> p j t", j=4)), e='act')
                if outT32 is not None:
                    k.copy(outT32[tt % 2][:, half * 4:half * 4 + 4, :],
                           V(p, p.h[:].rearrange("p (j t) -> p j t", j=4)), e='dve')

        mm_stage(0)
        for tt in range(NT):
            ln_stage(tt)
            if tt + 1 < NT:
                mm_stage(tt + 1)
            tr_stage(tt)
            if outT32 is not None and tt >= 1:
                outT32[2](tt - 1, outT32[(tt - 1) % 2])
        if outT32 is not None:
            outT32[2](NT - 1, outT32[(NT - 1) % 2])
        barrier(k)
        k.es = old


def cross_attn(k, es, x1T, memT_d, xq_d, xk_d, xv_d, attnT, PS, ones_bf):
    with ExitStack() as s2:
        old = k.es
        k.es = s2
        memT = k.sb([128, 8, 256], BF16)
        load_w_rows(k, memT, memT_d.v, 8)
        kT = k.sb([128, 8, 256], BF16)
        Vm = k.sb([128, 2, 1024], BF16)
        qT = k.sb([128, 8, TOK], BF16)
        with ExitStack() as s3:
            k.es = s3
            wk = k.sb([128, 8, 1024], BF16)
            wv = k.sb([128, 8, 1024], BF16)
            wq = k.sb([128, 8, 1024], BF16)
            load_w_rows(k, wk, xk_d.v, 8)
            load_w_rows(k, wv, xv_d.v, 8)
            load_w_rows(k, wq, xq_d.v, 8)
            for mt in range(8):
                p = PS[mt % 2]
                for kc in range(8):
                    k.mm(p[:, 0:256], wk[:, kc, mt * 128:(mt + 1) * 128], memT[:, kc, :], start=(kc == 0), stop=(kc == 7))
                k.copy(kT[:, mt, :], p[:, 0:256], e='act')
            for m in range(2):
                for nt in range(2):
                    p = PS[2 + nt]
                    for kc in range(8):
                        k.mm(p.v, memT[:, kc, m * 128:(m + 1) * 128], wv[:, kc, nt * 512:(nt + 1) * 512],
                             start=(kc == 0), stop=(kc == 7))
                    k.copy(Vm[:, m, nt * 512:(nt + 1) * 512], p.v, e='dve')
            i = 0
            for mt in range(8):
                for n in range(4):
                    p = PS[4 + i % 4]; i += 1
                    for kc in range(8):
                        k.mm(p.v, wq[:, kc, mt * 128:(mt + 1) * 128], x1T[:, kc, n * 512:(n + 1) * 512],
                             start=(kc == 0), stop=(kc == 7))
                    k.copy(qT[:, mt, n * 512:(n + 1) * 512], p.v, e=('act' if i % 2 else 'dve'))
            barrier(k)
        k.es = s2
        pT = [[k.sb([128, 512], BF16) for _ in range(2)] for _ in range(2)]
        rs = [k.sb([128, 512]) for _ in range(2)]
        its = [(h, n) for h in range(4) for n in range(4)]

        def scores(i):
            h, n = its[i]
            pp = pT[i % 2]
            for m in range(2):
                p = PS[(i % 2) * 5 + m]
                for dc in range(2):
                    k.mm(p.v, kT[:, 2 * h + dc, m * 128:(m + 1) * 128], qT[:, 2 * h + dc, n * 512:(n + 1) * 512],
                         start=(dc == 0), stop=(dc == 1))
                k.act(pp[m].v, p.v, AF.Exp, scale=1.0 / 16.0)

        scores(0)
        for i, (h, n) in enumerate(its):
            pp = pT[i % 2]; r_ = rs[i % 2]
            if i + 1 < len(its):
                scores(i + 1)
            ps_ = PS[2]
            for m in range(2):
                k.mm(ps_.v, ones_bf.v, pp[m].v, start=(m == 0), stop=(m == 1))
            k.recip(r_.v, ps_.v)
            for dc in range(2):
                p = PS[3 + dc]
                for m in range(2):
                    k.mm(p.v, Vm[:, m, (2 * h + dc) * 128:(2 * h + dc + 1) * 128], pp[m].v, start=(m == 0), stop=(m == 1))
                k.tt(attnT[:, 2 * h + dc, n * 512:(n + 1) * 512], p.v, r_.v, ALU.mult)
        barrier(k)
        k.es = old


def ffn(k, es, x2T, experts, acc, PS, gateT=None, sel=None):
    with ExitStack() as s2:
        old = k.es
        k.es = s2
        G = 4
        wgt = [k.sb([128, 8, 128], BF16) for _ in range(2)]
        wut = [k.sb([128, 8, 128], BF16) for _ in range(2)]
        wdt = [k.sb([128, G, 1024], BF16) for _ in range(2)]
        hT = [k.sb([128, G, TOK], BF16) for _ in range(1)]
        sg = [k.sb([128, 512]) for _ in range(2)]
        first = True
        wi = 0
        gi = 0
        for e, (wg_d, wu_d, wd_d) in enumerate(experts):
            F = wg_d.h.shape[1]
            nch = F // 128
            for g0 in range(0, nch, G):
                gn = min(G, nch - g0)
                wd_ = wdt[gi % 2]; gi += 1
                h_ = hT[0]
                for j in range(gn):
                    k.dma(wd_[:, j, :], wd_d[(g0 + j) * 128:(g0 + j + 1) * 128, :], q='pool')
                for j in range(gn):
                    mt = g0 + j
                    wg_ = wgt[wi % 2]; wu_ = wut[wi % 2]; wi += 1
                    k.dma(wg_.v, V(wg_d, wg_d.h[:, mt * 128:(mt + 1) * 128].rearrange("(kc p) m -> p kc m", p=128)), q='pool')
                    k.dma(wu_.v, V(wu_d, wu_d.h[:, mt * 128:(mt + 1) * 128].rearrange("(kc p) m -> p kc m", p=128)), q='pool')
                    for n in range(4):
                        pg = PS[n % 2]; pu = PS[2 + n % 2]; s_ = sg[n % 2]
                        for kc in range(8):
                            k.mm(pg.v, wg_[:, kc, :], x2T[:, kc, n * 512:(n + 1) * 512], start=(kc == 0), stop=(kc == 7))
                        for kc in range(8):
                            k.mm(pu.v, wu_[:, kc, :], x2T[:, kc, n * 512:(n + 1) * 512], start=(kc == 0), stop=(kc == 7))
                        k.act(s_.v, pg.v, AF.Silu)
                        k.tt(h_[:, j, n * 512:(n + 1) * 512], s_.v, pu.v, ALU.mult)
                for tt in range(NT):
                    for nt in range(2):
                        pd = PS[4 + (tt * 2 + nt) % 2]
                        for j in range(gn):
                            k.mm(pd.v, h_[:, j, tt * 128:(tt + 1) * 128], wd_[:, j, nt * 512:(nt + 1) * 512],
                                 start=(j == 0), stop=(j == gn - 1))
                        a_ = acc[:, tt, nt * 512:(nt + 1) * 512]
                        if gateT is None:
                            if first:
                                k.copy(a_, pd.v, e='act')
                            else:
                                k.tt(a_, a_, pd.v, ALU.add)
                        else:
                            g_ = gateT[:, tt, e:e + 1]
                            if first:
                                k.ts(a_, pd.v, g_, None, op0=ALU.mult)
                            else:
                                k.stt(a_, pd.v, g_, a_, ALU.mult, ALU.add)
                first = False
        barrier(k)
        k.es = old


def final_ln(k, es, acc, res_d, g_d, b_d, out_d, outT_d, ident, PS):
    with ExitStack() as s2:
        old = k.es
        k.es = s2
        g_bc = k.sb([128, 1024]); b_bc = k.sb([128, 1024])
        small = dict(st=k.sb([128, 2, 6]), mv=k.sb([128, 2]), rstd=k.sb([128, 1]), nmr=k.sb([128, 1]), eps=k.sb([128, 1]))
        k.memset(small['eps'].v, LN_EPS)
        xt = [k.sb([128, 1024]) for _ in range(2)]
        tt_ = [k.sb([128, 1024]) for _ in range(2)]
        ot = [k.sb([128, 1024]) for _ in range(2)]
        oT = [k.sb([128, 8, 128], BF16) for _ in range(2)]
        k.dma(g_bc.v, V(g_d, g_d.h[:].partition_broadcast(128)))
        k.dma(b_bc.v, V(b_d, b_d.h[:].partition_broadcast(128)))
        for tt in range(NT):
            x_ = xt[tt % 2]; t_ = tt_[tt % 2]; o_ = ot[tt % 2]
            k.dma(x_.v, res_d[tt * 128:(tt + 1) * 128, :])
            k.stt(t_.v, x_.v, ALPHA, acc[:, tt, :], ALU.mult, ALU.add)
            layer_norm_tile(k, t_, g_bc, b_bc, o_, small)
            k.dma(out_d[tt * 128:(tt + 1) * 128, :], o_.v, q='pool')
            if outT_d is not None:
                oT_ = oT[tt % 2]
                for half in range(2):
                    p = PS[2 + half]
                    for j in range(4):
                        kc = half * 4 + j
                        k.tr(p[:, j * 128:(j + 1) * 128], o_[:, kc * 128:(kc + 1) * 128], ident.v)
                    k.copy(oT_[:, half * 4:half * 4 + 4, :], V(p, p.h[:].rearrange("p (j t) -> p j t", j=4)), e='act')
                if callable(outT_d):
                    outT_d(tt, oT_)
                else:
                    k.dma(V(outT_d, outT_d.h[:, tt * 128:(tt + 1) * 128].rearrange("(kc p) t -> p kc t", p=128)), oT_.v)
        barrier(k)
        k.es = old


def moe_gate_tile(k, lg_ps, gate_tok, tt, tmp):
    lg, mx, nm1, ex, selm, den = tmp['lg'], tmp['mx'], tmp['nm1'], tmp['ex'], tmp['sel'], tmp['den']
    k.copy(lg.v, lg_ps, e='dve')
    k.max8(mx.v, lg.v)
    k.ts(nm1.v, mx[:, 0:1], -1.0, None, op0=ALU.mult)
    k.act(ex.v, lg.v, AF.Exp, bias=nm1.v)
    k.ts(selm.v, lg.v, mx[:, 1:2], None, op0=ALU.is_ge)
    k.tt(ex.v, ex.v, selm.v, ALU.mult)
    k.reduce(den.v, ex.v, ALU.add)
    k.recip(den.v, den.v)
    k.ts(gate_tok[:, tt, :], ex.v, den.v, None, op0=ALU.mult)


def phase_b(k, es, io, moe, PS=None, tag=''):
    ident = k.sb([128, 128])
    ones_bf = k.sb([128, 128], BF16)
    k.dma(ident.v, io['ident'].v)
    k.memset(ones_bf.v, 1.0)
    if PS is None:
        PS = [k.ps([128, 512]) for _ in range(8)]
    x1_d = io.get('x1_dbg') or k.dram('x1_scr' + tag, [TOK, 1024])
    x2_d = io.get('x2_dbg') or k.dram('x2_scr' + tag, [TOK, 1024])
    bufB = k.sb([128, 8, TOK], BF16)
    gateT = None
    outT32 = None
    if moe:
        gateT = k.sb([128, NT, 8])
        wr = k.sb([128, 8, 8])
        k.dma(wr.v, V(io['router'], io['router'].h[:].rearrange("(kc p) e -> p kc e", p=128)))
        sel = k.sb([8, 8 * 128])
        k.dma(sel.v, io['sel8'].v)
        tmp = dict(lg=k.sb([128, 8]), mx=k.sb([128, 8]), nm1=k.sb([128, 1]), ex=k.sb([128, 8]), sel=k.sb([128, 8]),
                   den=k.sb([128, 1]), gt=k.sb([128, 8]))
        x32 = [k.sb([128, 8, 128]) for _ in range(2)]

        def route(tt, xT32):
            p = PS[6]
            for kc in range(8):
                k.mm(p[:, 0:8], xT32[:, kc, :], wr[:, kc, :], start=(kc == 0), stop=(kc == 7))
            moe_gate_tile(k, p[:, 0:8], gateT, tt, tmp)
        outT32 = [x32[0], x32[1], route]
    sA = ExitStack()
    old_es = k.es
    k.es = sA
    bufA = k.sb([128, 8, TOK], BF16)
    if io.get('mix_loader') is not None:
        with k.nc.named_scope('mixload' + tag):
            io['mix_loader'](bufA)
    else:
        for kc in range(8):
            k.dma(bufA[:, kc, :], io['mixT'][kc * 128:(kc + 1) * 128, :])
    with k.nc.named_scope('projln1' + tag):
        proj_ln(k, es, bufA, io['w_out'], io['x'], io['ln1_g'], io['ln1_b'], x1_d, bufB, ident, PS)
    with k.nc.named_scope('xattn' + tag):
        cross_attn(k, es, bufB, io['memT'], io['xq'], io['xk'], io['xv'], bufA, PS, ones_bf)
    with k.nc.named_scope('projln2' + tag):
        proj_ln(k, es, bufA, io['xo'], x1_d, io['ln2_g'], io['ln2_b'], x2_d, bufB, ident, PS, outT32=outT32)
    barrier(k)
    sA.close()
    k.es = old_es
    acc = k.sb([128, NT, 1024])
    if moe:
        experts = [(V(io['moe_wg'], io['moe_wg'].h[e]), V(io['moe_wu'], io['moe_wu'].h[e]), V(io['moe_wd'], io['moe_wd'].h[e])) for e in range(NEXP)]
        experts = [tuple(Tsub(v) for v in ex) for ex in experts]
        ffn(k, es, bufB, experts, acc, PS, gateT=gateT, sel=sel)
    else:
        ffn(k, es, bufB, [(io['ffn_wg'], io['ffn_wu'], io['ffn_wd'])], acc, PS)
    with k.nc.named_scope('finalln' + tag):
        final_ln(k, es, acc, x2_d, io['ln3_g'], io['ln3_b'], io['out'], io.get('outT'), ident, PS)


class Tsub:
    def __init__(self, v):
        self.t = v.t
        self.h = v.ap
        self.name = v.t.name

    def __getitem__(self, idx):
        return V(self.t, self.h[idx])

    @property
    def v(self):
        return V(self.t, self.h)


SEQ = 4096
NBLK = 8
RMS_EPS = 1e-6
PAD = 2

O_AQ, O_AK, O_AV, O_AZ, O_AB, O_AD = 0, 384, 768, 1152, 1536, 1548
O_BQ, O_BK, O_BV = 1560, 1944, 2072
O_CF, O_CI, O_CQ, O_CG = 2200, 2712, 2968, 3224


def mmt(k, out, lhsT, rhs, start=True, stop=True, tp=None):
    if tp is None or tp == (0, 0):
        return k.mm(out, lhsT, rhs, start=start, stop=stop)
    return k.mm(out, lhsT, rhs, start=start, stop=stop, tile_position=tp)


def load_xT(k, xT, src_fn):
    k.memset(xT[:, :, 0:PAD], 0.0)
    k.memset(xT[:, :, PAD + SEQ:PAD + SEQ + PAD], 0.0)
    for kc in range(8):
        for hh in range(2):
            k.dma(xT[:, kc, PAD + hh * 2048:PAD + (hh + 1) * 2048], src_fn(kc, hh), q='pool')


def consts_a(k, io):
    c = {}
    for n in ['ident', 'bd64', 'rt128']:
        c[n] = k.sb([128, 128])
        k.dma(c[n].v, io[n].v)
    c['ones_bf'] = k.sb([128, 128], BF16)
    k.memset(c['ones_bf'].v, 1.0)
    c['eps'] = k.sb([128, 1])
    k.memset(c['eps'].v, RMS_EPS)
    return c


def rope_norm(k, ps, nwcol, cos_, sin_, out, c, W, PSr):
    xs, sq, rn, xn, t1 = W['xs'], W['sq'], W['rn'], W['xn'], W['t1']
    k.copy(xs.v, ps.v, e='act')
    k.tt(sq.v, xs.v, xs.v, ALU.mult)
    k.mm(PSr[0].v, c['bd64'].v, sq.v)
    k.act(rn.v, PSr[0].v, AF.Ln, bias=c['eps'].v, scale=1.0 / 64.0)
    k.act(rn.v, rn.v, AF.Exp, scale=-0.5)
    k.stt(xn.v, xs.v, nwcol, rn.v, ALU.mult, ALU.mult)
    k.mm(PSr[1].v, c['rt128'].v, xn.v)
    k.tt(t1.v, xn.v, cos_.v, ALU.mult)
    k.tt(sq.v, PSr[1].v, sin_.v, ALU.mult)
    k.tt(out, t1.v, sq.v, ALU.add)


def gqa_prepare(k, io, xT, c, PS, mix_units):
    wg = k.sb([128, 8, 384], BF16)
    wbv = k.sb([128, 8, 64], BF16)
    load_w_rows(k, wg, io['wg'].v, 8)
    load_w_rows(k, wbv, io['wbv'].v, 8)
    gnw = k.sb([128, 3])
    k.dma(gnw.v, io['gnw'].v)
    kT = k.sb([128, SEQ], BF16)
    Vsb = k.sb([128, 32, 128], BF16)
    cos_ = [k.sb([128, 512]) for _ in range(2)]
    sin_ = [k.sb([128, 512]) for _ in range(2)]
    W = dict(xs=k.sb([128, 512]), sq=k.sb([128, 512]), rn=k.sb([128, 512]), xn=k.sb([128, 512]), t1=k.sb([128, 512]))
    qT = [k.sb([128, 512], BF16) for _ in range(2)]
    pT = [k.sb([128, 512], BF16) for _ in range(3)]
    pT4 = [k.sb([128, 512], BF16) for _ in range(3)]
    rs = k.sb([128, 512])
    pacc = k.sb([128, 512])
    pacc2 = k.sb([128, 512])
    ones32 = k.sb([128, 64])
    k.memset(ones32.v, 1.0)
    for blk in range(NBLK):
        s = blk * 512
        cs, sn = cos_[blk % 2], sin_[blk % 2]
        k.dma(cs.v, io['cos'][:, s:s + 512])
        k.dma(sn.v, io['sin'][:, s:s + 512])
        p = PS[0]
        for kc in range(8):
            k.mm(p.v, wg[:, kc, 128:256], xT[:, kc, PAD + s:PAD + s + 512], start=(kc == 0), stop=(kc == 7))
        rope_norm(k, p, gnw[:, 1:2], cs, sn, kT[:, s:s + 512], c, W, PS[1:3])
        for t in range(4):
            ti = blk * 4 + t
            pv = PS[3 + t % 2]
            for kc in range(8):
                k.mm(pv[:, 0:64], xT[:, kc, PAD + ti * 128:PAD + (ti + 1) * 128], wbv[:, kc, :], start=(kc == 0), stop=(kc == 7))
            k.copy(Vsb[:, ti, 0:64], pv[:, 0:64], e='act')
            k.copy(Vsb[:, ti, 64:128], pv[:, 0:64], e='dve')

    def attn(PSa):
        for blk in range(NBLK):
            s = blk * 512
            cs, sn = cos_[blk % 2], sin_[blk % 2]
            k.dma(cs.v, io['cos'][:, s:s + 512])
            k.dma(sn.v, io['sin'][:, s:s + 512])
            for qi, (c0, nwc) in enumerate([(0, 0), (256, 2)]):
                p = PSa[0]
                for kc in range(8):
                    k.mm(p.v, wg[:, kc, c0:c0 + 128], xT[:, kc, PAD + s:PAD + s + 512], start=(kc == 0), stop=(kc == 7))
                rope_norm(k, p, gnw[:, nwc:nwc + 1], cs, sn, qT[qi].v, c, W, [PSa[1], PSa[0]])
                yield
            dests = mix_units(blk)
            for h in range(3):
                r = 64 if h == 1 else 0
                qsrc = qT[1] if h == 2 else qT[0]
                po = 64 if h >= 1 else 0
                pv = PSa[2][po:po + 64, :]

                GB = 0
                if GB and len(PSa) >= 7:
                    banks = [PSa[3:3 + GB], PSa[0:2] + PSa[6:7]] if GB == 3 else None
                    banks = [[PSa[0], PSa[1], PSa[3]], [PSa[4], PSa[5], PSa[6]]]
                    ngrp = 32 // GB + (1 if 32 % GB else 0)

                    def sgroup(g):
                        for j in range(GB):
                            kt = g * GB + j
                            if kt < 32:
                                mmt(k, banks[g % 2][j].v, kT[r:r + 64, kt * 128:(kt + 1) * 128], qsrc[r:r + 64, :], tp=(r, 0))
                    pTg = [pT[0], pT[1], pT[2], pT4[0], pT4[1], pT4[2]]
                    sgroup(0)
                    for g in range(ngrp):
                        for j in range(GB):
                            kt = g * GB + j
                            if kt < 32:
                                k.act(pTg[(g % 2) * 3 + j].v, banks[g % 2][j].v, AF.Exp, scale=0.125)
                        if g + 1 < ngrp:
                            sgroup(g + 1)
                        for j in range(GB):
                            kt = g * GB + j
                            if kt < 32:
                                p_ = pTg[(g % 2) * 3 + j]
                                k.mm(PSa[2].v, Vsb[:, kt, :], p_.v, start=(kt == 0), stop=(kt == 31))
                                eng_, acc_ = ('dve', pacc) if kt % 2 == 0 else ('pool', pacc2)
                                if kt < 2:
                                    k.copy(acc_.v, p_.v, e=eng_)
                                else:
                                    k.tt(acc_.v, acc_.v, p_.v, ALU.add, e=eng_)
                        yield
                else:
                    def scores(kt):
                        mmt(k, PSa[kt % 2].v, kT[r:r + 64, kt * 128:(kt + 1) * 128], qsrc[r:r + 64, :], tp=(r, 0))
                    scores(0)
                    for kt in range(32):
                        p_ = pT[kt % 3]
                        k.act(p_.v, PSa[kt % 2].v, AF.Exp, scale=0.125)
                        if kt + 1 < 32:
                            scores(kt + 1)
                        k.mm(PSa[2].v, Vsb[:, kt, :], p_.v, start=(kt == 0), stop=(kt == 31))
                        eng_, acc_ = ('dve', pacc) if kt % 2 == 0 else ('pool', pacc2)
                        if kt < 2:
                            k.copy(acc_.v, p_.v, e=eng_)
                        else:
                            k.tt(acc_.v, acc_.v, p_.v, ALU.add, e=eng_)
                        yield
                sm = PSa[0][po:po + 64, :]
                mmt(k, sm, ones32[:, 0:64], pacc.v, start=True, stop=False, tp=(0, po))
                mmt(k, sm, ones32[:, 0:64], pacc2.v, start=False, stop=True, tp=(0, po))
                k.recip(rs[po:po + 64, :], sm)
                k.tt(dests[h], pv, rs[po:po + 64, :], ALU.mult)
                yield
            mix_units(blk, done=True)
    return attn


def gqa(k, io, xT, c, PS, mix_units):
    with ExitStack() as s2:
        old = k.es
        k.es = s2
        attn = gqa_prepare(k, io, xT, c, PS, mix_units)
        for _ in attn([PS[5], PS[6], PS[3], PS[4], PS[0], PS[1], PS[2]]):
            pass
        barrier(k)
        k.es = old


def hgrn(k, io, xT, c, PS, layer, mix_dest, tick=None):
    if tick is None:
        tick = lambda n: None
    with ExitStack() as s2:
        old = k.es
        k.es = s2
        wfm = k.sb([128, 8, 512], BF16)
        wtm = k.sb([128, 8, 384], BF16)
        load_w_rows(k, wfm, io['wh_fm'].v, 8)
        load_w_rows(k, wtm, io['wh_tm'].v, 8)
        hnw = k.sb([128, 1]); k.dma(hnw.v, io['hnw'].v)
        lb_col = k.sb([128, 1]); oml_col = k.sb([128, 1]); lb_row = k.sb([128, 128]); oml_row = k.sb([128, 128])
        if layer == 0:
            k.memset(lb_col.v, 0.0); k.memset(lb_row.v, 0.0)
        else:
            lc = k.sb([128, 2]); lr = k.sb([128, 2, 128])
            k.dma(lc.v, io['lbl_col'].v)
            k.dma(lr.v, V(io['lbl_row'], io['lbl_row'].h[:].partition_broadcast(128)))
            k.tt(lb_col.v, lc[:, 1:2], lc[:, 0:1], ALU.subtract)
            k.act(lb_col.v, lb_col.v, AF.Sigmoid)
            k.tt(lb_row.v, lr[:, 1, :], lr[:, 0, :], ALU.subtract)
            k.act(lb_row.v, lb_row.v, AF.Sigmoid)
        k.ts(oml_col.v, lb_col.v, -1.0, 1.0, op0=ALU.mult, op1=ALU.add)
        k.ts(oml_row.v, lb_row.v, -1.0, 1.0, op0=ALU.mult, op1=ALU.add)
        ofw_d = k.dram('hg_ofw_l%d' % layer, [128, SEQ])
        fT = k.sb([128, 512]); kTf = k.sb([128, 512]); qs = k.sb([128, 512]); gate = k.sb([128, 512])
        obuf = k.sb([128, 512]); ofw = k.sb([128, 512])
        S = k.sb([128, 64])
        W = {n: [k.sb([128, 128]) for _ in range(2)] for n in ['ftok', 'logf', 'ktok', 'vtok', 'kitok', 'E', 'Einv', 'qd', 'ki', 'at0', 'at1']}
        sc = [k.sb([128, 6]) for _ in range(2)]
        Sm = [k.sb([128, 64]) for _ in range(2)]
        tmpu = [k.sb([128, 64]) for _ in range(2)]
        sq = k.sb([128, 512]); rn = k.sb([128, 512])
        for d in range(2):
            sfx = 'f' if d == 0 else 'b'
            trx = k.sb([128, 132]); trt = k.sb([128, 128]); mki = k.sb([128, 128])
            k.dma(trx.v, io['trx_' + sfx][:, 0:132]); k.dma(trt.v, io['trt_' + sfx].v); k.dma(mki.v, io['mki_' + sfx].v)
            k.memset(S.v, 0.0)
            blks = range(NBLK) if d == 0 else range(NBLK - 1, -1, -1)
            it = 0
            for blk in blks:
                s = blk * 512
                p = PS[2]
                for kc in range(8):
                    k.mm(p.v, wfm[:, kc, d * 128:(d + 1) * 128], xT[:, kc, PAD + s:PAD + s + 512], start=(kc == 0), stop=(kc == 7))
                k.act(fT.v, p.v, AF.Sigmoid)
                k.ts(fT.v, fT.v, oml_col.v, lb_col.v, op0=ALU.mult, op1=ALU.add)
                k.ts(kTf.v, fT.v, -1.0, 1.0, op0=ALU.mult, op1=ALU.add)
                p = PS[3]
                for kc in range(8):
                    k.mm(p.v, wfm[:, kc, 256:384], xT[:, kc, PAD + s:PAD + s + 512], start=(kc == 0), stop=(kc == 7))
                k.act(qs.v, p.v, AF.Silu)
                if d == 1:
                    p = PS[2]
                    for kc in range(8):
                        k.mm(p.v, wfm[:, kc, 384:512], xT[:, kc, PAD + s:PAD + s + 512], start=(kc == 0), stop=(kc == 7))
                    k.act(gate.v, p.v, AF.Sigmoid)
                    k.dma(ofw.v, ofw_d[:, s:s + 512])
                tiles = list(range(4)) if d == 0 else list(range(3, -1, -1))

                def Gst(t, par):
                    w = {n: W[n][par] for n in W}
                    sc_ = sc[par]
                    tc = slice(t * 128, (t + 1) * 128)
                    tok0 = PAD + s + t * 128
                    ptm = PS[2]
                    c0 = 0 if d == 0 else 128
                    for kc in range(8):
                        k.mm(ptm[:, 0:256], xT[:, kc, tok0:tok0 + 128], wtm[:, kc, c0:c0 + 256], start=(kc == 0), stop=(kc == 7))
                    yield
                    cf_ps = ptm[:, 0:128] if d == 0 else ptm[:, 128:256]
                    ci_ps = ptm[:, 128:256] if d == 0 else ptm[:, 0:128]
                    k.act(w['ftok'].v, cf_ps, AF.Exp, scale=-1.0)
                    k.copy(w['vtok'].v, ci_ps, e='act')
                    k.ts(w['ftok'].v, w['ftok'].v, 1.0, None, op0=ALU.add)
                    k.recip(w['ftok'].v, w['ftok'].v)
                    yield
                    k.tt(w['ftok'].v, w['ftok'].v, oml_row.v, ALU.mult)
                    k.tt(w['ftok'].v, w['ftok'].v, lb_row.v, ALU.add)
                    k.act(w['logf'].v, w['ftok'].v, AF.Ln)
                    k.ts(w['ktok'].v, w['ftok'].v, -1.0, 1.0, op0=ALU.mult, op1=ALU.add)
                    tick(2)
                    yield
                    pc1 = PS[3]
                    k.mm(pc1[:, 0:128], trt.v, w['logf'].v)
                    pc2 = V(PS[3], PS[3].h[:, 256:512])
                    k.mm(pc2[:, 0:132], w['logf'].v, trx.v)
                    yield
                    k.act(w['kitok'].v, pc1[:, 0:128], AF.Exp, scale=-1.0)
                    k.tt(w['kitok'].v, w['kitok'].v, w['ktok'].v, ALU.mult)
                    k.act(w['E'].v, pc2[:, 0:128], AF.Exp)
                    k.act(w['Einv'].v, pc2[:, 0:128], AF.Exp, scale=-1.0)
                    k.copy(sc_[:, 0:4], pc2[:, 128:132], e='act')
                    yield
                    k.tt(sc_[:, 4:6], sc_[:, 2:4], sc_[:, 0:2], ALU.subtract)
                    k.act(sc_.v, sc_.v, AF.Exp)
                    k.stt(w['qd'].v, qs[:, tc], 0.125, w['E'].v, ALU.mult, ALU.mult)
                    k.tt(w['ki'].v, kTf[:, tc], w['Einv'].v, ALU.mult)
                    tick(2)
                    yield
                    atm = [w['at0'], w['at1']]
                    for hh in range(2):
                        ph = hh * 64
                        pa_ = PS[5]
                        mmt(k, pa_[:, 0:128], w['ki'][ph:ph + 64, :], w['qd'][ph:ph + 64, :], tp=(ph, 0))
                        k.tt(atm[hh].v, pa_[:, 0:128], mki.v, ALU.mult)
                        yield
                    tick(2)

                def Sst(t, par):
                    w = {n: W[n][par] for n in W}
                    sc_ = sc[par]
                    tc = slice(t * 128, (t + 1) * 128)
                    atm = [w['at0'], w['at1']]
                    pso = PS[6]
                    chunks = (0, 1) if d == 0 else (1, 0)
                    for ci_, cc in enumerate(chunks):
                        pc_ = cc * 64
                        cols = slice(pc_, pc_ + 64)
                        sm_ = Sm[ci_]; tu = tmpu[ci_]
                        k.ts(sm_.v, S.v, sc_[:, cc:cc + 1], None, op0=ALU.mult)
                        yield
                        psu = PS[7]
                        for hh in range(2):
                            ph = hh * 64
                            mmt(k, pso[ph:ph + 64, cols], sm_[ph:ph + 64, :], w['qd'][ph:ph + 64, cols], start=True, stop=False, tp=(ph, ph))
                            mmt(k, pso[ph:ph + 64, cols], w['vtok'][pc_:pc_ + 64, ph:ph + 64], atm[hh][pc_:pc_ + 64, cols], start=False, stop=True, tp=(pc_, ph))
                            mmt(k, psu[ph:ph + 64, 0:64], w['kitok'][pc_:pc_ + 64, ph:ph + 64], w['vtok'][pc_:pc_ + 64, ph:ph + 64], tp=(pc_, ph))
                        yield
                        k.ts(tu.v, psu[:, 0:64], sc_[:, 4 + cc:5 + cc], None, op0=ALU.mult)
                        k.stt(S.v, S.v, sc_[:, 2 + cc:3 + cc], tu.v, ALU.mult, ALU.add)
                        tick(3)
                        yield
                    if d == 0:
                        k.copy(obuf[:, tc], pso[:, 0:128], e='act')
                    else:
                        k.tt(obuf[:, tc], pso[:, 0:128], ofw[:, tc], ALU.add)

                def merged(gens):
                    alive = list(gens)
                    while alive:
                        for g_ in list(alive):
                            try:
                                next(g_)
                            except StopIteration:
                                alive.remove(g_)

                merged([Gst(tiles[0], it % 2)])
                for ti, t in enumerate(tiles):
                    gens = [Sst(t, it % 2)]
                    if ti + 1 < len(tiles):
                        gens.append(Gst(tiles[ti + 1], (it + 1) % 2))
                    merged(gens)
                    it += 1
                if d == 0:
                    k.dma(ofw_d[:, s:s + 512], obuf.v)
                else:
                    if io.get('hg_dbg') is not None:
                        k.dma(io['hg_dbg'][:, s:s + 512], obuf.v)
                    k.tt(sq.v, obuf.v, obuf.v, ALU.mult)
                    pn = PS[2]
                    k.mm(pn.v, c['bd64'].v, sq.v)
                    k.act(rn.v, pn.v, AF.Sqrt, bias=c['eps'].v, scale=1.0 / 64.0)
                    k.recip(rn.v, rn.v)
                    k.stt(sq.v, obuf.v, hnw.v, rn.v, ALU.mult, ALU.mult)
                    k.tt(mix_dest(blk), sq.v, gate.v, ALU.mult)
                    mix_dest(blk, done=True)
        barrier(k)
        k.es = old


def gdn(k, io, xT, c, PS, layer, mix_dest):
    with k.nc.named_scope('gdn%d' % layer):
        return _gdn(k, io, xT, c, PS, layer, mix_dest)


def _gdn(k, io, xT, c, PS, layer, mix_dest):
    with ExitStack() as s2:
        old = k.es
        k.es = s2
        wcs = [k.sb([128, 8, 640], BF16) for _ in range(5)]
        with ExitStack() as s3:
            k.es = s3
            cwb = k.sb([128, 5, 640])
            k.dma(cwb.v, V(io['cw'], io['cw'].h[:].partition_broadcast(128)))
            stg = [k.sb([128, 640]) for _ in range(2)]
            for kc in range(8):
                st = stg[kc % 2]
                k.dma(st.v, io['wc'][kc * 128:(kc + 1) * 128, :])
                for j in range(5):
                    k.tt(wcs[j][:, kc, :], st.v, cwb[:, j, :], ALU.mult)
            barrier(k)
        k.es = s2
        wz = k.sb([128, 8, 256], BF16)
        wgt = k.sb([128, 8, 12], BF16)
        load_w_rows(k, wz, io['wz'].v, 8)
        load_w_rows(k, wgt, io['wgt'].v, 8)
        gpar = k.sb([128, 12])
        k.dma(gpar.v, V(io['gpar'], io['gpar'].h[:].partition_broadcast(128)))
        negA = k.sb([128, 6])
        k.act(negA.v, gpar[:, 0:6], AF.Exp)
        k.ts(negA.v, negA.v, -1.0, None, op0=ALU.mult)
        gnorm = k.sb([128, 1]); k.dma(gnorm.v, io['gnorm'].v)
        sel3 = k.sb([3, 384]); k.dma(sel3.v, io['sel3'].v)
        ofw_d = [k.dram('gd_ofw%d_l%d' % (i, layer), [128, SEQ]) for i in range(2)]
        cstash = [k.dram('gd_cs%d_l%d' % (i, layer), [128, SEQ]) for i in range(5)]
        cs = [k.sb([128, 512]) for _ in range(5)]
        zs = [k.sb([128, 512]) for _ in range(2)]
        sq = k.sb([128, 512]); rn = k.sb([128, 512])
        obuf = [k.sb([128, 512]) for _ in range(2)]
        ofw = [k.sb([128, 512]) for _ in range(2)]
        k.memset(obuf[1].v, 0.0)
        S = [k.sb([128, 64]) for _ in range(3)]
        TOK = k.sb([128, 4, 128])
        gsm = {n: k.sb([128, 6]) for n in ['bd', 'gcl', 'x1']}
        gcol = {n: k.sb([128, 3]) for n in ['beta', 'nbeta', 'g', 'ngc', 'bg', 'kdc', 't']}
        gT = k.sb([3, 256])
        H = [{n: k.sb([128, 128]) for n in ['D', 'DT', 'at', 'tmp', 'tmp2']} for _ in range(3)]
        identbf = k.sb([128, 128], BF16)
        k.copy(identbf.v, c['ident'].v)
        for h in range(3):
            for n in ['T0', 'T1']:
                H[h][n] = k.sb([128, 384], BF16)
            for n in ['vb', 'kbg']:
                H[h][n] = k.sb([128, 64], BF16)
            for n in ['kdec', 'u', 'vnew']:
                H[h][n] = k.sb([128, 64])
            H[h]['wT'] = k.sb([128, 128]); H[h]['qd'] = k.sb([128, 128]); H[h]['egb'] = k.sb([128, 128]); H[h]['ecd0'] = k.sb([128, 2]); H[h]['ecd1'] = k.sb([128, 2])
        RB = [0, 64, 0]
        QT = [cs[0], cs[0], cs[2]]
        KT = [cs[1], cs[1], cs[3]]
        KTOK = [(0, 0), (0, 64), (3, 0)]
        VTOK = [(1, 0), (1, 64), (2, 64)]
        PO = [0, 64, 0]
        for d in range(2):
            sfx = 'f' if d == 0 else 'b'
            tb = k.sb([128, 256]); negs = k.sb([128, 128]); negi = k.sb([128, 128])
            k.dma(tb.v, io['tb_' + sfx].v); k.dma(negs.v, io['negs_' + sfx].v); k.dma(negi.v, io['negi_' + sfx].v)
            for h in range(3):
                k.memset(S[h].v, 0.0)
            blks = range(NBLK) if d == 0 else range(NBLK - 1, -1, -1)
            for blk in blks:
                s = blk * 512
                if d == 0:
                    for ct in range(5):
                        p = PS[ct % 2]
                        n_ = 0
                        for j in range(5):
                            for kc in range(8):
                                k.mm(p.v, wcs[j][:, kc, ct * 128:(ct + 1) * 128], xT[:, kc, s + j:s + j + 512], start=(n_ == 0), stop=(n_ == 39))
                                n_ += 1
                        k.act(cs[ct].v, p.v, AF.Silu)
                    for ct, scl, rows in [(0, 0.125, 128), (1, 1.0, 128), (2, 0.125, 64), (3, 1.0, 128)]:
                        k.tt(sq.v, cs[ct].v, cs[ct].v, ALU.mult)
                        pn = PS[2]
                        k.mm(pn.v, c['bd64'].v, sq.v)
                        k.act(rn.v, pn.v, AF.Sqrt, bias=c['eps'].v)
                        k.recip(rn.v, rn.v)
                        k.stt(cs[ct][0:rows, :], cs[ct][0:rows, :], scl, rn[0:rows, :], ALU.mult, ALU.mult)
                    for ct in range(5):
                        k.dma(cstash[ct][:, s:s + 512], cs[ct].v, q='pool')
                else:
                    for ct in range(5):
                        k.dma(cs[ct].v, cstash[ct][:, s:s + 512])
                if d == 1:
                    for zi in range(2):
                        p = PS[zi]
                        for kc in range(8):
                            k.mm(p.v, wz[:, kc, zi * 128:(zi + 1) * 128], xT[:, kc, PAD + s:PAD + s + 512], start=(kc == 0), stop=(kc == 7))
                        k.act(zs[zi].v, p.v, AF.Silu)
                    for i in range(2):
                        k.dma(ofw[i].v, ofw_d[i][:, s:s + 512])
                tiles = list(range(4)) if d == 0 else list(range(3, -1, -1))

                def Gstage(t, par):
                    tc = slice(t * 128, (t + 1) * 128)
                    tok0 = PAD + s + t * 128
                    ptr = PS[3]
                    for si, src in enumerate([cs[1], cs[4], cs[2], cs[3]]):
                        k.tr(ptr[:, si * 128:(si + 1) * 128], src[:, tc], c['ident'].v)
                    pg = PS[4]
                    for kc in range(8):
                        k.mm(pg[:, 0:6], xT[:, kc, tok0:tok0 + 128], wgt[:, kc, d * 6:(d + 1) * 6], start=(kc == 0), stop=(kc == 7))
                    yield
                    k.copy(gsm['bd'].v, pg[:, 0:6], e='dve')
                    k.copy(TOK.v, V(ptr, ptr.h[:].rearrange("p (a b) -> p a b", a=4)), e='dve')
                    yield
                    k.act(gcol['beta'].v, gsm['bd'][:, 0:3], AF.Exp, scale=-1.0)
                    k.tt(gcol['t'].v, gsm['bd'][:, 3:6], gpar[:, 6 + d * 3:9 + d * 3], ALU.add)
                    k.act(gcol['t'].v, gcol['t'].v, AF.Exp)
                    k.ts(gcol['beta'].v, gcol['beta'].v, 1.0, None, op0=ALU.add)
                    k.recip(gcol['beta'].v, gcol['beta'].v)
                    yield
                    k.act(gcol['t'].v, gcol['t'].v, AF.Ln, bias=1.0)
                    k.tt(gcol['g'].v, gcol['t'].v, negA[:, d * 3:(d + 1) * 3], ALU.mult)
                    yield
                    k.mm(pg[:, 8:11], tb[:, 0:128], gcol['g'].v)
                    k.mm(pg[:, 11:14], tb[:, 128:256], gcol['g'].v)
                    pg2 = PS[5]
                    k.mm(pg2[0:3, 0:256], gcol['g'].v, tb.v)
                    yield
                    k.copy(gsm['gcl'].v, pg[:, 8:14], e='dve')
                    k.copy(gT.v, pg2[0:3, 0:256], e='act')
                    k.ts(gcol['ngc'].v, gsm['gcl'][:, 0:3], -1.0, None, op0=ALU.mult)
                    k.ts(gcol['nbeta'].v, gcol['beta'].v, -1.0, None, op0=ALU.mult)
                    k.act(gcol['bg'].v, gsm['gcl'][:, 0:3], AF.Exp)
                    k.tt(gcol['kdc'].v, gsm['gcl'][:, 3:6], gsm['gcl'][:, 0:3], ALU.subtract)
                    yield
                    k.tt(gcol['bg'].v, gcol['bg'].v, gcol['beta'].v, ALU.mult)
                    k.act(gcol['kdc'].v, gcol['kdc'].v, AF.Exp)
                    for h in range(3):
                        pb = PS[h]
                        k.mm(pb.v[:, 256:512], sel3[:, h * 128:(h + 1) * 128], gT.v)
                    yield
                    for h in range(3):
                        hh = H[h]; rb = RB[h]; pb = PS[h]
                        k.tt(hh['tmp'].v, negs.v, pb[:, 256:384], ALU.subtract)
                        k.tt(hh['tmp2'].v, pb[:, 256:384], negi.v, ALU.add)
                        k.act(hh['egb'][rb:rb + 64, :], pb[rb:rb + 64, 256:384], AF.Exp)
                        k.act(hh['ecd%d' % par][rb:rb + 64, 0:1], pb[rb:rb + 64, 384:385], AF.Exp)
                        k.act(hh['ecd%d' % par][rb:rb + 64, 1:2], pb[rb:rb + 64, 448:449], AF.Exp)
                        yield
                    for h in range(3):
                        hh = H[h]
                        k.act(hh['D'].v, hh['tmp'].v, AF.Exp, bias=gsm['gcl'][:, h:h + 1])
                        k.act(hh['DT'].v, hh['tmp2'].v, AF.Exp, bias=gcol['ngc'][:, h:h + 1])
                    yield

                def Mstage(t):
                    tc = slice(t * 128, (t + 1) * 128)
                    for h in range(3):
                        hh = H[h]; rb = RB[h]; ph_ = PS[h]
                        kT_ = KT[h][rb:rb + 64, tc]; qT_ = QT[h][rb:rb + 64, tc]
                        mmt(k, ph_[:, 0:128], kT_, kT_, tp=(rb, 0))
                        mmt(k, ph_[:, 128:256], kT_, qT_, tp=(rb, 0))
                        k.stt(hh['T0'][:, 0:128], ph_[:, 0:128], gcol['nbeta'][:, h:h + 1], hh['D'].v, ALU.mult, ALU.mult)
                        k.tt(hh['at'].v, ph_[:, 128:256], hh['DT'].v, ALU.mult)
                        k.tt(hh['qd'][rb:rb + 64, :], qT_, hh['egb'][rb:rb + 64, :], ALU.mult)
                        ks, ko = KTOK[h]; vs, vo = VTOK[h]
                        k.ts(hh['vb'].v, TOK[:, vs, vo:vo + 64], gcol['beta'][:, h:h + 1], None, op0=ALU.mult)
                        k.ts(hh['kbg'].v, TOK[:, ks, ko:ko + 64], gcol['bg'][:, h:h + 1], None, op0=ALU.mult)
                        k.ts(hh['kdec'].v, TOK[:, ks, ko:ko + 64], gcol['kdc'][:, h:h + 1], None, op0=ALU.mult)
                    for h in range(3):
                        hh = H[h]; ph_ = PS[h]
                        k.mm(ph_[:, 128:256], hh['T0'][:, 0:128], identbf.v)
                        k.copy(hh['T0'][:, 128:256], ph_[:, 128:256], e='act')
                    Tc, Tn = 'T0', 'T1'
                    for lvl in range(6):
                        for h in range(3):
                            hh = H[h]; ph_ = PS[h]
                            if lvl == 0:
                                k.mm(ph_[:, 128:256], hh[Tc][:, 0:128], hh[Tc][:, 128:256])
                            elif lvl <= 3:
                                k.mm(ph_[:, 128:384], hh[Tc][:, 0:128], hh[Tc][:, 128:384])
                            else:
                                k.mm(ph_[:, 256:384], hh[Tc][:, 0:128], hh[Tc][:, 256:384])
                            if lvl <= 4:
                                k.mm(ph_[:, 0:128], hh[Tc][:, 128:256], hh[Tc][:, 0:128])
                        for h in range(3):
                            hh = H[h]; ph_ = PS[h]
                            if lvl <= 3:
                                k.copy(hh[Tn][:, 0:256], ph_[:, 0:256], e='act')
                            elif lvl == 4:
                                k.copy(hh[Tn][:, 0:128], ph_[:, 0:128], e='act')
                            if lvl == 0:
                                k.tt(hh[Tn][:, 256:384], hh[Tc][:, 128:256], c['ident'].v, ALU.add)
                            else:
                                k.tt(hh[Tn][:, 256:384], hh[Tc][:, 256:384], ph_[:, 256:384], ALU.add)
                        Tc, Tn = Tn, Tc
                    for h in range(3):
                        hh = H[h]; rb = RB[h]; ph_ = PS[h]
                        X_ = hh[Tc][:, 256:384]
                        k.mm(ph_[:, 384:448], X_, hh['vb'].v)
                        mmt(k, ph_[rb:rb + 64, 0:128], hh['kbg'].v, X_, tp=(0, rb))
                        k.copy(hh['u'].v, ph_[:, 384:448], e='act')
                        k.copy(hh['wT'][rb:rb + 64, :], ph_[rb:rb + 64, 0:128], e='dve')

                def Sstage(t, par):
                    tc = slice(t * 128, (t + 1) * 128)
                    pso = PS[7]; psx = PS[6]
                    chunks = (0, 1) if d == 0 else (1, 0)
                    for cc in chunks:
                        pc_ = cc * 64
                        cols = slice(pc_, pc_ + 64)
                        for h in (0, 2, 1):
                            hh = H[h]; rb = RB[h]
                            mmt(k, psx[pc_:pc_ + 64, h * 64:(h + 1) * 64], hh['wT'][rb:rb + 64, cols], S[h][rb:rb + 64, :], tp=(rb, pc_))
                        yield
                        for h in (0, 2, 1):
                            hh = H[h]
                            k.tt(hh['vnew'][pc_:pc_ + 64, :], hh['u'][pc_:pc_ + 64, :], psx[pc_:pc_ + 64, h * 64:(h + 1) * 64], ALU.subtract)
                        yield
                        for h in (0, 2, 1):
                            hh = H[h]; rb = RB[h]; po = PO[h]
                            oc_ = slice((128 if h == 2 else 0) + pc_, (128 if h == 2 else 0) + pc_ + 64)
                            mmt(k, pso[po:po + 64, oc_], S[h][rb:rb + 64, :], hh['qd'][rb:rb + 64, cols], start=True, stop=False, tp=(rb, po))
                            mmt(k, pso[po:po + 64, oc_], hh['vnew'][pc_:pc_ + 64, :], hh['at'][pc_:pc_ + 64, cols], start=False, stop=True, tp=(pc_, po))
                            mmt(k, psx[rb:rb + 64, 256 + h * 64:256 + (h + 1) * 64], hh['kdec'][pc_:pc_ + 64, :], hh['vnew'][pc_:pc_ + 64, :], tp=(pc_, rb))
                        yield
                        for h in (0, 2, 1):
                            hh = H[h]; rb = RB[h]
                            k.stt(S[h][rb:rb + 64, :], S[h][rb:rb + 64, :], hh['ecd%d' % par][rb:rb + 64, cc:cc + 1],
                                  psx[rb:rb + 64, 256 + h * 64:256 + (h + 1) * 64], ALU.mult, ALU.add)
                        yield
                    if d == 0:
                        k.copy(obuf[0][:, tc], pso[:, 0:128], e='act')
                        k.copy(obuf[1][0:64, tc], pso[0:64, 128:256], e='act')
                    else:
                        k.tt(obuf[0][:, tc], pso[:, 0:128], ofw[0][:, tc], ALU.add)
                        k.tt(obuf[1][0:64, tc], pso[0:64, 128:256], ofw[1][0:64, tc], ALU.add)

                def merged(gens):
                    alive = list(gens)
                    while alive:
                        for g_ in list(alive):
                            try:
                                next(g_)
                            except StopIteration:
                                alive.remove(g_)

                merged([Gstage(tiles[0], 0)])
                for ti, t in enumerate(tiles):
                    Mstage(t)
                    gens = [Sstage(t, ti % 2)]
                    if ti + 1 < len(tiles):
                        gens.append(Gstage(tiles[ti + 1], (ti + 1) % 2))
                    merged(gens)
                if d == 0:
                    for i in range(2):
                        k.dma(ofw_d[i][:, s:s + 512], obuf[i].v, q='pool')
                else:
                    if io.get('gd_dbg') is not None:
                        for i in range(2):
                            k.dma(io['gd_dbg'][i * 128:(i + 1) * 128, s:s + 512], obuf[i].v)
                    dst = mix_dest(blk)
                    for i in range(2):
                        rows = 128 if i == 0 else 64
                        k.tt(sq.v, obuf[i].v, obuf[i].v, ALU.mult)
                        pn = PS[2]
                        k.mm(pn.v, c['bd64'].v, sq.v)
                        k.act(rn.v, pn.v, AF.Sqrt, bias=c['eps'].v, scale=1.0 / 64.0)
                        k.recip(rn.v, rn.v)
                        k.stt(sq[0:rows, :], obuf[i][0:rows, :], gnorm[0:rows, :], rn[0:rows, :], ALU.mult, ALU.mult)
                        k.tt(dst[i], sq[0:rows, :], zs[i][0:rows, :], ALU.mult)
                    mix_dest(blk, done=True)
        barrier(k)
        k.es = old


O_AQ, O_AK, O_AV, O_AZ, O_AB, O_AD = 0, 384, 768, 1152, 1536, 1548
O_BQ, O_BK, O_BV = 1560, 1944, 2072
O_CF, O_CI, O_CQ, O_CG = 2200, 2712, 2968, 3224

def host_consts():
    c = {}
    c['ident'] = np.eye(128, dtype=np.float32)
    bd = np.zeros((128,128), np.float32); bd[:64,:64]=1; bd[64:,64:]=1
    c['bd64'] = bd
    rt = np.zeros((128,128), np.float32)
    for b in range(4):
        for i in range(16):
            rt[b*32+i+16, b*32+i] = -1.0
            rt[b*32+i, b*32+i+16] = 1.0
    c['rt128'] = rt
    s = np.arange(4096)
    pos = np.stack([(s//64).astype(np.float32), (s%64).astype(np.float32)], 0)
    inv = (np.float32(10000.0) ** (-np.arange(0, 32, 2, dtype=np.float32) / np.float32(32))).astype(np.float32)
    d = np.arange(64)
    ang = (pos[d//32][:, :] * inv[d%16][:, None]).astype(np.float32)
    c['cos'] = np.ascontiguousarray(np.concatenate([np.cos(ang), np.cos(ang)], 0).astype(np.float32))
    c['sin'] = np.ascontiguousarray(np.concatenate([np.sin(ang), np.sin(ang)], 0).astype(np.float32))
    return c

def gqa_w(w_in_l, qw, kw, hf):
    z64 = np.zeros((1024,64), np.float32)
    bq = lambda h: w_in_l[:, O_BQ+h*64:O_BQ+(h+1)*64]
    bk = w_in_l[:, O_BK+hf*64:O_BK+(hf+1)*64]
    wg = np.concatenate([bq(3*hf), bq(3*hf+1), bk, bk, bq(3*hf+2), z64], 1)
    wbv = w_in_l[:, O_BV+hf*64:O_BV+(hf+1)*64]
    gnw = np.stack([np.concatenate([qw,qw]), np.concatenate([kw,kw]), np.concatenate([qw, np.zeros(64,np.float32)])], 1)
    return dict(wg=np.ascontiguousarray(wg), wbv=np.ascontiguousarray(wbv), gnw=np.ascontiguousarray(gnw.astype(np.float32)))

def scan_consts():
    c = {}
    idx = np.arange(128); ch = idx // 64
    same = ch[:, None] == ch[None, :]
    for sfx in ('f', 'b'):
        if sfx == 'f':
            tri = same & (idx[None, :] <= idx[:, None])
            mid = ch * 64 + 31
        else:
            tri = same & (idx[None, :] >= idx[:, None])
            mid = ch * 64 + 32
        tri = tri.astype(np.float32)
        trirel = tri - tri[mid, :]
        cext = np.zeros((128, 4), np.float32)
        for cc in range(2):
            cext[:, cc] = tri[cc * 64 + (31 if sfx == 'f' else 32), :]
            cext[:, 2 + cc] = (ch == cc).astype(np.float32)
        c['trt_' + sfx] = np.ascontiguousarray(trirel.T)
        c['trx_' + sfx] = np.ascontiguousarray(np.concatenate([trirel.T, cext, np.zeros((128, 124), np.float32)], 1))
        c['mki_' + sfx] = np.ascontiguousarray(tri.T)
        c['tria_' + sfx] = np.ascontiguousarray(tri.T)
    return c

def hgrn_w(w_in_l, lbl, hnw, hf):
    hs = [2*hf, 2*hf+1]
    def cols(o): return np.concatenate([w_in_l[:, o+h*64:o+(h+1)*64] for h in hs], 1)
    wfm = np.concatenate([cols(O_CF), cols(O_CF+256), cols(O_CQ), cols(O_CG)], 1)
    wtm = np.concatenate([cols(O_CF), cols(O_CI), cols(O_CF+256)], 1)
    ch = np.concatenate([np.arange(h*64,(h+1)*64) for h in hs])
    return dict(wh_fm=np.ascontiguousarray(wfm), wh_tm=np.ascontiguousarray(wtm),
                lbl_col=np.ascontiguousarray(lbl[:, ch].T.astype(np.float32)), lbl_row=np.ascontiguousarray(lbl[:, ch].astype(np.float32)),
                hnw=np.ascontiguousarray(np.concatenate([hnw,hnw])[:,None].astype(np.float32)))

def gdn_consts():
    c = {}
    idx = np.arange(128); ch = idx // 64
    same = ch[:, None] == ch[None, :]
    bd = same.astype(np.float32)
    for sfx in ('f', 'b'):
        if sfx == 'f':
            incl = same & (idx[None, :] <= idx[:, None])
            strict = same & (idx[None, :] < idx[:, None])
        else:
            incl = same & (idx[None, :] >= idx[:, None])
            strict = same & (idx[None, :] > idx[:, None])
        c['tb_' + sfx] = np.ascontiguousarray(np.concatenate([incl.T.astype(np.float32), bd], 1))
        c['negs_' + sfx] = np.where(strict, 0.0, -30000.0).astype(np.float32)
        c['negi_' + sfx] = np.where(incl.T, 0.0, -30000.0).astype(np.float32)
    c['sel3'] = np.repeat(np.eye(3, dtype=np.float32), 128, axis=1)
    return c

def gdn_w(w_in_l, conv_w_l, a_log_l, dt_bias_l, gnw, hf):
    hs = [3*hf, 3*hf+1, 3*hf+2]
    q = lambda h: O_AQ + h*64; kk = lambda h: O_AK + h*64; v = lambda h: O_AV + h*64
    chans = [q(hs[0]), q(hs[1]), kk(hs[0]), kk(hs[1]), q(hs[2]), v(hs[2]), kk(hs[2]), None, v(hs[0]), v(hs[1])]
    wc = np.zeros((1024, 640), np.float32); cw = np.zeros((5, 640), np.float32)
    for u, o in enumerate(chans):
        if o is None: continue
        wc[:, u*64:(u+1)*64] = w_in_l[:, o:o+64]
        cw[:, u*64:(u+1)*64] = conv_w_l[:, o:o+64]
    wz = np.zeros((1024, 256), np.float32)
    for u, h in enumerate(hs):
        wz[:, u*64:(u+1)*64] = w_in_l[:, O_AZ + h*64:O_AZ + (h+1)*64]
    cols = [O_AB + 0*6 + h for h in hs] + [O_AD + 0*6 + h for h in hs] + [O_AB + 6 + h for h in hs] + [O_AD + 6 + h for h in hs]
    wgt = np.ascontiguousarray(w_in_l[:, cols])
    gpar = np.concatenate([a_log_l[0, hs], a_log_l[1, hs], dt_bias_l[0, hs], dt_bias_l[1, hs]]).astype(np.float32)
    return dict(wc=wc, cw=cw, wz=wz, wgt=wgt, gpar=gpar, gnorm=np.concatenate([gnw, gnw])[:, None].astype(np.float32))


A_INPUTS = {
    'wc': [1024, 640], 'cw': [5, 640], 'wz': [1024, 256], 'wgt': [1024, 12], 'gpar': [12], 'gnorm': [128, 1],
    'wh_fm': [1024, 512], 'wh_tm': [1024, 384], 'hnw': [128, 1], 'wg': [1024, 384], 'wbv': [1024, 64], 'gnw': [128, 3],
}
A_CONSTS = {
    'ident': [128, 128], 'bd64': [128, 128], 'rt128': [128, 128], 'cos': [128, SEQ], 'sin': [128, SEQ], 'sel3': [3, 384],
    'tb_f': [128, 256], 'negs_f': [128, 128], 'negi_f': [128, 128], 'tb_b': [128, 256], 'negs_b': [128, 128], 'negi_b': [128, 128],
    'trx_f': [128, 256], 'trt_f': [128, 128], 'mki_f': [128, 128], 'trx_b': [128, 256], 'trt_b': [128, 128], 'mki_b': [128, 128],
    'sel8': [8, 1024], 'lbl_col': [128, 2], 'lbl_row': [2, 128], 'selw': [128, 2], 'memT': [1024, 256],
}
B_INPUTS = {'w_out': [1024, 1024], 'xq': [1024, 1024], 'xk': [1024, 1024], 'xv': [1024, 1024], 'xo': [1024, 1024],
            'ln1_g': [1024], 'ln1_b': [1024], 'ln2_g': [1024], 'ln2_b': [1024], 'ln3_g': [1024], 'ln3_b': [1024]}
B_DENSE = {'ffn_wg': [1024, 2816], 'ffn_wu': [1024, 2816], 'ffn_wd': [2816, 1024]}
B_MOE = {'router': [1024, 8], 'moe_wg': [8, 1024, 3584], 'moe_wu': [8, 1024, 3584], 'moe_wd': [8, 3584, 1024]}
DEPTH = 2


def phase_a(k, io, layer, xsrc, out_view, PS):
    with ExitStack() as sa:
        old = k.es
        k.es = sa
        c = consts_a(k, io)
        xT = k.sb([128, 8, SEQ + 2 * PAD], BF16)
        load_xT(k, xT, xsrc)
        mt = [k.sb([128, 2, 512], BF16) for _ in range(2)]
        for m_ in mt:
            k.memset(m_.v, 0.0)

        def gd_dest(blk, done=False):
            m_ = mt[blk % 2]
            if not done:
                return (m_[:, 0, :], m_[0:64, 1, :])
            k.dma(out_view(0, 128, blk * 512, (blk + 1) * 512), m_[:, 0, :], q='pool')
            k.dma(out_view(128, 192, blk * 512, (blk + 1) * 512), m_[0:64, 1, :], q='pool')

        def hg_dest(blk, done=False):
            m_ = mt[blk % 2]
            if not done:
                return m_[:, 0, :]
            k.dma(out_view(256, 384, blk * 512, (blk + 1) * 512), m_[:, 0, :])

        mtq = [k.sb([128, 2, 512], BF16) for _ in range(2)]

        def gq_dest(blk, done=False):
            m_ = mtq[blk % 2]
            if not done:
                return [m_[0:64, 0, :], m_[64:128, 0, :], m_[64:128, 1, :]]
            k.dma(out_view(384, 512, blk * 512, (blk + 1) * 512), m_[:, 0, :])
            k.dma(out_view(192, 256, blk * 512, (blk + 1) * 512), m_[64:128, 1, :])

        gdn(k, io, xT, c, PS, layer, gd_dest)
        with ExitStack() as sg:
            old2 = k.es
            k.es = sg
            attn = gqa_prepare(k, io, xT, c, PS, gq_dest)
            gen = attn([PS[0], PS[1], PS[4]])

            def tick(n):
                for _ in range(n):
                    next(gen, None)
            hgrn(k, io, xT, c, PS, layer, hg_dest, tick=tick)
            for _ in gen:
                pass
            barrier(k)
            k.es = old2
        barrier(k)
        k.es = old


def build_fused():
    nc = bass.Bass("TRN2", target_bir_lowering=False)
    names = []
    with ExitStack() as es:
        k = K(nc, es)

        def inp(n, shape, dt=F32):
            names.append(n)
            return k.dram(n, shape, dt, kind='ExternalInput')
        cst = {n: inp(n, s) for n, s in A_CONSTS.items()}
        xT0 = inp('xT0', [1024, SEQ])
        x0 = inp('x0', [TOK, 1024])
        out_final = k.dram('out', [TOK, 1024], F32, kind='ExternalOutput')
        PS = [k.ps([128, 512]) for _ in range(8)]
        x_res = x0
        xg = None
        for l in range(DEPTH):
            moe = (l % 2 == 1)
            last = (l == DEPTH - 1)
            ioa = dict(cst)
            for n, s in A_INPUTS.items():
                ioa[n] = inp('%s_l%d' % (n, l), s)
            if l == 0:
                xsrc = lambda kc, hh: xT0[kc * 128:(kc + 1) * 128, hh * 2048:(hh + 1) * 2048]
            else:
                xsrc = (lambda g: (lambda kc, hh: g[kc // 4][hh * 512 + (kc % 4) * 128:hh * 512 + (kc % 4 + 1) * 128, :]))(xg)
            mixh = [k.dram('mixh%d_l%d' % (j, l), [256, SEQ], BF16) for j in range(2)]
            mixf = [k.dram('mixf%d_l%d' % (j, l), [512, SEQ], BF16) for j in range(2)]
            with nc.named_scope('A%d' % l):
                phase_a(k, ioa, l, xsrc, lambda r0, r1, c0, c1: mixh[r0 // 256][r0 % 256:r0 % 256 + (r1 - r0), c0:c1], PS)
            with nc.named_scope('cc_mix%d' % l):
                for j in range(2):
                    k.allgather_pairs(mixf[j], mixh[j])
            iob = {'ident': cst['ident'], 'memT': cst['memT'], 'sel8': cst['sel8'], 'x': x_res}
            spec = dict(B_INPUTS)
            spec.update(B_MOE if moe else B_DENSE)
            for n, s in spec.items():
                iob[n] = inp('%s_l%d' % (n, l), s)
            with ExitStack() as sb_:
                old = k.es
                k.es = sb_

                def mix_loader(bufA, mixf=mixf):
                    with ExitStack() as sl:
                        o2 = k.es
                        k.es = sl
                        bufA2 = k.sb([128, 8, TOK], BF16)
                        selw = k.sb([128, 2])
                        k.dma(selw.v, cst['selw'].v)
                        for th, buf in enumerate([bufA, bufA2]):
                            for kc in range(8):
                                hf, i = kc // 4, kc % 4
                                r0 = hf * 256 + (i % 2) * 128
                                k.dma(buf[:, kc, :], mixf[i // 2][r0:r0 + 128, th * TOK:(th + 1) * TOK])
                        for kc in range(8):
                            k.ts(bufA2[:, kc, :], bufA2[:, kc, :], selw[:, 1:2], None, op0=ALU.mult)
                            k.stt(bufA[:, kc, :], bufA[:, kc, :], selw[:, 0:1], bufA2[:, kc, :], ALU.mult, ALU.add)
                        barrier(k)
                        k.es = o2
                iob['mix_loader'] = mix_loader
                if last:
                    iob['out'] = out_final
                else:
                    x_next = k.dram('x3_l%d' % l, [TOK, 1024])
                    x3T = [k.dram('x3T%d_l%d' % (j, l), [512, TOK], BF16) for j in range(2)]
                    iob['out'] = x_next

                    def outT_writer(tt, oT_, x3T=x3T):
                        for j in range(2):
                            k.dma(V(x3T[j], x3T[j].h[:, tt * 128:(tt + 1) * 128].rearrange("(kc p) t -> p kc t", p=128)),
                                  oT_[:, j * 4:(j + 1) * 4, :], q='pool')
                    iob['outT'] = outT_writer
                with nc.named_scope('B%d' % l):
                    phase_b(k, sb_, iob, moe, PS=PS, tag='_l%d' % l)
                barrier(k)
                k.es = old
            if not last:
                xg = [k.dram('xg%d_l%d' % (j, l), [1024, TOK], BF16) for j in range(2)]
                for j in range(2):
                    k.allgather_pairs(xg[j], x3T[j])
                x_res = x_next
        k.finish()
    return nc, names


def mix_perm():
    oa = lambda h: list(range(h * 64, (h + 1) * 64))
    ob = lambda h: list(range(384 + h * 64, 384 + (h + 1) * 64))
    oc = lambda h: list(range(768 + h * 64, 768 + (h + 1) * 64))
    p = []
    for hf in range(2):
        p += oa(3 * hf) + oa(3 * hf + 1) + oa(3 * hf + 2) + ob(3 * hf + 2) + oc(2 * hf) + oc(2 * hf + 1) + ob(3 * hf) + ob(3 * hf + 1)
    return np.array(p)


def kernel(**inp):
    inp = {n: np.asarray(v) for n, v in inp.items()}
    x = np.ascontiguousarray(inp['x'], dtype=np.float32)
    B = x.shape[0]
    C = host_consts()
    C.update(scan_consts())
    C.update(gdn_consts())
    C['sel8'] = np.repeat(np.eye(8, dtype=np.float32), 128, axis=1)
    perm = mix_perm()
    nc, names = build_fused()
    cores = list(range(8))
    shared = {}
    for l in range(DEPTH):
        for n in ['xq', 'xk', 'xv', 'xo', 'ln1_g', 'ln1_b', 'ln2_g', 'ln2_b', 'ln3_g', 'ln3_b']:
            shared['%s_l%d' % (n, l)] = inp[n][l]
        shared['w_out_l%d' % l] = inp['w_out'][l][perm]
        if l % 2 == 1:
            shared['router_l%d' % l] = inp['moe_router'][l // 2]
            for n in ['moe_wg', 'moe_wu', 'moe_wd']:
                shared['%s_l%d' % (n, l)] = inp[n][l // 2]
        else:
            for n in ['ffn_wg', 'ffn_wu', 'ffn_wd']:
                shared['%s_l%d' % (n, l)] = inp[n][l // 2]
    shared = {n: np.ascontiguousarray(v, dtype=np.float32) for n, v in shared.items()}
    per_hf = []
    for hf in range(2):
        m = {}
        for l in range(DEPTH):
            w = {}
            w.update(gdn_w(inp['w_in'][l], inp['conv_w'][l], inp['gdn_a_log'][l], inp['gdn_dt_bias'][l], inp['gdn_norm_w'][l], hf))
            hw = hgrn_w(inp['w_in'][l], inp['hgrn_lb_logits'], inp['hgrn_norm_w'][l], hf)
            m['lbl_col'] = hw.pop('lbl_col'); m['lbl_row'] = hw.pop('lbl_row')
            w.update(hw)
            w.update(gqa_w(inp['w_in'][l], inp['q_norm_w'][l], inp['k_norm_w'][l], hf))
            for n, v in w.items():
                m['%s_l%d' % (n, l)] = np.ascontiguousarray(v, dtype=np.float32)
        per_hf.append(m)
    maps = []
    for core in cores:
        b, r = core // 2, core % 2
        m = dict(C)
        m.update(shared)
        m.update(per_hf[r])
        m['xT0'] = np.ascontiguousarray(x[b].T)
        m['x0'] = np.ascontiguousarray(x[b, r * TOK:(r + 1) * TOK])
        m['memT'] = np.ascontiguousarray(inp['mem'][b].T.astype(np.float32))
        sw = np.zeros((128, 2), np.float32); sw[:, r] = 1.0
        m['selw'] = sw
        maps.append({n: m[n] for n in names})
    res = run_bass_kernel_spmd(nc, maps, core_ids=cores)
    out = np.empty((B, SEQ, 1024), np.float32)
    for core in cores:
        b, r = core // 2, core % 2
        out[b, r * TOK:(r + 1) * TOK] = res.results[core]['out']
    return out
```
